# Optimizing a Trainium2 kernel written in Bass

```python
import jax, jax.numpy as jnp
from jax import lax
import numpy as np

D_MODEL = 1024
BATCH = 16
SEQ = 2048
DEPTH = 2

CHUNK = 64
N_MIXERS = 2
N_GLA_LAYERS = (DEPTH + 1) // 2
N_LRU_LAYERS = DEPTH // 2
EPS = 1e-6

GLA_HEADS = 4
GLA_DK = D_MODEL // 2 // GLA_HEADS
GLA_DV = D_MODEL // GLA_HEADS
GLA_QK = GLA_HEADS * GLA_DK
GLA_VD = GLA_HEADS * GLA_DV
GLA_GATE_RANK = 16
GLA_GATE_TAU = 16.0
GLA_IN = 2 * GLA_QK + 2 * GLA_VD + GLA_GATE_RANK

LRU_WIDTH = D_MODEL
LRU_BLOCKS = 4
LRU_BLOCK_W = LRU_WIDTH // LRU_BLOCKS
CONV_W = 4
LRU_C = 8.0

N_EXPERTS = 16
N_GROUPS = 4
EXPERTS_PER_GROUP = N_EXPERTS // N_GROUPS
TOP_K = 2
D_EXPERT = 512

kernel_name = "hybrid_gla_rglru_grouped_moe_adaln"


def rmsnorm(x, g):
    x32 = x.astype(jnp.float32)
    inv = lax.rsqrt(jnp.mean(x32 * x32, axis=-1, keepdims=True) + EPS)
    return (x32 * inv).astype(x.dtype) * g


def modulate(h, shift, scale):
    return h * (1 + scale[:, None]) + shift[:, None]


def gla_mixer(h, w_in, w_gate_up, b_gate, norm_g, w_out):
    B, S, _ = h.shape
    N = S // CHUNK
    proj = h @ w_in
    q, k, v, g, a_lr = jnp.split(
        proj, [GLA_QK, 2 * GLA_QK, 2 * GLA_QK + GLA_VD, 2 * GLA_QK + 2 * GLA_VD], axis=-1)
    log_a = jax.nn.log_sigmoid((a_lr @ w_gate_up + b_gate).astype(jnp.float32)) / GLA_GATE_TAU

    def chunked(t, d):
        return t.reshape(B, N, CHUNK, GLA_HEADS, d)

    q = chunked(q, GLA_DK) * (GLA_DK ** -0.5)
    k = chunked(k, GLA_DK)
    v = chunked(v, GLA_DV)
    cum = jnp.cumsum(chunked(log_a, GLA_DK), axis=2)
    total = cum[:, :, -1]
    k_dec = k * jnp.exp(total[:, :, None] - cum).astype(k.dtype)
    gamma = jnp.exp(total).astype(h.dtype)

    def step(s_prev, xs):
        qn, kn, vn, gn = xs
        inter = jnp.einsum('bchk,bhkv->bchv', qn * gn[:, None], s_prev)
        scores = jnp.einsum('bchk,bshk->bhcs', qn, kn)
        intra = jnp.einsum('bhcs,bshv->bchv', scores, vn)
        s_new = gn[..., None] * s_prev + jnp.einsum('bshk,bshv->bhkv', kn, vn)
        return s_new, inter + intra

    s0 = jnp.zeros((B, GLA_HEADS, GLA_DK, GLA_DV), h.dtype)
    xs = (jnp.moveaxis(q, 1, 0), jnp.moveaxis(k_dec, 1, 0),
          jnp.moveaxis(v, 1, 0), jnp.moveaxis(gamma, 1, 0))
    _, o = lax.scan(step, s0, xs)
    o = jnp.moveaxis(o, 0, 1).reshape(B, S, GLA_HEADS, GLA_DV)
    o = rmsnorm(o, norm_g) * jax.nn.silu(g).reshape(B, S, GLA_HEADS, GLA_DV)
    return o.reshape(B, S, GLA_VD) @ w_out


def lru_mixer(h, w_in, conv_w, conv_b, w_r, b_r, w_i, b_i, lam, w_out):
    B, S, _ = h.shape
    proj = h @ w_in
    gate_br, xb = jnp.split(proj, 2, axis=-1)
    xp = jnp.pad(xb, ((0, 0), (CONV_W - 1, 0), (0, 0)))
    xc = conv_b + sum(xp[:, j:j + S] * conv_w[j] for j in range(CONV_W))
    xg = xc.reshape(B, S, LRU_BLOCKS, LRU_BLOCK_W)
    r = jax.nn.sigmoid(jnp.einsum('bshi,hij->bshj', xg, w_r) + b_r).reshape(B, S, LRU_WIDTH)
    i = jax.nn.sigmoid(jnp.einsum('bshi,hij->bshj', xg, w_i) + b_i).reshape(B, S, LRU_WIDTH)
    log_a = (-LRU_C * r.astype(jnp.float32)) * jax.nn.softplus(-lam.astype(jnp.float32))
    a = jnp.exp(log_a)
    mult = jnp.sqrt(-jnp.expm1(2.0 * log_a))
    b = (xc * i).astype(jnp.float32) * mult

    def combine(left, right):
        a1, b1 = left
        a2, b2 = right
        return a1 * a2, a2 * b1 + b2

    _, hs = lax.associative_scan(combine, (a, b), axis=1)
    y = hs.astype(h.dtype) * jax.nn.gelu(gate_br)
    return y @ w_out


def grouped_moe(h, router_w, router_bias, w_gate, w_up, w_down):
    B, S, D = h.shape
    t = h.reshape(-1, D)
    scores = jax.nn.sigmoid((t @ router_w).astype(jnp.float32))
    sel = scores + router_bias.astype(jnp.float32)
    grouped = sel.reshape(-1, N_GROUPS, EXPERTS_PER_GROUP)
    group_score = lax.top_k(grouped, TOP_K)[0].sum(-1)
    grp = jnp.argmax(group_score, axis=-1)
    in_group = jnp.take_along_axis(grouped, grp[:, None, None], axis=1)[:, 0]
    _, local = lax.top_k(in_group, TOP_K)
    expert_idx = grp[:, None] * EXPERTS_PER_GROUP + local
    w = jnp.take_along_axis(scores, expert_idx, axis=1)
    w = w / jnp.sum(w, axis=-1, keepdims=True)
    combine = jnp.sum(jax.nn.one_hot(expert_idx, N_EXPERTS, dtype=jnp.float32) * w[..., None], axis=1)
    combine = combine.astype(t.dtype)
    out = jnp.zeros_like(t)
    for e in range(N_EXPERTS):
        he = jax.nn.silu(t @ w_gate[e]) * (t @ w_up[e])
        out = out + combine[:, e:e + 1] * (he @ w_down[e])
    return out.reshape(B, S, D)


def setup_inputs(seed: int = 0) -> dict:
    key = jax.random.key(seed)
    ks = iter(jax.random.split(key, 40))
    f32 = jnp.float32

    def nrm(shape, scale):
        return jax.random.normal(next(ks), shape, f32) * scale

    def gain(shape):
        return 1.0 + nrm(shape, 0.05)

    D = D_MODEL
    p = {}
    p["x"] = nrm((BATCH, SEQ, D), 1.0)
    p["c"] = nrm((BATCH, D), 1.0)
    p["gla_w_in"] = nrm((N_GLA_LAYERS, D, GLA_IN), D ** -0.5)
    p["gla_w_gate_up"] = nrm((N_GLA_LAYERS, GLA_GATE_RANK, GLA_QK), GLA_GATE_RANK ** -0.5)
    p["gla_b_gate"] = nrm((N_GLA_LAYERS, GLA_QK), 0.1)
    p["gla_norm_g"] = gain((N_GLA_LAYERS, GLA_DV))
    p["gla_w_out"] = nrm((N_GLA_LAYERS, GLA_VD, D), GLA_VD ** -0.5)
    p["lru_w_in"] = nrm((N_LRU_LAYERS, D, 2 * LRU_WIDTH), D ** -0.5)
    p["lru_conv_w"] = nrm((N_LRU_LAYERS, CONV_W, LRU_WIDTH), CONV_W ** -0.5)
    p["lru_conv_b"] = nrm((N_LRU_LAYERS, LRU_WIDTH), 0.02)
    p["lru_w_r"] = nrm((N_LRU_LAYERS, LRU_BLOCKS, LRU_BLOCK_W, LRU_BLOCK_W), LRU_BLOCK_W ** -0.5)
    p["lru_b_r"] = nrm((N_LRU_LAYERS, LRU_BLOCKS, LRU_BLOCK_W), 0.02)
    p["lru_w_i"] = nrm((N_LRU_LAYERS, LRU_BLOCKS, LRU_BLOCK_W, LRU_BLOCK_W), LRU_BLOCK_W ** -0.5)
    p["lru_b_i"] = nrm((N_LRU_LAYERS, LRU_BLOCKS, LRU_BLOCK_W), 0.02)
    u = jax.random.uniform(next(ks), (N_LRU_LAYERS, LRU_WIDTH), f32, 0.9, 0.999)
    a_base = u ** (1.0 / LRU_C)
    p["lru_lambda"] = jnp.log(a_base) - jnp.log1p(-a_base)
    p["lru_w_out"] = nrm((N_LRU_LAYERS, LRU_WIDTH, D), LRU_WIDTH ** -0.5)
    p["router_w"] = nrm((D, N_EXPERTS), D ** -0.5)
    p["router_bias"] = nrm((N_EXPERTS,), 0.01)
    p["moe_w_gate"] = nrm((DEPTH, N_EXPERTS, D, D_EXPERT), D ** -0.5)
    p["moe_w_up"] = nrm((DEPTH, N_EXPERTS, D, D_EXPERT), D ** -0.5)
    p["moe_w_down"] = nrm((DEPTH, N_EXPERTS, D_EXPERT, D), D_EXPERT ** -0.5)
    p["norm_mix_g"] = gain((DEPTH, D))
    p["norm_ffn_g"] = gain((DEPTH, D))
    p["ada_w"] = nrm((DEPTH, D, 6 * D), 0.5 * D ** -0.5)
    p["ada_b"] = nrm((DEPTH, 6 * D), 0.02)
    p["final_norm_g"] = gain((D,))
    return p


def reference(x, c, gla_w_in, gla_w_gate_up, gla_b_gate, gla_norm_g, gla_w_out,
              lru_w_in, lru_conv_w, lru_conv_b, lru_w_r, lru_b_r, lru_w_i, lru_b_i,
              lru_lambda, lru_w_out, router_w, router_bias, moe_w_gate, moe_w_up,
              moe_w_down, norm_mix_g, norm_ffn_g, ada_w, ada_b, final_norm_g):
    cond = jax.nn.silu(c)
    for i in range(DEPTH):
        sh1, sc1, g1, sh2, sc2, g2 = jnp.split(cond @ ada_w[i] + ada_b[i], 6, axis=-1)
        h = modulate(rmsnorm(x, norm_mix_g[i]), sh1, sc1)
        j = i // N_MIXERS
        if i % N_MIXERS == 0:
            mix = gla_mixer(h, gla_w_in[j], gla_w_gate_up[j], gla_b_gate[j],
                            gla_norm_g[j], gla_w_out[j])
        else:
            mix = lru_mixer(h, lru_w_in[j], lru_conv_w[j], lru_conv_b[j], lru_w_r[j],
                            lru_b_r[j], lru_w_i[j], lru_b_i[j], lru_lambda[j], lru_w_out[j])
        x = x + g1[:, None] * mix
        h = modulate(rmsnorm(x, norm_ffn_g[i]), sh2, sc2)
        x = x + g2[:, None] * grouped_moe(h, router_w, router_bias,
                                          moe_w_gate[i], moe_w_up[i], moe_w_down[i])
    return rmsnorm(x, final_norm_g)
```

```python
import numpy as np
import concourse.bass as bass
import concourse.mybir as mybir
from concourse.bass_utils import run_bass_kernel_spmd

F32 = mybir.dt.float32
BF16 = mybir.dt.bfloat16
AF = mybir.ActivationFunctionType
ALU = mybir.AluOpType
ESZ = {F32: 4, BF16: 2}

D = 1024
S = 2048
NB = 4
TB = 512
NE = 16
DE = 512
EPS = 1e-6
GRAN = 512
N_CORES = 8


class Op:
    __slots__ = ("eng", "idx", "fn", "deps", "dma", "dsem", "dval", "sig", "sigval", "waits", "gidx")


class Sched:
    ENGS = ("pe", "act", "dve", "pool", "sp")
    NDS = 12

    def __init__(self, nc):
        self.nc = nc
        self.ops = {e: [] for e in self.ENGS}
        self.last_w = {}
        self.readers = {}
        self.ndma = {e: 0 for e in self.ENGS}
        self.dma_ops = {e: [] for e in self.ENGS}
        self.tok_cache = {}
        self.gcount = 0

    def tokens(self, ap):
        sp = str(ap.space)
        if "SB" not in sp and "PSUM" not in sp:
            return ()
        if "PSUM" in sp:
            return ((ap.tensor.name, 0),)
        key = (ap.tensor.name, ap.offset, ap.ap, ap.dtype)
        t = self.tok_cache.get(key)
        if t is None:
            es = ESZ[ap.dtype]
            pstride = ap.ap[0][0]
            off = ap.offset % pstride if pstride > 0 else ap.offset
            name = ap.tensor.name
            dims = [(abs(st), cnt) for st, cnt in ap.ap[1:] if cnt > 1 and st != 0]
            dims.sort(reverse=True)
            outer = dims[:-1] if dims else []
            n_outer = 1
            for _, cnt in outer:
                n_outer *= cnt
            gs = set()
            if dims and n_outer <= 512:
                lst, lcnt = dims[-1]
                bases = [off]
                for st, cnt in outer:
                    bases = [b0 + i * st for b0 in bases for i in range(cnt)]
                for b0 in bases:
                    lo = b0 * es
                    hi = (b0 + (lcnt - 1) * lst + 1) * es
                    gs.update(range(lo // GRAN, (hi - 1) // GRAN + 1))
            else:
                span = 0
                for st, cnt in dims:
                    span += (cnt - 1) * st
                lo = off * es
                hi = (off + span + 1) * es
                gs.update(range(lo // GRAN, (hi - 1) // GRAN + 1))
            t = tuple((name, g) for g in sorted(gs))
            self.tok_cache[key] = t
        return t

    def add(self, eng, fn, reads=(), writes=(), dma=False):
        op = Op()
        op.eng = eng
        op.idx = len(self.ops[eng])
        op.fn = fn
        op.dma = dma
        op.sig = False
        op.sigval = 0
        op.gidx = self.gcount
        self.gcount += 1
        deps = {}
        rt = []
        for ap in reads:
            rt.extend(self.tokens(ap))
        wt = []
        for ap in writes:
            wt.extend(self.tokens(ap))
        for r in rt:
            w = self.last_w.get(r)
            if w is not None:
                deps[id(w)] = w
        for r in wt:
            w = self.last_w.get(r)
            if w is not None:
                deps[id(w)] = w
            rd = self.readers.get(r)
            if rd:
                for o in rd.values():
                    deps[id(o)] = o
        deps.pop(id(op), None)
        op.deps = list(deps.values())
        for r in rt:
            d = self.readers.get(r)
            if d is None:
                d = {}
                self.readers[r] = d
            if dma:
                d[(eng, op.idx)] = op
            else:
                d[eng] = op
        for r in wt:
            self.last_w[r] = op
            self.readers[r] = {}
        if dma:
            i = self.ndma[eng]
            self.ndma[eng] = i + 1
            op.dsem = i % self.NDS
            op.dval = 16 * (i // self.NDS + 1)
            if i >= self.NDS:
                op.deps.append(self.dma_ops[eng][i - self.NDS])
            self.dma_ops[eng].append(op)
        self.ops[eng].append(op)
        return op

    def finalize(self):
        for eng in self.ENGS:
            known = {}
            for op in self.ops[eng]:
                need = {}
                for d in op.deps:
                    if d.dma:
                        k = ("d", d.eng, d.dsem)
                        v = d.dval
                        if v > need.get(k, (0, None))[0]:
                            need[k] = (v, d)
                    else:
                        if d.eng == "pe" and eng == "pe" and not op.dma:
                            continue
                        k = ("c", d.eng)
                        v = d.idx + 1
                        if v > need.get(k, (0, None))[0]:
                            need[k] = (v, d)
                waits = []
                for k, (v, d) in need.items():
                    if known.get(k, 0) >= v:
                        continue
                    known[k] = v
                    waits.append(d)
                    if not d.dma:
                        d.sig = True
                op.waits = waits
        for eng in self.ENGS:
            c = 0
            for op in self.ops[eng]:
                if op.sig and not op.dma:
                    c += 1
                    op.sigval = c

    def emit(self, eng, e, csem, dsems):
        for op in self.ops[eng]:
            for d in op.waits:
                if d.dma:
                    e.wait_ge(dsems[d.eng][d.dsem], d.dval)
                else:
                    e.wait_ge(csem[d.eng], d.sigval)
            ins = op.fn(e)
            if op.dma:
                ins.then_inc(dsems[eng][op.dsem], 16)
            elif op.sig:
                ins.then_inc(csem[eng], 1)


def build_program(n_seq=2, dbg=None):
    dbg = dbg or {}
    nc = bass.Bass("TRN2", target_bir_lowering=False)
    Sd = Sched(nc)

    def din(name, shape):
        return nc.dram_tensor(name, list(shape), F32, kind="ExternalInput").ap()

    xT_d = din("xT", [2, D, S])
    cT_d = din("cT", [128, 16])
    ada_w_d = din("ada_w", [2, D, 6 * D])
    ada_b_d = din("ada_b", [128, 96])
    vecs_d = din("vecs", [128, NV])
    gla_w_in_d = din("gla_w_in", [D, 3088])
    gla_w_out_d = din("gla_w_out", [D, D])
    wgu_d = din("wgu", [17, 512])
    lru_w_in_d = din("lru_w_in", [D, 2 * D])
    lru_w_out_d = din("lru_w_out", [D, D])
    lru_wr_d = din("lru_wr", [128, 8 * 256])
    lru_wi_d = din("lru_wi", [128, 8 * 256])
    rw_d = din("rw", [128, 8 * 16])
    rb_d = din("rb", [128, 16])
    wg_d = din("moe_wg", [2, NE, D, DE])
    wu_d = din("moe_wu", [2, NE, D, DE])
    wd_d = din("moe_wd", [2, NE, DE, D])
    ident_d = din("ident", [128, 128])
    mtri_d = din("mtri", [128, 128])
    ind_d = din("ind", [128, 2])
    i16_d = din("i16", [16, 16])
    outT_d = nc.dram_tensor("outT", [2, D, S], F32, kind="ExternalOutput").ap()
    dbg_d = {}
    for k, shp in dbg.items():
        if k.startswith("_"):
            continue
        dbg_d[k] = nc.dram_tensor("dbg_" + k, list(shp), F32, kind="ExternalOutput").ap()

    import contextlib
    stack = contextlib.ExitStack()

    def SB(name, shape, dt=F32):
        return stack.enter_context(nc.sbuf_tensor("s_" + name, list(shape), dt))

    def PS(name):
        return stack.enter_context(nc.psum_tensor(name, [128, 512], F32))

    xT = SB("xT", [128, 8, S])
    hbuf = SB("hbuf", [128, 16384], BF16)
    W = SB("W", [128, 36864], BF16)
    scr = SB("scr", [128, 10240], BF16)
    cTr = SB("cTr", [128, 2048])
    ident = SB("ident", [128, 128])
    mtri = SB("mtri", [128, 128])
    ind = SB("ind", [128, 2])
    i32f = SB("i32f", [32, 16])
    i32b = SB("i32b", [32, 16], BF16)
    ones_d = SB("ones_d", [128, 128], BF16)
    ones_v = SB("ones_v", [128, 128], BF16)
    vecs = SB("vecs", [128, NV])
    cT_s = SB("cT_s", [128, 16])
    cond = SB("cond", [128, 16], BF16)
    ada_b = SB("ada_b", [128, 96])
    ada = SB("ada", [128, 2, 48, 2])
    mod = SB("mod", [128, 2, 4, 8, 2])
    rw = SB("rw", [128, 8, 16], BF16)
    rb = SB("rb", [128, 16])
    wgu = SB("wgu", [32, 512])
    smallw = SB("smallw", [128, 256])
    halo = SB("halo", [128, 8, 4])
    carry = SB("carry", [128, 8])
    rt = scr[:, 0:5120].bitcast(F32)

    ps = [PS("ps%d" % i) for i in range(8)]

    def mm(out, lhsT, rhs, start=True, stop=True):
        return Sd.add("pe", lambda e: e.matmul(out, lhsT, rhs, start=start, stop=stop),
                      reads=[lhsT, rhs], writes=[out])

    def tr(out, in_, idn):
        return Sd.add("pe", lambda e: e.transpose(out, in_, idn), reads=[in_, idn], writes=[out])

    def act(out, in_, func, bias=None, scale=None, eng="act"):
        rd = [in_]
        kw = {}
        if bias is not None:
            kw["bias"] = bias
            if not isinstance(bias, (int, float)):
                rd.append(bias)
        if scale is not None:
            kw["scale"] = scale
            if not isinstance(scale, (int, float)):
                rd.append(scale)
        return Sd.add("act", lambda e: e.activation(out, in_, func, **kw), reads=rd, writes=[out])

    def stt(out, in0, scalar, in1, op0, op1, eng="dve"):
        rd = [in0, in1]
        if not isinstance(scalar, (int, float)):
            rd.append(scalar)
        return Sd.add(eng, lambda e: e.scalar_tensor_tensor(out, in0, scalar, in1, op0, op1),
                      reads=rd, writes=[out])

    def ts(out, in0, s1, s2, op0, op1=None, eng="dve"):
        rd = [in0]
        for s in (s1, s2):
            if s is not None and not isinstance(s, (int, float)):
                rd.append(s)
        if op1 is None:
            return Sd.add(eng, lambda e: e.tensor_scalar(out, in0, s1, None, op0), reads=rd, writes=[out])
        return Sd.add(eng, lambda e: e.tensor_scalar(out, in0, s1, s2, op0, op1), reads=rd, writes=[out])

    def tt(out, in0, in1, op, eng="dve"):
        return Sd.add(eng, lambda e: e.tensor_tensor(out, in0, in1, op), reads=[in0, in1], writes=[out])

    def cp(out, in_, eng="dve"):
        return Sd.add(eng, lambda e: e.tensor_copy(out, in_), reads=[in_], writes=[out])

    def recip(out, in_):
        return Sd.add("dve", lambda e: e.reciprocal(out, in_), reads=[in_], writes=[out])

    def red(out, in_, op, eng="dve"):
        return Sd.add(eng, lambda e: e.tensor_reduce(out, in_, mybir.AxisListType.X, op),
                      reads=[in_], writes=[out])

    def scan(out, d0, d1, init, op0, op1):
        rd = [d0, d1]
        if not isinstance(init, (int, float)):
            rd.append(init)
        return Sd.add("dve", lambda e: e.tensor_tensor_scan(out, d0, d1, init, op0, op1),
                      reads=rd, writes=[out])

    def memset(ap, val, eng="dve"):
        return Sd.add(eng, lambda e: e.memset(ap, val), writes=[ap])

    def dma(q, out, in_, **kw):
        return Sd.add(q, lambda e: e.dma_start(out=out, in_=in_, **kw), reads=[in_], writes=[out], dma=True)

    def f32v(t, off_b, shape):
        n = int(np.prod(shape[1:]))
        v = t[0:shape[0], off_b // 2: off_b // 2 + 2 * n].bitcast(F32)
        if len(shape) == 3:
            v = v.rearrange("p (a b) -> p a b", a=shape[1])
        elif len(shape) == 4:
            v = v.rearrange("p (a b c) -> p a b c", a=shape[1], b=shape[2])
        return v

    def b16v(t, off_b, shape):
        n = int(np.prod(shape[1:]))
        v = t[0:shape[0], off_b // 2: off_b // 2 + n]
        if len(shape) == 3:
            v = v.rearrange("p (a b) -> p a b", a=shape[1])
        elif len(shape) == 4:
            v = v.rearrange("p (a b c) -> p a b c", a=shape[1], b=shape[2])
        return v

    def vcol(i):
        return vecs[:, i:i + 1]

    dma("sp", ident[:], ident_d)
    dma("sp", mtri[:], mtri_d)
    dma("sp", ind[:], ind_d)
    dma("sp", i32f[0:16, :], i16_d)
    dma("sp", i32f[16:32, :], i16_d)
    cp(i32b[:], i32f[:], eng="dve")
    dma("sp", vecs[:], vecs_d)
    dma("sp", cT_s[:], cT_d)
    dma("sp", ada_b[:], ada_b_d)
    dma("sp", rb[:], rb_d)
    dma("sp", wgu[0:17, :], wgu_d)
    dma("pool", rw[:].rearrange("p a b -> p (a b)"), rw_d)
    memset(ones_d[:], 1.0 / 1024.0)
    memset(ones_v[:], 1.0 / 256.0)
    act(cond[:], cT_s[:], AF.Silu)

    NPIECE = 12
    for l in range(2):
        for pc in range(NPIECE):
            slot = (l * NPIECE + pc) % 3
            wv = b16v(hbuf, slot * 8192, [128, 8, 512])
            dma("pool", wv, ada_w_d[l, :, pc * 512:(pc + 1) * 512].rearrange("(c p) n -> p c n", p=128))
            for oc4 in range(4):
                oc = pc * 4 + oc4
                for kc in range(8):
                    mm(ps[7][:, (l * 48 + oc) * 2:(l * 48 + oc) * 2 + 2],
                       wv[:, kc, oc4 * 128:(oc4 + 1) * 128],
                       cond[:, kc * 2:kc * 2 + 2], start=(kc == 0), stop=(kc == 7))
    for l in range(2):
        tt(ada[:, l, :, :], ps[7][:, l * 96:(l + 1) * 96].rearrange("p (a b) -> p a b", b=2),
           ada_b[:, l * 48:(l + 1) * 48].unsqueeze(2).to_broadcast([128, 48, 2]), ALU.add)
    for l in range(2):
        for j, (which, gcol) in enumerate(((1, V_NMG + l * 8), (4, V_NFG + l * 8))):
            ts(mod[:, l, j, :, :], ada[:, l, which * 8:(which + 1) * 8, :], 1.0, None, ALU.add)
            tt(mod[:, l, j, :, :], mod[:, l, j, :, :],
               vecs[:, gcol:gcol + 8].unsqueeze(2).to_broadcast([128, 8, 2]), ALU.mult)

    def A_of(l, j, c, b):
        return mod[:, l, j, c, b:b + 1]

    def ada_col(l, which, c, b):
        return ada[:, l, which * 8 + c, b:b + 1]

    lam = vecs[:, V_LAM:V_LAM + 8]
    sw_a = smallw[:, 0:8]
    sw_b = smallw[:, 8:16]
    cneg = smallw[:, 16:24]
    cneg2 = smallw[:, 24:32]
    ts(sw_b, lam, 0.0, None, ALU.min)
    stt(sw_a, sw_b, 2.0, lam, ALU.mult, ALU.subtract)
    act(sw_a, sw_a, AF.Exp)
    act(sw_a, sw_a, AF.Ln, bias=1.0)
    tt(sw_a, sw_a, sw_b, ALU.subtract)
    ts(cneg, sw_a, -8.0, None, ALU.mult)
    ts(cneg2, sw_a, -16.0, None, ALU.mult)

    def norm_a0(jb):
        t0 = jb * TB
        sq = b16v(scr, 0, [128, 8, 512])
        act(sq[:, :, :], xT[:, :, t0:t0 + TB], AF.Square)

    def norm_a(jb, b, l, j, npre=2, do_sq=True):
        t0 = jb * TB
        sq = b16v(scr, 0, [128, 8, 512])
        rstd = f32v(scr, 8192, [128, 512])
        tmp = [f32v(scr, 10240, [128, 512]), f32v(scr, 12288, [128, 512])]
        if do_sq:
            norm_a0(jb)
        for c in range(8):
            mm(ps[6][:], ones_d[:], sq[:, c, :], start=(c == 0), stop=(c == 7))
        act(rstd, ps[6][:], AF.Ln, bias=EPS)
        act(rstd, rstd, AF.Exp, scale=-0.5)
        for c in range(npre):
            stt(tmp[c % 2], xT[:, c, t0:t0 + TB], A_of(l, j, c, b), rstd, ALU.mult, ALU.mult)

    def norm_b(jb, b, l, j, shift_which, dst, npre=2):
        t0 = jb * TB
        rstd = f32v(scr, 8192, [128, 512])
        tmp = [f32v(scr, 10240, [128, 512]), f32v(scr, 12288, [128, 512])]
        for c in range(8):
            tm = tmp[c % 2]
            if c >= npre:
                stt(tm, xT[:, c, t0:t0 + TB], A_of(l, j, c, b), rstd, ALU.mult, ALU.mult)
            act(dst[:, c, :], tm, AF.Identity, bias=ada_col(l, shift_which, c, b))

    def norm_block(jb, b, l, j, shift_which, dst):
        norm_a(jb, b, l, j, npre=0)
        norm_b(jb, b, l, j, shift_which, dst, npre=0)

    def resid_update(pbank, m, jb, gcol):
        t0 = jb * TB
        stt(xT[:, m, t0:t0 + TB], pbank, gcol, xT[:, m, t0:t0 + TB], ALU.mult, ALU.add)

    wa = SB("wa", [128, 8, 128], BF16)
    memset(wa[:], 0.0)

    def gla_views():
        return dict(
            wout=b16v(W, 0, [128, 8, 1024]),
            Sst=f32v(W, 16384, [128, 4, 256]),
            Sb=b16v(W, 20480, [128, 4, 256]),
            gam=f32v(W, 22528, [128, 4, 32]),
            wq=b16v(W, 24576, [128, 8, 512]),
            wk=b16v(W, 32768, [128, 8, 512]),
            wv=b16v(W, 40960, [128, 8, 1024]),
            wg=b16v(W, 57344, [128, 8, 1024]),
        )

    def gla_prefetch():
        v = gla_views()
        src = gla_w_in_d.rearrange("(c p) n -> p c n", p=128)
        dma("pool", wa[:, :, 0:16], src[:, :, 3072:3088])
        dma("pool", v["wq"], src[:, :, 0:512])
        dma("pool", v["wg"], src[:, :, 2048:3072])
        dma("pool", v["wk"], src[:, :, 512:1024])
        dma("pool", v["wv"], src[:, :, 1024:2048])

    def gla_layer(b, l=0, after_proj=None):
        V = gla_views()
        wout, Sst, Sb, gam, wq, wk, wv, wg = (V[k] for k in ("wout", "Sst", "Sb", "gam", "wq", "wk", "wv", "wg"))
        dma("pool", wout, gla_w_out_d.rearrange("(c p) n -> p c n", p=128))
        memset(Sst, 0.0)
        hT = b16v(hbuf, 0, [128, 8, 512])
        qT = b16v(hbuf, 8192, [128, 4, 512])
        kdec = b16v(hbuf, 12288, [128, 4, 512])
        vv = b16v(hbuf, 16384, [128, 4, 1024])
        sgT = b16v(hbuf, 24576, [128, 8, 512])
        oT = f32v(scr, 0, [128, 8, 512])
        m4 = f32v(scr, 0, [128, 4, 512])
        u4 = f32v(scr, 8192, [128, 4, 512])
        alrT = f32v(scr, 16384, [32, 512])
        expD = f32v(scr, 18432, [128, 512])
        la = cTr[:, :].rearrange("p (a b) -> p a b", a=4)
        yT = hT
        rstd4 = f32v(hbuf, 8192, [128, 4, 512])
        sq2 = b16v(hbuf, 16384, [128, 8, 512])
        tn = [f32v(scr, 16384, [128, 512]), f32v(scr, 18432, [128, 512])]
        QS = 128.0 ** -0.5
        norm_a(0, b, l, 0)
        for jb in range(NB):
            t0 = jb * TB
            norm_b(jb, b, l, 0, 0, hT)
            for kc in range(8):
                mm(ps[2][:], wa[:, kc, :], hT[:, kc, :], start=(kc == 0), stop=(kc == 7))
            memset(alrT, 1.0)
            cp(alrT[0:16, :], ps[2][0:16, :], eng="dve")
            for tt_ in range(4):
                pz = ps[3 + tt_ % 2]
                mm(pz[:], alrT[0:17, tt_ * 128:(tt_ + 1) * 128], wgu[0:17, :])
                ts(m4[:, tt_, :], pz[:], 0.0, None, ALU.min)
                stt(u4[:, tt_, :], m4[:, tt_, :], 2.0, pz[:], ALU.mult, ALU.subtract)
            for hd in range(4):
                pb = ps[hd % 2]
                for kc in range(8):
                    mm(pb[:], wq[:, kc, hd * 128:(hd + 1) * 128], hT[:, kc, :], start=(kc == 0), stop=(kc == 7))
                act(qT[:, hd, :], pb[:], AF.Copy, scale=QS)
            for gc in range(8):
                pb = ps[gc % 2]
                for kc in range(8):
                    mm(pb[:], wg[:, kc, gc * 128:(gc + 1) * 128], hT[:, kc, :], start=(kc == 0), stop=(kc == 7))
                act(sgT[:, gc, :], pb[:], AF.Silu)
            act(u4[:, :, :], u4[:, :, :], AF.Exp)
            act(u4[:, :, :], u4[:, :, :], AF.Ln, bias=1.0)
            tt(la[:, :, :], m4[:, :, :], u4[:, :, :], ALU.subtract)
            for tt_ in range(4):
                mm(ps[3][:], mtri[:], la[:, tt_, :])
                for hd in range(4):
                    mm(ps[7][:, hd * 2:hd * 2 + 2], la[:, tt_, hd * 128:(hd + 1) * 128], ind[:])
                for kc in range(8):
                    mm(ps[4 + tt_ % 2][:], hT[:, kc, tt_ * 128:(tt_ + 1) * 128], wk[:, kc, :],
                       start=(kc == 0), stop=(kc == 7))
                act(expD, ps[3][:], AF.Exp)
                n0 = jb * 8 + tt_ * 2
                act(gam[:, :, n0:n0 + 2], ps[7][:, 0:8].rearrange("p (a b) -> p a b", b=2), AF.Exp)
                tt(kdec[:, tt_, :], ps[4 + tt_ % 2][:], expD, ALU.mult)
                for vh in range(2):
                    pb = ps[vh]
                    for kc in range(8):
                        mm(pb[:], hT[:, kc, tt_ * 128:(tt_ + 1) * 128], wv[:, kc, vh * 512:(vh + 1) * 512],
                           start=(kc == 0), stop=(kc == 7))
                    act(vv[:, tt_, vh * 512:(vh + 1) * 512], pb[:], AF.Copy)
            if jb == NB - 1 and after_proj is not None:
                after_proj()
            def kv_mm(n):
                tt_ = n // 2
                p0 = (n % 2) * 64
                kvb = (ps[0], ps[1]) if n % 2 == 0 else (ps[4], ps[5])
                for hd in range(4):
                    mm(kvb[hd // 2][:, (hd % 2) * 256:(hd % 2 + 1) * 256],
                       kdec[p0:p0 + 64, tt_, hd * 128:(hd + 1) * 128],
                       vv[p0:p0 + 64, tt_, hd * 256:(hd + 1) * 256])
            kv_mm(0)
            for n in range(8):
                ng = jb * 8 + n
                kvb = (ps[0], ps[1]) if n % 2 == 0 else (ps[4], ps[5])
                if n + 1 < 8:
                    kv_mm(n + 1)
                for hd in range(4):
                    stt(Sst[:, hd, :], Sst[:, hd, :], gam[:, hd, ng:ng + 1],
                        kvb[hd // 2][:, (hd % 2) * 256:(hd % 2 + 1) * 256], ALU.mult, ALU.add)
                    act(Sb[:, hd, :], Sst[:, hd, :], AF.Copy)
                    for dvc in range(2):
                        ch = hd * 2 + dvc
                        mm(ps[2 + n % 2][:, ch * 64:(ch + 1) * 64], Sb[:, hd, dvc * 128:(dvc + 1) * 128],
                           qT[:, hd, n * 64:(n + 1) * 64])
                if n >= 1:
                    cp(oT[:, :, (n - 1) * 64:n * 64], ps[2 + (n - 1) % 2][:].rearrange("p (a b) -> p a b", a=8),
                       eng="dve")
            cp(oT[:, :, 7 * 64:8 * 64], ps[2 + 7 % 2][:].rearrange("p (a b) -> p a b", a=8), eng="dve")
            for hd in range(4):
                act(sq2[:, 2 * hd:2 * hd + 2, :], oT[:, 2 * hd:2 * hd + 2, :], AF.Square)
                pn = ps[6 + hd % 2]
                for dvc in range(2):
                    mm(pn[:], ones_v[:], sq2[:, hd * 2 + dvc, :], start=(dvc == 0), stop=(dvc == 1))
                act(rstd4[:, hd, :], pn[:], AF.Ln, bias=EPS)
                act(rstd4[:, hd, :], rstd4[:, hd, :], AF.Exp, scale=-0.5)
            for hd in range(4):
                for dvc in range(2):
                    ch = hd * 2 + dvc
                    stt(tn[dvc], oT[:, ch, :], vcol(V_GNG + dvc), rstd4[:, hd, :], ALU.mult, ALU.mult)
                    tt(yT[:, ch, :], tn[dvc], sgT[:, ch, :], ALU.mult)
            if jb + 1 < NB:
                norm_a0(jb + 1)
            for m in range(8):
                if m == 4 and jb + 1 < NB:
                    norm_a(jb + 1, b, l, 0, do_sq=False)
                pb = ps[m % 2]
                for kc in range(8):
                    mm(pb[:], wout[:, kc, m * 128:(m + 1) * 128], yT[:, kc, :], start=(kc == 0), stop=(kc == 7))
                resid_update(pb[:], m, jb, ada_col(l, 2, m, b))

    def lru_views():
        return dict(
            wout=b16v(W, 0, [128, 8, 1024]),
            wgt=b16v(W, 24576, [128, 8, 1024]),
            wx=b16v(W, 40960, [128, 8, 1024]),
            wr=b16v(W, 57344, [128, 8, 256]),
            wi=b16v(W, 61440, [128, 8, 256]),
        )

    def lru_prefetch():
        v = lru_views()
        src = lru_w_in_d.rearrange("(c p) n -> p c n", p=128)
        dma("pool", v["wgt"], src[:, :, 0:1024])
        dma("pool", v["wx"], src[:, :, 1024:2048])
        dma("pool", v["wr"], lru_wr_d.rearrange("p (a b) -> p a b", a=8))
        dma("pool", v["wi"], lru_wi_d.rearrange("p (a b) -> p a b", a=8))

    hbr = smallw[:, 32:40]
    hbi = smallw[:, 40:48]
    cnh = smallw[:, 48:56]
    ts(hbr, vecs[:, V_BR:V_BR + 8], 0.5, None, ALU.mult)
    ts(hbi, vecs[:, V_BI:V_BI + 8], 0.5, None, ALU.mult)
    ts(cnh, cneg, 0.5, None, ALU.mult)

    def lru_layer(b, l=1, after_gate=None):
        V = lru_views()
        wout, wgt, wx, wr, wi = (V[k] for k in ("wout", "wgt", "wx", "wr", "wi"))
        dma("pool", wout, lru_w_out_d.rearrange("(c p) n -> p c n", p=128))
        hT = b16v(hbuf, 0, [128, 8, 512])
        yT = b16v(hbuf, 8192, [128, 8, 512])
        xb2 = [f32v(hbuf, 16384, [128, 2, 516]), f32v(hbuf, 27136, [128, 2, 516])]
        xc2 = [f32v(hbuf, 20992, [128, 2, 512]), cTr[:, 0:1024].rearrange("p (a b) -> p a b", a=2)]
        xcb = [b16v(hbuf, 25088, [128, 2, 512]),
               cTr[:, 1024:1536].bitcast(BF16).rearrange("p (a b) -> p a b", a=2)]
        t_r = f32v(scr, 0, [128, 2, 512])
        t_i = f32v(scr, 4096, [128, 2, 512])
        a_ = f32v(scr, 8192, [128, 2, 512])
        m_ = f32v(scr, 12288, [128, 2, 512])
        hs = t_r
        memset(halo[:], 0.0)
        memset(carry[:], 0.0)
        norm_a(0, b, l, 0)
        for jb in range(NB):
            t0 = jb * TB
            norm_b(jb, b, l, 0, 0, hT)
            def Gmm(hb):
                for cc in range(2):
                    c = hb * 2 + cc
                    for kc in range(8):
                        mm(ps[cc][:], wgt[:, kc, c * 128:(c + 1) * 128], hT[:, kc, :],
                           start=(kc == 0), stop=(kc == 7))

            def A1(hb):
                X, XC, XB = xb2[hb % 2], xc2[hb % 2], xcb[hb % 2]
                for cc in range(2):
                    c = hb * 2 + cc
                    cp(X[:, cc, 0:3], halo[:, c, 0:3], eng="dve")
                for cc in range(2):
                    c = hb * 2 + cc
                    pb = ps[2 + cc]
                    for kc in range(8):
                        mm(pb[:], wx[:, kc, c * 128:(c + 1) * 128], hT[:, kc, :], start=(kc == 0), stop=(kc == 7))
                    act(X[:, cc, 3:515], pb[:], AF.Copy)
                for cc in range(2):
                    c = hb * 2 + cc
                    cp(halo[:, c, 0:3], X[:, cc, 512:515], eng="dve")
                    ts(XC[:, cc, :], X[:, cc, 0:512], vcol(V_CW + 0 * 8 + c), vcol(V_CB + c), ALU.mult, ALU.add)
                    for j in range(1, 4):
                        stt(XC[:, cc, :], X[:, cc, j:j + 512], vcol(V_CW + j * 8 + c), XC[:, cc, :],
                            ALU.mult, ALU.add)

            def A2(hb):
                X, XC, XB = xb2[hb % 2], xc2[hb % 2], xcb[hb % 2]
                for cc in range(2):
                    act(XB[:, cc, :], XC[:, cc, :], AF.Copy)
                for oc in range(2):
                    for kk in range(2):
                        mm(ps[4 + 2 * oc][:], wr[:, hb * 2 + kk, oc * 128:(oc + 1) * 128], XB[:, kk, :],
                           start=(kk == 0), stop=(kk == 1))
                    for kk in range(2):
                        mm(ps[5 + 2 * oc][:], wi[:, hb * 2 + kk, oc * 128:(oc + 1) * 128], XB[:, kk, :],
                           start=(kk == 0), stop=(kk == 1))

            def B1(hb):
                for cc in range(2):
                    act(yT[:, hb * 2 + cc, :], ps[cc][:], AF.Gelu_apprx_tanh)
                for oc in range(2):
                    c = hb * 2 + oc
                    act(t_r[:, oc, :], ps[4 + 2 * oc][:], AF.Tanh, bias=hbr[:, c:c + 1], scale=0.5)
                    act(t_i[:, oc, :], ps[5 + 2 * oc][:], AF.Tanh, bias=hbi[:, c:c + 1], scale=0.5)
                for oc in range(2):
                    c = hb * 2 + oc
                    act(a_[:, oc, :], t_r[:, oc, :], AF.Exp, bias=cnh[:, c:c + 1], scale=cnh[:, c:c + 1])
                    act(m_[:, oc, :], t_r[:, oc, :], AF.Exp, bias=cneg[:, c:c + 1], scale=cneg[:, c:c + 1])
                for oc in range(2):
                    act(m_[:, oc, :], m_[:, oc, :], AF.Sqrt, bias=0.25, scale=-0.25)

            def B2(hb):
                XC = xc2[hb % 2]
                for oc in range(2):
                    c = hb * 2 + oc
                    stt(t_i[:, oc, :], t_i[:, oc, :], 1.0, XC[:, oc, :], ALU.add, ALU.mult)
                    tt(m_[:, oc, :], m_[:, oc, :], t_i[:, oc, :], ALU.mult)
                    scan(hs[:, oc, :], a_[:, oc, :], m_[:, oc, :], carry[:, c:c + 1], ALU.mult, ALU.add)
                    cp(carry[:, c:c + 1], hs[:, oc, 511:512], eng="dve")
                    tt(yT[:, c, :], hs[:, oc, :], yT[:, c, :], ALU.mult)

            Gmm(0); A1(0); A2(0)
            for hb in range(4):
                if hb + 1 < 4:
                    A1(hb + 1)
                B1(hb)
                if hb + 1 < 4:
                    Gmm(hb + 1)
                    A2(hb + 1)
                elif jb == NB - 1 and after_gate is not None:
                    after_gate()
                B2(hb)
            if jb + 1 < NB:
                norm_a0(jb + 1)
            for m in range(8):
                if m == 4 and jb + 1 < NB:
                    norm_a(jb + 1, b, l, 0, do_sq=False)
                pb = ps[m % 2]
                for kc in range(8):
                    mm(pb[:], wout[:, kc, m * 128:(m + 1) * 128], yT[:, kc, :], start=(kc == 0), stop=(kc == 7))
                resid_update(pb[:], m, jb, ada_col(l, 2, m, b))

    def moe_wviews(slot):
        base = slot * 24576
        return (b16v(W, base, [128, 8, 512]), b16v(W, base + 8192, [128, 8, 512]),
                b16v(W, base + 16384, [128, 4, 1024]))

    moe_state = {"slot": 0}

    def moe_load(l, e, slot, parts=("g", "u", "d")):
        wg, wu, wd = moe_wviews(slot)
        if "g" in parts:
            dma("pool", wg, wg_d[l, e].rearrange("(c p) n -> p c n", p=128))
        if "u" in parts:
            dma("pool", wu, wu_d[l, e].rearrange("(c p) n -> p c n", p=128))
        if "d" in parts:
            dma("pool", wd, wd_d[l, e].rearrange("(c p) n -> p c n", p=128))

    SLOTS = [1, 2, 0, 1, 2, 0, 1, 2, 0, 1, 2, 0, 1, 2, 1, 0]

    def moe_layer(b, l, pre_last=None):
        hT = b16v(hbuf, 0, [128, 8, S])
        for jb in range(NB):
            norm_block(jb, b, l, 1, 3, hT[:, :, jb * TB:(jb + 1) * TB])
        for t in range(16):
            for kc in range(8):
                mm(ps[6][:, t * 16:(t + 1) * 16], hT[:, kc, t * 128:(t + 1) * 128], rw[:, kc, :],
                   start=(kc == 0), stop=(kc == 7))
        R = lambda i: rt[:, i * 256:(i + 1) * 256]
        R3 = lambda i: rt[:, i * 256:(i + 1) * 256].rearrange("p (t e) -> p t e", e=16)
        R4 = lambda i: rt[:, i * 256:(i + 1) * 256].rearrange("p (t g j) -> p t g j", g=4, j=4)
        G = lambda i: rt[:, 2304 + i * 64:2304 + (i + 1) * 64]
        G3 = lambda i: rt[:, 2304 + i * 64:2304 + (i + 1) * 64].rearrange("p (t g) -> p t g", g=4)
        sc_, sel_, msk, tmp_ = 0, 1, 2, 3
        act(R(sc_), ps[6][:, 0:256], AF.Sigmoid)
        tt(R3(sel_), R3(sc_), rb[:, :].unsqueeze(1).to_broadcast([128, 16, 16]), ALU.add)
        red(G(0), R4(sel_), ALU.max)
        tt(R4(msk), R4(sel_), G3(0).unsqueeze(3).to_broadcast([128, 16, 4, 4]), ALU.is_ge)
        stt(R(tmp_), R(msk), -1e30, R(sel_), ALU.mult, ALU.add)
        red(G(1), R4(tmp_), ALU.max)
        tt(G(2), G(0), G(1), ALU.add)
        red(rt[:, 1664:1680], G3(2), ALU.max)
        tt(G3(3), G3(2), rt[:, 1664:1680].unsqueeze(2).to_broadcast([128, 16, 4]), ALU.is_ge)
        tt(R4(msk), R4(sel_), G3(1).unsqueeze(3).to_broadcast([128, 16, 4, 4]), ALU.is_ge)
        tt(R4(msk), R4(msk), G3(3).unsqueeze(3).to_broadcast([128, 16, 4, 4]), ALU.mult)
        tt(R(tmp_), R(msk), R(sc_), ALU.mult)
        red(rt[:, 1680:1696], R3(tmp_), ALU.add)
        recip(rt[:, 1680:1696], rt[:, 1680:1696])
        tt(R3(tmp_), R3(tmp_), rt[:, 1680:1696].unsqueeze(2).to_broadcast([128, 16, 16]), ALU.mult)
        pk = rt[:, 1024:1536].rearrange("p (t e) -> p t e", e=32)
        hib = rt[:, 1536:1664].bitcast(BF16).rearrange("p (t e) -> p t e", e=16)
        cp(hib, R3(tmp_), eng="dve")
        cp(pk[:, :, 0:16], hib, eng="dve")
        tt(pk[:, :, 16:32], R3(tmp_), pk[:, :, 0:16], ALU.subtract)
        combT = cTr[0:32, 0:1024].bitcast(BF16)
        for t4 in range(4):
            for q in range(4):
                t = t4 * 4 + q
                tr(ps[7][0:32, q * 128:(q + 1) * 128], pk[:, t, :], ident[:])
            act(combT[:, t4 * 512:(t4 + 1) * 512], ps[7][0:32, :], AF.Copy)
        if "comb" in dbg_d and b == dbg.get("_b", 0) and l == dbg.get("_l", 0):
            dma("sp", dbg_d["comb"], combT)
        he = [b16v(scr, 0, [128, 4, 512]), b16v(scr, 4096, [128, 4, 512])]
        sg = [f32v(scr, 8192, [128, 512]), f32v(scr, 10240, [128, 512])]
        tb = [f32v(scr, 12288, [128, 512]), f32v(scr, 14336, [128, 512])]
        cbs = [f32v(scr, 16384, [128, 512]), f32v(scr, 18432, [128, 512])]
        it = 0
        for e in range(NE):
            slot = SLOTS[e]
            if e + 1 < NE:
                moe_load(l, e + 1, SLOTS[e + 1])
            elif pre_last is not None:
                pre_last()
            wg, wu, wd = moe_wviews(slot)
            for jb in range(NB):
                t0 = jb * TB
                k2 = it % 2
                it += 1
                mm(ps[6][:], i32b[:, e:e + 1].to_broadcast([32, 128]), combT[:, t0:t0 + TB])
                act(cbs[k2], ps[6][:], AF.Copy)
                for c in range(4):
                    pg = ps[0 + c % 2]
                    pu = ps[2 + c % 2]
                    for kc in range(8):
                        mm(pg[:], wg[:, kc, c * 128:(c + 1) * 128], hT[:, kc, t0:t0 + TB],
                           start=(kc == 0), stop=(kc == 7))
                    for kc in range(8):
                        mm(pu[:], wu[:, kc, c * 128:(c + 1) * 128], hT[:, kc, t0:t0 + TB],
                           start=(kc == 0), stop=(kc == 7))
                    act(sg[c % 2], pg[:], AF.Silu)
                    tt(tb[c % 2], pu[:], sg[c % 2], ALU.mult)
                    tt(he[k2][:, c, :], tb[c % 2], cbs[k2], ALU.mult)
                for m in range(8):
                    pd = ps[4 + m % 2]
                    for kc in range(4):
                        mm(pd[:], wd[:, kc, m * 128:(m + 1) * 128], he[k2][:, kc, :],
                           start=(kc == 0), stop=(kc == 3))
                    resid_update(pd[:], m, jb, ada_col(l, 5, m, b))

    def final_store(b, si):
        sq = b16v(scr, 0, [128, 8, 512])
        rstd = f32v(scr, 8192, [128, 512])
        ob = [f32v(hbuf, 0, [128, 8, 512]), f32v(hbuf, 16384, [128, 8, 512])]
        outs = []
        for jb in range(NB):
            t0 = jb * TB
            act(sq[:, :, :], xT[:, :, t0:t0 + TB], AF.Square)
            for c in range(8):
                mm(ps[6][:], ones_d[:], sq[:, c, :], start=(c == 0), stop=(c == 7))
            act(rstd, ps[6][:], AF.Ln, bias=EPS)
            act(rstd, rstd, AF.Exp, scale=-0.5)
            o = ob[jb % 2]
            for c in range(8):
                stt(o[:, c, :], xT[:, c, t0:t0 + TB], vcol(V_FNG + c), rstd, ALU.mult, ALU.mult)
            outs.append(dma("sp", outT_d[si, :, t0:t0 + TB].rearrange("(c p) t -> p c t", p=128), o))
        return outs

    out_ops = []
    phases = dbg.get("_phases", ("gla", "moe0", "lru", "moe1", "final"))
    for si in range(n_seq):
        b = si
        dma("sp", xT[:, :, :], xT_d[si].rearrange("(c p) t -> p c t", p=128))
        if si == 0 and "gla" in phases:
            gla_prefetch()
        if "gla" in phases:
            gla_layer(b, after_proj=(lambda: moe_load(0, 0, SLOTS[0])) if "moe0" in phases else None)
        if "xmid0" in dbg_d and si == dbg.get("_b", 0):
            out_ops.append(dma("sp", dbg_d["xmid0"].rearrange("(c p) t -> p c t", p=128), xT[:, :, :]))
        if "moe0" in phases:
            if "gla" not in phases:
                moe_load(0, 0, SLOTS[0])
            moe_layer(b, 0, pre_last=lru_prefetch if "lru" in phases else None)
        if "xmid1" in dbg_d and si == dbg.get("_b", 0):
            out_ops.append(dma("sp", dbg_d["xmid1"].rearrange("(c p) t -> p c t", p=128), xT[:, :, :]))
        if "lru" in phases:
            if "moe0" not in phases:
                lru_prefetch()
            lru_layer(b, after_gate=(lambda: moe_load(1, 0, SLOTS[0], ("g", "u"))) if "moe1" in phases else None)
            if "moe1" in phases:
                moe_load(1, 0, SLOTS[0], ("d",))
        if "xmid2" in dbg_d and si == dbg.get("_b", 0):
            out_ops.append(dma("sp", dbg_d["xmid2"].rearrange("(c p) t -> p c t", p=128), xT[:, :, :]))
        if "moe1" in phases:
            if "lru" not in phases:
                moe_load(1, 0, SLOTS[0])
            moe_layer(b, 1, pre_last=gla_prefetch if (si + 1 < n_seq and "gla" in phases) else None)
        if "final" in phases:
            out_ops += final_store(b, si)

    Sd.add("sp", lambda e: e.nop(), reads=[], writes=[]).deps.extend(
        [o for o in Sd.dma_ops["sp"]])

    Sd.finalize()

    sem_stack = contextlib.ExitStack()
    csem = {e: sem_stack.enter_context(nc.semaphore("c_" + e)) for e in Sched.ENGS}
    dsems = {e: [sem_stack.enter_context(nc.semaphore("d_%s_%d" % (e, i))) for i in range(Sched.NDS)]
             for e in ("sp", "pool")}
    with nc.Block() as block:
        @block.tensor
        def _(e):
            Sd.emit("pe", e, csem, dsems)

        @block.scalar
        def _(e):
            Sd.emit("act", e, csem, dsems)

        @block.vector
        def _(e):
            Sd.emit("dve", e, csem, dsems)

        @block.gpsimd
        def _(e):
            Sd.emit("pool", e, csem, dsems)

        @block.sync
        def _(e):
            Sd.emit("sp", e, csem, dsems)
    sem_stack.close()
    stack.close()
    return nc, Sd


V_NMG = 0
V_NFG = 16
V_FNG = 32
V_GNG = 40
V_CW = 42
V_CB = 74
V_BR = 82
V_BI = 90
V_LAM = 98
NV = 106


def _fm(v):
    v = np.asarray(v, np.float32).reshape(-1, 128)
    return np.ascontiguousarray(v.T)


def prep_inputs(inp):
    f = lambda a: np.ascontiguousarray(np.asarray(a, dtype=np.float32))
    x = f(inp["x"])
    c = f(inp["c"])
    vec = np.zeros((128, NV), np.float32)
    for l in range(2):
        vec[:, V_NMG + l * 8:V_NMG + (l + 1) * 8] = _fm(inp["norm_mix_g"][l])
        vec[:, V_NFG + l * 8:V_NFG + (l + 1) * 8] = _fm(inp["norm_ffn_g"][l])
    vec[:, V_FNG:V_FNG + 8] = _fm(inp["final_norm_g"])
    vec[:, V_GNG:V_GNG + 2] = _fm(inp["gla_norm_g"][0])
    for j in range(4):
        vec[:, V_CW + j * 8:V_CW + (j + 1) * 8] = _fm(inp["lru_conv_w"][0, j])
    vec[:, V_CB:V_CB + 8] = _fm(inp["lru_conv_b"][0])
    vec[:, V_BR:V_BR + 8] = _fm(np.asarray(inp["lru_b_r"][0]).reshape(-1))
    vec[:, V_BI:V_BI + 8] = _fm(np.asarray(inp["lru_b_i"][0]).reshape(-1))
    vec[:, V_LAM:V_LAM + 8] = _fm(inp["lru_lambda"][0])
    ada_b = np.concatenate([_fm(inp["ada_b"][0]), _fm(inp["ada_b"][1])], axis=1)
    wgu = np.concatenate([f(inp["gla_w_gate_up"][0]), f(inp["gla_b_gate"][0])[None, :]], axis=0)

    def blk(w):
        w = f(w).reshape(4, 2, 128, 256).transpose(2, 0, 1, 3)
        return np.ascontiguousarray(w).reshape(128, 8 * 256)

    rwl = f(inp["router_w"]).reshape(8, 128, 16).transpose(1, 0, 2).reshape(128, 128)
    shared = {
        "ada_w": f(inp["ada_w"]), "ada_b": ada_b, "vecs": vec,
        "gla_w_in": f(inp["gla_w_in"][0]), "gla_w_out": f(inp["gla_w_out"][0]), "wgu": wgu,
        "lru_w_in": f(inp["lru_w_in"][0]), "lru_w_out": f(inp["lru_w_out"][0]),
        "lru_wr": blk(inp["lru_w_r"][0]), "lru_wi": blk(inp["lru_w_i"][0]),
        "rw": np.ascontiguousarray(rwl), "rb": np.ascontiguousarray(np.tile(f(inp["router_bias"])[None, :], (128, 1))),
        "moe_wg": f(inp["moe_w_gate"]), "moe_wu": f(inp["moe_w_up"]), "moe_wd": f(inp["moe_w_down"]),
        "ident": np.eye(128, dtype=np.float32),
    }
    s_i = np.arange(128)[:, None]
    c_i = np.arange(128)[None, :]
    shared["mtri"] = (((s_i > c_i) & (s_i // 64 == c_i // 64)).astype(np.float32) / 16.0)
    shared["ind"] = ((s_i // 64) == np.arange(2)[None, :]).astype(np.float32) / 16.0
    shared["i16"] = np.eye(16, dtype=np.float32)
    in_maps = []
    for i in range(N_CORES):
        m = dict(shared)
        m["xT"] = np.ascontiguousarray(x[2 * i:2 * i + 2].transpose(0, 2, 1))
        cc = c[2 * i:2 * i + 2]
        m["cT"] = np.ascontiguousarray(cc.reshape(2, 8, 128).transpose(2, 1, 0).reshape(128, 16))
        in_maps.append(m)
    return in_maps


_CACHE = {}


def kernel(**inputs):
    in_maps = prep_inputs(inputs)
    if "nc" not in _CACHE:
        _CACHE["nc"] = build_program()[0]
    nc = _CACHE["nc"]
    res = run_bass_kernel_spmd(nc, in_maps, core_ids=list(range(N_CORES)))
    out = np.empty((16, S, D), np.float32)
    for i in range(N_CORES):
        o = res.results[i]["outT"]
        out[2 * i:2 * i + 2] = o.transpose(0, 2, 1)
    return out
```

```python
import numpy as np
import concourse.bass as bass
import concourse.mybir as mybir
from concourse.bass_utils import run_bass_kernel_spmd

F32 = mybir.dt.float32
BF16 = mybir.dt.bfloat16
AF = mybir.ActivationFunctionType
ALU = mybir.AluOpType
ESZ = {F32: 4, BF16: 2}

D = 1024
S = 2048
NB = 4
TB = 512
NE = 16
DE = 512
EPS = 1e-6
GRAN = 512
N_CORES = 8


class Op:
    __slots__ = ("eng", "idx", "fn", "deps", "dma", "dsem", "dval", "sig", "sigval", "waits", "gidx")


class Sched:
    ENGS = ("pe", "act", "dve", "pool", "sp")
    NDS = 12

    def __init__(self, nc):
        self.nc = nc
        self.ops = {e: [] for e in self.ENGS}
        self.last_w = {}
        self.readers = {}
        self.ndma = {e: 0 for e in self.ENGS}
        self.dma_ops = {e: [] for e in self.ENGS}
        self.tok_cache = {}
        self.gcount = 0

    def tokens(self, ap):
        sp = str(ap.space)
        if "SB" not in sp and "PSUM" not in sp:
            return ()
        if "PSUM" in sp:
            return ((ap.tensor.name, 0),)
        key = (ap.tensor.name, ap.offset, ap.ap, ap.dtype)
        t = self.tok_cache.get(key)
        if t is None:
            es = ESZ[ap.dtype]
            pstride = ap.ap[0][0]
            off = ap.offset % pstride if pstride > 0 else ap.offset
            name = ap.tensor.name
            dims = [(abs(st), cnt) for st, cnt in ap.ap[1:] if cnt > 1 and st != 0]
            dims.sort(reverse=True)
            outer = dims[:-1] if dims else []
            n_outer = 1
            for _, cnt in outer:
                n_outer *= cnt
            gs = set()
            if dims and n_outer <= 512:
                lst, lcnt = dims[-1]
                bases = [off]
                for st, cnt in outer:
                    bases = [b0 + i * st for b0 in bases for i in range(cnt)]
                for b0 in bases:
                    lo = b0 * es
                    hi = (b0 + (lcnt - 1) * lst + 1) * es
                    gs.update(range(lo // GRAN, (hi - 1) // GRAN + 1))
            else:
                span = 0
                for st, cnt in dims:
                    span += (cnt - 1) * st
                lo = off * es
                hi = (off + span + 1) * es
                gs.update(range(lo // GRAN, (hi - 1) // GRAN + 1))
            t = tuple((name, g) for g in sorted(gs))
            self.tok_cache[key] = t
        return t

    def add(self, eng, fn, reads=(), writes=(), dma=False):
        op = Op()
        op.eng = eng
        op.idx = len(self.ops[eng])
        op.fn = fn
        op.dma = dma
        op.sig = False
        op.sigval = 0
        op.gidx = self.gcount
        self.gcount += 1
        deps = {}
        rt = []
        for ap in reads:
            rt.extend(self.tokens(ap))
        wt = []
        for ap in writes:
            wt.extend(self.tokens(ap))
        for r in rt:
            w = self.last_w.get(r)
            if w is not None:
                deps[id(w)] = w
        for r in wt:
            w = self.last_w.get(r)
            if w is not None:
                deps[id(w)] = w
            rd = self.readers.get(r)
            if rd:
                for o in rd.values():
                    deps[id(o)] = o
        deps.pop(id(op), None)
        op.deps = list(deps.values())
        for r in rt:
            d = self.readers.get(r)
            if d is None:
                d = {}
                self.readers[r] = d
            if dma:
                d[(eng, op.idx)] = op
            else:
                d[eng] = op
        for r in wt:
            self.last_w[r] = op
            self.readers[r] = {}
        if dma:
            i = self.ndma[eng]
            self.ndma[eng] = i + 1
            op.dsem = i % self.NDS
            op.dval = 16 * (i // self.NDS + 1)
            if i >= self.NDS:
                op.deps.append(self.dma_ops[eng][i - self.NDS])
            self.dma_ops[eng].append(op)
        self.ops[eng].append(op)
        return op

    def finalize(self):
        for eng in self.ENGS:
            known = {}
            for op in self.ops[eng]:
                need = {}
                for d in op.deps:
                    if d.dma:
                        k = ("d", d.eng, d.dsem)
                        v = d.dval
                        if v > need.get(k, (0, None))[0]:
                            need[k] = (v, d)
                    else:
                        if d.eng == "pe" and eng == "pe" and not op.dma:
                            continue
                        k = ("c", d.eng)
                        v = d.idx + 1
                        if v > need.get(k, (0, None))[0]:
                            need[k] = (v, d)
                waits = []
                for k, (v, d) in need.items():
                    if known.get(k, 0) >= v:
                        continue
                    known[k] = v
                    waits.append(d)
                    if not d.dma:
                        d.sig = True
                op.waits = waits
        for eng in self.ENGS:
            c = 0
            for op in self.ops[eng]:
                if op.sig and not op.dma:
                    c += 1
                    op.sigval = c

    def emit(self, eng, e, csem, dsems):
        for op in self.ops[eng]:
            for d in op.waits:
                if d.dma:
                    e.wait_ge(dsems[d.eng][d.dsem], d.dval)
                else:
                    e.wait_ge(csem[d.eng], d.sigval)
            ins = op.fn(e)
            if op.dma:
                ins.then_inc(dsems[eng][op.dsem], 16)
            elif op.sig:
                ins.then_inc(csem[eng], 1)


def build_program(n_seq=2, dbg=None):
    dbg = dbg or {}
    nc = bass.Bass("TRN2", target_bir_lowering=False)
    Sd = Sched(nc)

    def din(name, shape):
        return nc.dram_tensor(name, list(shape), F32, kind="ExternalInput").ap()

    xT_d = din("xT", [2, D, S])
    cT_d = din("cT", [128, 16])
    ada_w_d = din("ada_w", [2, D, 6 * D])
    ada_b_d = din("ada_b", [128, 96])
    vecs_d = din("vecs", [128, NV])
    gla_w_in_d = din("gla_w_in", [D, 3088])
    gla_w_out_d = din("gla_w_out", [D, D])
    wgu_d = din("wgu", [17, 512])
    lru_w_in_d = din("lru_w_in", [D, 2 * D])
    lru_w_out_d = din("lru_w_out", [D, D])
    lru_wr_d = din("lru_wr", [128, 8 * 256])
    lru_wi_d = din("lru_wi", [128, 8 * 256])
    rw_d = din("rw", [128, 8 * 16])
    rb_d = din("rb", [128, 16])
    wg_d = din("moe_wg", [2, NE, D, DE])
    wu_d = din("moe_wu", [2, NE, D, DE])
    wd_d = din("moe_wd", [2, NE, DE, D])
    ident_d = din("ident", [128, 128])
    mtri_d = din("mtri", [128, 128])
    ind_d = din("ind", [128, 2])
    i16_d = din("i16", [16, 16])
    outT_d = nc.dram_tensor("outT", [2, D, S], F32, kind="ExternalOutput").ap()
    dbg_d = {}
    for k, shp in dbg.items():
        if k.startswith("_"):
            continue
        dbg_d[k] = nc.dram_tensor("dbg_" + k, list(shp), F32, kind="ExternalOutput").ap()

    import contextlib
    stack = contextlib.ExitStack()

    def SB(name, shape, dt=F32):
        return stack.enter_context(nc.sbuf_tensor("s_" + name, list(shape), dt))

    def PS(name):
        return stack.enter_context(nc.psum_tensor(name, [128, 512], F32))

    xT = SB("xT", [128, 8, S])
    hbuf = SB("hbuf", [128, 16384], BF16)
    W = SB("W", [128, 36864], BF16)
    scr = SB("scr", [128, 10240], BF16)
    cTr = SB("cTr", [128, 2048])
    ident = SB("ident", [128, 128])
    mtri = SB("mtri", [128, 128])
    ind = SB("ind", [128, 2])
    i32f = SB("i32f", [32, 16])
    i32b = SB("i32b", [32, 16], BF16)
    ones_d = SB("ones_d", [128, 128], BF16)
    ones_v = SB("ones_v", [128, 128], BF16)
    vecs = SB("vecs", [128, NV])
    cT_s = SB("cT_s", [128, 16])
    cond = SB("cond", [128, 16], BF16)
    ada_b = SB("ada_b", [128, 96])
    ada = SB("ada", [128, 2, 48, 2])
    mod = SB("mod", [128, 2, 4, 8, 2])
    rw = SB("rw", [128, 8, 16], BF16)
    rb = SB("rb", [128, 16])
    wgu = SB("wgu", [32, 512])
    smallw = SB("smallw", [128, 256])
    halo = SB("halo", [128, 8, 4])
    carry = SB("carry", [128, 8])
    rt = scr[:, 0:5120].bitcast(F32)

    ps = [PS("ps%d" % i) for i in range(8)]

    def mm(out, lhsT, rhs, start=True, stop=True):
        return Sd.add("pe", lambda e: e.matmul(out, lhsT, rhs, start=start, stop=stop),
                      reads=[lhsT, rhs], writes=[out])

    def tr(out, in_, idn):
        return Sd.add("pe", lambda e: e.transpose(out, in_, idn), reads=[in_, idn], writes=[out])

    def act(out, in_, func, bias=None, scale=None, eng="act"):
        rd = [in_]
        kw = {}
        if bias is not None:
            kw["bias"] = bias
            if not isinstance(bias, (int, float)):
                rd.append(bias)
        if scale is not None:
            kw["scale"] = scale
            if not isinstance(scale, (int, float)):
                rd.append(scale)
        return Sd.add("act", lambda e: e.activation(out, in_, func, **kw), reads=rd, writes=[out])

    def stt(out, in0, scalar, in1, op0, op1, eng="dve"):
        rd = [in0, in1]
        if not isinstance(scalar, (int, float)):
            rd.append(scalar)
        return Sd.add(eng, lambda e: e.scalar_tensor_tensor(out, in0, scalar, in1, op0, op1),
                      reads=rd, writes=[out])

    def ts(out, in0, s1, s2, op0, op1=None, eng="dve"):
        rd = [in0]
        for s in (s1, s2):
            if s is not None and not isinstance(s, (int, float)):
                rd.append(s)
        if op1 is None:
            return Sd.add(eng, lambda e: e.tensor_scalar(out, in0, s1, None, op0), reads=rd, writes=[out])
        return Sd.add(eng, lambda e: e.tensor_scalar(out, in0, s1, s2, op0, op1), reads=rd, writes=[out])

    def tt(out, in0, in1, op, eng="dve"):
        return Sd.add(eng, lambda e: e.tensor_tensor(out, in0, in1, op), reads=[in0, in1], writes=[out])

    def cp(out, in_, eng="dve"):
        return Sd.add(eng, lambda e: e.tensor_copy(out, in_), reads=[in_], writes=[out])

    def recip(out, in_):
        return Sd.add("dve", lambda e: e.reciprocal(out, in_), reads=[in_], writes=[out])

    def red(out, in_, op, eng="dve"):
        return Sd.add(eng, lambda e: e.tensor_reduce(out, in_, mybir.AxisListType.X, op),
                      reads=[in_], writes=[out])

    def scan(out, d0, d1, init, op0, op1):
        rd = [d0, d1]
        if not isinstance(init, (int, float)):
            rd.append(init)
        return Sd.add("dve", lambda e: e.tensor_tensor_scan(out, d0, d1, init, op0, op1),
                      reads=rd, writes=[out])

    def memset(ap, val, eng="dve"):
        return Sd.add(eng, lambda e: e.memset(ap, val), writes=[ap])

    def dma(q, out, in_, **kw):
        return Sd.add(q, lambda e: e.dma_start(out=out, in_=in_, **kw), reads=[in_], writes=[out], dma=True)

    def f32v(t, off_b, shape):
        n = int(np.prod(shape[1:]))
        v = t[0:shape[0], off_b // 2: off_b // 2 + 2 * n].bitcast(F32)
        if len(shape) == 3:
            v = v.rearrange("p (a b) -> p a b", a=shape[1])
        elif len(shape) == 4:
            v = v.rearrange("p (a b c) -> p a b c", a=shape[1], b=shape[2])
        return v

    def b16v(t, off_b, shape):
        n = int(np.prod(shape[1:]))
        v = t[0:shape[0], off_b // 2: off_b // 2 + n]
        if len(shape) == 3:
            v = v.rearrange("p (a b) -> p a b", a=shape[1])
        elif len(shape) == 4:
            v = v.rearrange("p (a b c) -> p a b c", a=shape[1], b=shape[2])
        return v

    def vcol(i):
        return vecs[:, i:i + 1]

    dma("sp", ident[:], ident_d)
    dma("sp", mtri[:], mtri_d)
    dma("sp", ind[:], ind_d)
    dma("sp", i32f[0:16, :], i16_d)
    dma("sp", i32f[16:32, :], i16_d)
    cp(i32b[:], i32f[:], eng="dve")
    dma("sp", vecs[:], vecs_d)
    dma("sp", cT_s[:], cT_d)
    dma("sp", ada_b[:], ada_b_d)
    dma("sp", rb[:], rb_d)
    dma("sp", wgu[0:17, :], wgu_d)
    dma("pool", rw[:].rearrange("p a b -> p (a b)"), rw_d)
    memset(ones_d[:], 1.0 / 1024.0)
    memset(ones_v[:], 1.0 / 256.0)
    act(cond[:], cT_s[:], AF.Silu)

    NPIECE = 12
    for l in range(2):
        for pc in range(NPIECE):
            slot = (l * NPIECE + pc) % 3
            wv = b16v(hbuf, slot * 8192, [128, 8, 512])
            dma("pool", wv, ada_w_d[l, :, pc * 512:(pc + 1) * 512].rearrange("(c p) n -> p c n", p=128))
            for oc4 in range(4):
                oc = pc * 4 + oc4
                for kc in range(8):
                    mm(ps[7][:, (l * 48 + oc) * 2:(l * 48 + oc) * 2 + 2],
                       wv[:, kc, oc4 * 128:(oc4 + 1) * 128],
                       cond[:, kc * 2:kc * 2 + 2], start=(kc == 0), stop=(kc == 7))
    for l in range(2):
        tt(ada[:, l, :, :], ps[7][:, l * 96:(l + 1) * 96].rearrange("p (a b) -> p a b", b=2),
           ada_b[:, l * 48:(l + 1) * 48].unsqueeze(2).to_broadcast([128, 48, 2]), ALU.add)
    for l in range(2):
        for j, (which, gcol) in enumerate(((1, V_NMG + l * 8), (4, V_NFG + l * 8))):
            ts(mod[:, l, j, :, :], ada[:, l, which * 8:(which + 1) * 8, :], 1.0, None, ALU.add)
            tt(mod[:, l, j, :, :], mod[:, l, j, :, :],
               vecs[:, gcol:gcol + 8].unsqueeze(2).to_broadcast([128, 8, 2]), ALU.mult)

    def A_of(l, j, c, b):
        return mod[:, l, j, c, b:b + 1]

    def ada_col(l, which, c, b):
        return ada[:, l, which * 8 + c, b:b + 1]

    lam = vecs[:, V_LAM:V_LAM + 8]
    sw_a = smallw[:, 0:8]
    sw_b = smallw[:, 8:16]
    cneg = smallw[:, 16:24]
    cneg2 = smallw[:, 24:32]
    ts(sw_b, lam, 0.0, None, ALU.min)
    stt(sw_a, sw_b, 2.0, lam, ALU.mult, ALU.subtract)
    act(sw_a, sw_a, AF.Exp)
    act(sw_a, sw_a, AF.Ln, bias=1.0)
    tt(sw_a, sw_a, sw_b, ALU.subtract)
    ts(cneg, sw_a, -8.0, None, ALU.mult)
    ts(cneg2, sw_a, -16.0, None, ALU.mult)

    def norm_a0(jb):
        t0 = jb * TB
        sq = b16v(scr, 0, [128, 8, 512])
        act(sq[:, :, :], xT[:, :, t0:t0 + TB], AF.Square)

    def norm_a(jb, b, l, j, npre=2, do_sq=True):
        t0 = jb * TB
        sq = b16v(scr, 0, [128, 8, 512])
        rstd = f32v(scr, 8192, [128, 512])
        tmp = [f32v(scr, 10240, [128, 512]), f32v(scr, 12288, [128, 512])]
        if do_sq:
            norm_a0(jb)
        for c in range(8):
            mm(ps[6][:], ones_d[:], sq[:, c, :], start=(c == 0), stop=(c == 7))
        act(rstd, ps[6][:], AF.Ln, bias=EPS)
        act(rstd, rstd, AF.Exp, scale=-0.5)
        for c in range(npre):
            stt(tmp[c % 2], xT[:, c, t0:t0 + TB], A_of(l, j, c, b), rstd, ALU.mult, ALU.mult)

    def norm_b(jb, b, l, j, shift_which, dst, npre=2):
        t0 = jb * TB
        rstd = f32v(scr, 8192, [128, 512])
        tmp = [f32v(scr, 10240, [128, 512]), f32v(scr, 12288, [128, 512])]
        for c in range(8):
            tm = tmp[c % 2]
            if c >= npre:
                stt(tm, xT[:, c, t0:t0 + TB], A_of(l, j, c, b), rstd, ALU.mult, ALU.mult)
            act(dst[:, c, :], tm, AF.Identity, bias=ada_col(l, shift_which, c, b))

    def norm_block(jb, b, l, j, shift_which, dst):
        norm_a(jb, b, l, j, npre=0)
        norm_b(jb, b, l, j, shift_which, dst, npre=0)

    def resid_update(pbank, m, jb, gcol):
        t0 = jb * TB
        stt(xT[:, m, t0:t0 + TB], pbank, gcol, xT[:, m, t0:t0 + TB], ALU.mult, ALU.add)

    wa = SB("wa", [128, 8, 128], BF16)
    memset(wa[:], 0.0)

    def gla_views():
        return dict(
            wout=b16v(W, 0, [128, 8, 1024]),
            Sst=f32v(W, 16384, [128, 4, 256]),
            Sb=b16v(W, 20480, [128, 4, 256]),
            gam=f32v(W, 22528, [128, 4, 32]),
            wq=b16v(W, 24576, [128, 8, 512]),
            wk=b16v(W, 32768, [128, 8, 512]),
            wv=b16v(W, 40960, [128, 8, 1024]),
            wg=b16v(W, 57344, [128, 8, 1024]),
        )

    def gla_prefetch():
        v = gla_views()
        src = gla_w_in_d.rearrange("(c p) n -> p c n", p=128)
        dma("pool", wa[:, :, 0:16], src[:, :, 3072:3088])
        dma("pool", v["wq"], src[:, :, 0:512])
        dma("pool", v["wg"], src[:, :, 2048:3072])
        dma("pool", v["wk"], src[:, :, 512:1024])
        dma("pool", v["wv"], src[:, :, 1024:2048])

    def gla_layer(b, l=0, after_proj=None):
        V = gla_views()
        wout, Sst, Sb, gam, wq, wk, wv, wg = (V[k] for k in ("wout", "Sst", "Sb", "gam", "wq", "wk", "wv", "wg"))
        dma("pool", wout, gla_w_out_d.rearrange("(c p) n -> p c n", p=128))
        memset(Sst, 0.0)
        hT = b16v(hbuf, 0, [128, 8, 512])
        qT = b16v(hbuf, 8192, [128, 4, 512])
        kdec = b16v(hbuf, 12288, [128, 4, 512])
        vv = b16v(hbuf, 16384, [128, 4, 1024])
        sgT = b16v(hbuf, 24576, [128, 8, 512])
        oT = f32v(scr, 0, [128, 8, 512])
        m4 = f32v(scr, 0, [128, 4, 512])
        u4 = f32v(scr, 8192, [128, 4, 512])
        alrT = f32v(scr, 16384, [32, 512])
        expD = f32v(scr, 18432, [128, 512])
        la = cTr[:, :].rearrange("p (a b) -> p a b", a=4)
        yT = hT
        rstd4 = f32v(hbuf, 8192, [128, 4, 512])
        sq2 = b16v(hbuf, 16384, [128, 8, 512])
        tn = [f32v(scr, 16384, [128, 512]), f32v(scr, 18432, [128, 512])]
        QS = 128.0 ** -0.5
        norm_a(0, b, l, 0)
        for jb in range(NB):
            t0 = jb * TB
            norm_b(jb, b, l, 0, 0, hT)
            for kc in range(8):
                mm(ps[2][:], wa[:, kc, :], hT[:, kc, :], start=(kc == 0), stop=(kc == 7))
            memset(alrT, 1.0)
            cp(alrT[0:16, :], ps[2][0:16, :], eng="dve")
            for tt_ in range(4):
                pz = ps[3 + tt_ % 2]
                mm(pz[:], alrT[0:17, tt_ * 128:(tt_ + 1) * 128], wgu[0:17, :])
                ts(m4[:, tt_, :], pz[:], 0.0, None, ALU.min)
                stt(u4[:, tt_, :], m4[:, tt_, :], 2.0, pz[:], ALU.mult, ALU.subtract)
            for hd in range(4):
                pb = ps[hd % 2]
                for kc in range(8):
                    mm(pb[:], wq[:, kc, hd * 128:(hd + 1) * 128], hT[:, kc, :], start=(kc == 0), stop=(kc == 7))
                act(qT[:, hd, :], pb[:], AF.Copy, scale=QS)
            for gc in range(8):
                pb = ps[gc % 2]
                for kc in range(8):
                    mm(pb[:], wg[:, kc, gc * 128:(gc + 1) * 128], hT[:, kc, :], start=(kc == 0), stop=(kc == 7))
                act(sgT[:, gc, :], pb[:], AF.Silu)
            act(u4[:, :, :], u4[:, :, :], AF.Exp)
            act(u4[:, :, :], u4[:, :, :], AF.Ln, bias=1.0)
            tt(la[:, :, :], m4[:, :, :], u4[:, :, :], ALU.subtract)
            for tt_ in range(4):
                mm(ps[3][:], mtri[:], la[:, tt_, :])
                for hd in range(4):
                    mm(ps[7][:, hd * 2:hd * 2 + 2], la[:, tt_, hd * 128:(hd + 1) * 128], ind[:])
                for kc in range(8):
                    mm(ps[4 + tt_ % 2][:], hT[:, kc, tt_ * 128:(tt_ + 1) * 128], wk[:, kc, :],
                       start=(kc == 0), stop=(kc == 7))
                act(expD, ps[3][:], AF.Exp)
                n0 = jb * 8 + tt_ * 2
                act(gam[:, :, n0:n0 + 2], ps[7][:, 0:8].rearrange("p (a b) -> p a b", b=2), AF.Exp)
                tt(kdec[:, tt_, :], ps[4 + tt_ % 2][:], expD, ALU.mult)
                for vh in range(2):
                    pb = ps[vh]
                    for kc in range(8):
                        mm(pb[:], hT[:, kc, tt_ * 128:(tt_ + 1) * 128], wv[:, kc, vh * 512:(vh + 1) * 512],
                           start=(kc == 0), stop=(kc == 7))
                    act(vv[:, tt_, vh * 512:(vh + 1) * 512], pb[:], AF.Copy)
            if jb == NB - 1 and after_proj is not None:
                after_proj()
            def kv_mm(n):
                tt_ = n // 2
                p0 = (n % 2) * 64
                kvb = (ps[0], ps[1]) if n % 2 == 0 else (ps[4], ps[5])
                for hd in range(4):
                    mm(kvb[hd // 2][:, (hd % 2) * 256:(hd % 2 + 1) * 256],
                       kdec[p0:p0 + 64, tt_, hd * 128:(hd + 1) * 128],
                       vv[p0:p0 + 64, tt_, hd * 256:(hd + 1) * 256])
            kv_mm(0)
            for n in range(8):
                ng = jb * 8 + n
                kvb = (ps[0], ps[1]) if n % 2 == 0 else (ps[4], ps[5])
                if n + 1 < 8:
                    kv_mm(n + 1)
                for hd in range(4):
                    stt(Sst[:, hd, :], Sst[:, hd, :], gam[:, hd, ng:ng + 1],
                        kvb[hd // 2][:, (hd % 2) * 256:(hd % 2 + 1) * 256], ALU.mult, ALU.add)
                    act(Sb[:, hd, :], Sst[:, hd, :], AF.Copy)
                    for dvc in range(2):
                        ch = hd * 2 + dvc
                        mm(ps[2 + n % 2][:, ch * 64:(ch + 1) * 64], Sb[:, hd, dvc * 128:(dvc + 1) * 128],
                           qT[:, hd, n * 64:(n + 1) * 64])
                if n >= 1:
                    cp(oT[:, :, (n - 1) * 64:n * 64], ps[2 + (n - 1) % 2][:].rearrange("p (a b) -> p a b", a=8),
                       eng="dve")
            cp(oT[:, :, 7 * 64:8 * 64], ps[2 + 7 % 2][:].rearrange("p (a b) -> p a b", a=8), eng="dve")
            for hd in range(4):
                act(sq2[:, 2 * hd:2 * hd + 2, :], oT[:, 2 * hd:2 * hd + 2, :], AF.Square)
                pn = ps[6 + hd % 2]
                for dvc in range(2):
                    mm(pn[:], ones_v[:], sq2[:, hd * 2 + dvc, :], start=(dvc == 0), stop=(dvc == 1))
                act(rstd4[:, hd, :], pn[:], AF.Ln, bias=EPS)
                act(rstd4[:, hd, :], rstd4[:, hd, :], AF.Exp, scale=-0.5)
            for hd in range(4):
                for dvc in range(2):
                    ch = hd * 2 + dvc
                    stt(tn[dvc], oT[:, ch, :], vcol(V_GNG + dvc), rstd4[:, hd, :], ALU.mult, ALU.mult)
                    tt(yT[:, ch, :], tn[dvc], sgT[:, ch, :], ALU.mult)
            if jb + 1 < NB:
                norm_a0(jb + 1)
            for m in range(8):
                if m == 4 and jb + 1 < NB:
                    norm_a(jb + 1, b, l, 0, do_sq=False)
                pb = ps[m % 2]
                for kc in range(8):
                    mm(pb[:], wout[:, kc, m * 128:(m + 1) * 128], yT[:, kc, :], start=(kc == 0), stop=(kc == 7))
                resid_update(pb[:], m, jb, ada_col(l, 2, m, b))

    def lru_views():
        return dict(
            wout=b16v(W, 0, [128, 8, 1024]),
            wgt=b16v(W, 24576, [128, 8, 1024]),
            wx=b16v(W, 40960, [128, 8, 1024]),
            wr=b16v(W, 57344, [128, 8, 256]),
            wi=b16v(W, 61440, [128, 8, 256]),
        )

    def lru_prefetch():
        v = lru_views()
        src = lru_w_in_d.rearrange("(c p) n -> p c n", p=128)
        dma("pool", v["wgt"], src[:, :, 0:1024])
        dma("pool", v["wx"], src[:, :, 1024:2048])
        dma("pool", v["wr"], lru_wr_d.rearrange("p (a b) -> p a b", a=8))
        dma("pool", v["wi"], lru_wi_d.rearrange("p (a b) -> p a b", a=8))

    hbr = smallw[:, 32:40]
    hbi = smallw[:, 40:48]
    cnh = smallw[:, 48:56]
    ts(hbr, vecs[:, V_BR:V_BR + 8], 0.5, None, ALU.mult)
    ts(hbi, vecs[:, V_BI:V_BI + 8], 0.5, None, ALU.mult)
    ts(cnh, cneg, 0.5, None, ALU.mult)

    def lru_layer(b, l=1, after_gate=None):
        V = lru_views()
        wout, wgt, wx, wr, wi = (V[k] for k in ("wout", "wgt", "wx", "wr", "wi"))
        dma("pool", wout, lru_w_out_d.rearrange("(c p) n -> p c n", p=128))
        hT = b16v(hbuf, 0, [128, 8, 512])
        yT = b16v(hbuf, 8192, [128, 8, 512])
        xb2 = [f32v(hbuf, 16384, [128, 2, 516]), f32v(hbuf, 27136, [128, 2, 516])]
        xc2 = [f32v(hbuf, 20992, [128, 2, 512]), cTr[:, 0:1024].rearrange("p (a b) -> p a b", a=2)]
        xcb = [b16v(hbuf, 25088, [128, 2, 512]),
               cTr[:, 1024:1536].bitcast(BF16).rearrange("p (a b) -> p a b", a=2)]
        TR = [f32v(scr, 0, [128, 2, 512]), f32v(W, 16384, [128, 2, 512])]
        TI = [f32v(scr, 4096, [128, 2, 512]), f32v(W, 20480, [128, 2, 512])]
        AA = [f32v(scr, 8192, [128, 2, 512]), f32v(W, 65536, [128, 2, 512])]
        MM = [f32v(scr, 12288, [128, 2, 512]), f32v(W, 69632, [128, 2, 512])]
        memset(halo[:], 0.0)
        memset(carry[:], 0.0)
        norm_a(0, b, l, 0)
        for jb in range(NB):
            t0 = jb * TB
            norm_b(jb, b, l, 0, 0, hT)
            def Gmm(hb):
                for cc in range(2):
                    c = hb * 2 + cc
                    for kc in range(8):
                        mm(ps[cc][:], wgt[:, kc, c * 128:(c + 1) * 128], hT[:, kc, :],
                           start=(kc == 0), stop=(kc == 7))

            def A1mm(hb):
                for cc in range(2):
                    c = hb * 2 + cc
                    pb = ps[2 + cc]
                    for kc in range(8):
                        mm(pb[:], wx[:, kc, c * 128:(c + 1) * 128], hT[:, kc, :], start=(kc == 0), stop=(kc == 7))

            def A1(hb):
                X, XC, XB = xb2[hb % 2], xc2[hb % 2], xcb[hb % 2]
                for cc in range(2):
                    c = hb * 2 + cc
                    act(X[:, cc, 0:3], halo[:, c, 0:3], AF.Copy)
                for cc in range(2):
                    act(X[:, cc, 3:515], ps[2 + cc][:], AF.Copy)
                for cc in range(2):
                    c = hb * 2 + cc
                    cp(halo[:, c, 0:3], X[:, cc, 512:515], eng="dve")
                    ts(XC[:, cc, :], X[:, cc, 0:512], vcol(V_CW + 0 * 8 + c), vcol(V_CB + c), ALU.mult, ALU.add)
                    for j in range(1, 4):
                        stt(XC[:, cc, :], X[:, cc, j:j + 512], vcol(V_CW + j * 8 + c), XC[:, cc, :],
                            ALU.mult, ALU.add)

            def A2(hb):
                X, XC, XB = xb2[hb % 2], xc2[hb % 2], xcb[hb % 2]
                for cc in range(2):
                    act(XB[:, cc, :], XC[:, cc, :], AF.Copy)
                for oc in range(2):
                    for kk in range(2):
                        mm(ps[4 + 2 * oc][:], wr[:, hb * 2 + kk, oc * 128:(oc + 1) * 128], XB[:, kk, :],
                           start=(kk == 0), stop=(kk == 1))
                    for kk in range(2):
                        mm(ps[5 + 2 * oc][:], wi[:, hb * 2 + kk, oc * 128:(oc + 1) * 128], XB[:, kk, :],
                           start=(kk == 0), stop=(kk == 1))

            def B1(hb):
                t_r, t_i, a_, m_ = TR[hb % 2], TI[hb % 2], AA[hb % 2], MM[hb % 2]
                for cc in range(2):
                    act(yT[:, hb * 2 + cc, :], ps[cc][:], AF.Gelu_apprx_tanh)
                for oc in range(2):
                    c = hb * 2 + oc
                    act(t_r[:, oc, :], ps[4 + 2 * oc][:], AF.Tanh, bias=hbr[:, c:c + 1], scale=0.5)
                    act(t_i[:, oc, :], ps[5 + 2 * oc][:], AF.Tanh, bias=hbi[:, c:c + 1], scale=0.5)
                for oc in range(2):
                    c = hb * 2 + oc
                    act(a_[:, oc, :], t_r[:, oc, :], AF.Exp, bias=cnh[:, c:c + 1], scale=cnh[:, c:c + 1])
                    act(m_[:, oc, :], t_r[:, oc, :], AF.Exp, bias=cneg[:, c:c + 1], scale=cneg[:, c:c + 1])
                for oc in range(2):
                    act(m_[:, oc, :], m_[:, oc, :], AF.Sqrt, bias=0.25, scale=-0.25)

            def B2(hb):
                t_r, t_i, a_, m_ = TR[hb % 2], TI[hb % 2], AA[hb % 2], MM[hb % 2]
                hs = t_r
                XC = xc2[hb % 2]
                for oc in range(2):
                    c = hb * 2 + oc
                    stt(t_i[:, oc, :], t_i[:, oc, :], 1.0, XC[:, oc, :], ALU.add, ALU.mult)
                    tt(m_[:, oc, :], m_[:, oc, :], t_i[:, oc, :], ALU.mult)
                    scan(hs[:, oc, :], a_[:, oc, :], m_[:, oc, :], carry[:, c:c + 1], ALU.mult, ALU.add)
                    cp(carry[:, c:c + 1], hs[:, oc, 511:512], eng="dve")
                    tt(yT[:, c, :], hs[:, oc, :], yT[:, c, :], ALU.mult)

            Gmm(0); A1mm(0); A1(0); A1mm(1); A2(0)
            for hb in range(4):
                if hb + 1 < 4:
                    A1(hb + 1)
                if hb + 2 < 4:
                    A1mm(hb + 2)
                B1(hb)
                if hb + 1 < 4:
                    Gmm(hb + 1)
                    A2(hb + 1)
                elif jb == NB - 1 and after_gate is not None:
                    after_gate()
                B2(hb)
            if jb + 1 < NB:
                norm_a0(jb + 1)
            for m in range(8):
                if m == 4 and jb + 1 < NB:
                    norm_a(jb + 1, b, l, 0, do_sq=False)
                pb = ps[m % 2]
                for kc in range(8):
                    mm(pb[:], wout[:, kc, m * 128:(m + 1) * 128], yT[:, kc, :], start=(kc == 0), stop=(kc == 7))
                resid_update(pb[:], m, jb, ada_col(l, 2, m, b))

    def moe_wviews(slot):
        base = slot * 24576
        return (b16v(W, base, [128, 8, 512]), b16v(W, base + 8192, [128, 8, 512]),
                b16v(W, base + 16384, [128, 4, 1024]))

    moe_state = {"slot": 0}

    def moe_load(l, e, slot, parts=("g", "u", "d")):
        wg, wu, wd = moe_wviews(slot)
        if "g" in parts:
            dma("pool", wg, wg_d[l, e].rearrange("(c p) n -> p c n", p=128))
        if "u" in parts:
            dma("pool", wu, wu_d[l, e].rearrange("(c p) n -> p c n", p=128))
        if "d" in parts:
            dma("pool", wd, wd_d[l, e].rearrange("(c p) n -> p c n", p=128))

    SLOTS = [1, 2, 0, 1, 2, 0, 1, 2, 0, 1, 2, 0, 1, 2, 1, 0]

    def moe_layer(b, l, pre_last=None):
        hT = b16v(hbuf, 0, [128, 8, S])
        for jb in range(NB):
            norm_block(jb, b, l, 1, 3, hT[:, :, jb * TB:(jb + 1) * TB])
        for t in range(16):
            for kc in range(8):
                mm(ps[6][:, t * 16:(t + 1) * 16], hT[:, kc, t * 128:(t + 1) * 128], rw[:, kc, :],
                   start=(kc == 0), stop=(kc == 7))
        R = lambda i: rt[:, i * 256:(i + 1) * 256]
        R3 = lambda i: rt[:, i * 256:(i + 1) * 256].rearrange("p (t e) -> p t e", e=16)
        R4 = lambda i: rt[:, i * 256:(i + 1) * 256].rearrange("p (t g j) -> p t g j", g=4, j=4)
        G = lambda i: rt[:, 2304 + i * 64:2304 + (i + 1) * 64]
        G3 = lambda i: rt[:, 2304 + i * 64:2304 + (i + 1) * 64].rearrange("p (t g) -> p t g", g=4)
        sc_, sel_, msk, tmp_ = 0, 1, 2, 3
        act(R(sc_), ps[6][:, 0:256], AF.Sigmoid)
        tt(R3(sel_), R3(sc_), rb[:, :].unsqueeze(1).to_broadcast([128, 16, 16]), ALU.add)
        red(G(0), R4(sel_), ALU.max)
        tt(R4(msk), R4(sel_), G3(0).unsqueeze(3).to_broadcast([128, 16, 4, 4]), ALU.is_ge)
        stt(R(tmp_), R(msk), -1e30, R(sel_), ALU.mult, ALU.add)
        red(G(1), R4(tmp_), ALU.max)
        tt(G(2), G(0), G(1), ALU.add)
        red(rt[:, 1664:1680], G3(2), ALU.max)
        tt(G3(3), G3(2), rt[:, 1664:1680].unsqueeze(2).to_broadcast([128, 16, 4]), ALU.is_ge)
        tt(R4(msk), R4(sel_), G3(1).unsqueeze(3).to_broadcast([128, 16, 4, 4]), ALU.is_ge)
        tt(R4(msk), R4(msk), G3(3).unsqueeze(3).to_broadcast([128, 16, 4, 4]), ALU.mult)
        tt(R(tmp_), R(msk), R(sc_), ALU.mult)
        red(rt[:, 1680:1696], R3(tmp_), ALU.add)
        recip(rt[:, 1680:1696], rt[:, 1680:1696])
        tt(R3(tmp_), R3(tmp_), rt[:, 1680:1696].unsqueeze(2).to_broadcast([128, 16, 16]), ALU.mult)
        pk = rt[:, 1024:1536].rearrange("p (t e) -> p t e", e=32)
        hib = rt[:, 1536:1664].bitcast(BF16).rearrange("p (t e) -> p t e", e=16)
        cp(hib, R3(tmp_), eng="dve")
        cp(pk[:, :, 0:16], hib, eng="dve")
        tt(pk[:, :, 16:32], R3(tmp_), pk[:, :, 0:16], ALU.subtract)
        combT = cTr[0:32, 0:1024].bitcast(BF16)
        for t4 in range(4):
            for q in range(4):
                t = t4 * 4 + q
                tr(ps[7][0:32, q * 128:(q + 1) * 128], pk[:, t, :], ident[:])
            act(combT[:, t4 * 512:(t4 + 1) * 512], ps[7][0:32, :], AF.Copy)
        if "comb" in dbg_d and b == dbg.get("_b", 0) and l == dbg.get("_l", 0):
            dma("sp", dbg_d["comb"], combT)
        he = [b16v(scr, 0, [128, 4, 512]), b16v(scr, 4096, [128, 4, 512])]
        sg = [f32v(scr, 8192, [128, 512]), f32v(scr, 10240, [128, 512])]
        tb = [f32v(scr, 12288, [128, 512]), f32v(scr, 14336, [128, 512])]
        cbs = [f32v(scr, 16384, [128, 512]), f32v(scr, 18432, [128, 512])]
        it = 0
        for e in range(NE):
            slot = SLOTS[e]
            if e + 1 < NE:
                moe_load(l, e + 1, SLOTS[e + 1])
            elif pre_last is not None:
                pre_last()
            wg, wu, wd = moe_wviews(slot)
            for jb in range(NB):
                t0 = jb * TB
                k2 = it % 2
                it += 1
                mm(ps[6][:], i32b[:, e:e + 1].to_broadcast([32, 128]), combT[:, t0:t0 + TB])
                act(cbs[k2], ps[6][:], AF.Copy)
                for c in range(4):
                    pg = ps[0 + c % 2]
                    pu = ps[2 + c % 2]
                    for kc in range(8):
                        mm(pg[:], wg[:, kc, c * 128:(c + 1) * 128], hT[:, kc, t0:t0 + TB],
                           start=(kc == 0), stop=(kc == 7))
                    for kc in range(8):
                        mm(pu[:], wu[:, kc, c * 128:(c + 1) * 128], hT[:, kc, t0:t0 + TB],
                           start=(kc == 0), stop=(kc == 7))
                    act(sg[c % 2], pg[:], AF.Silu)
                    tt(tb[c % 2], pu[:], sg[c % 2], ALU.mult)
                    tt(he[k2][:, c, :], tb[c % 2], cbs[k2], ALU.mult)
                for m in range(8):
                    pd = ps[4 + m % 2]
                    for kc in range(4):
                        mm(pd[:], wd[:, kc, m * 128:(m + 1) * 128], he[k2][:, kc, :],
                           start=(kc == 0), stop=(kc == 3))
                    resid_update(pd[:], m, jb, ada_col(l, 5, m, b))

    def final_store(b, si):
        sq = b16v(scr, 0, [128, 8, 512])
        rstd = f32v(scr, 8192, [128, 512])
        ob = [f32v(hbuf, 0, [128, 8, 512]), f32v(hbuf, 16384, [128, 8, 512])]
        outs = []
        for jb in range(NB):
            t0 = jb * TB
            act(sq[:, :, :], xT[:, :, t0:t0 + TB], AF.Square)
            for c in range(8):
                mm(ps[6][:], ones_d[:], sq[:, c, :], start=(c == 0), stop=(c == 7))
            act(rstd, ps[6][:], AF.Ln, bias=EPS)
            act(rstd, rstd, AF.Exp, scale=-0.5)
            o = ob[jb % 2]
            for c in range(8):
                stt(o[:, c, :], xT[:, c, t0:t0 + TB], vcol(V_FNG + c), rstd, ALU.mult, ALU.mult)
            outs.append(dma("sp", outT_d[si, :, t0:t0 + TB].rearrange("(c p) t -> p c t", p=128), o))
        return outs

    out_ops = []
    phases = dbg.get("_phases", ("gla", "moe0", "lru", "moe1", "final"))
    for si in range(n_seq):
        b = si
        dma("sp", xT[:, :, :], xT_d[si].rearrange("(c p) t -> p c t", p=128))
        if si == 0 and "gla" in phases:
            gla_prefetch()
        if "gla" in phases:
            gla_layer(b, after_proj=(lambda: moe_load(0, 0, SLOTS[0])) if "moe0" in phases else None)
        if "xmid0" in dbg_d and si == dbg.get("_b", 0):
            out_ops.append(dma("sp", dbg_d["xmid0"].rearrange("(c p) t -> p c t", p=128), xT[:, :, :]))
        if "moe0" in phases:
            if "gla" not in phases:
                moe_load(0, 0, SLOTS[0])
            moe_layer(b, 0, pre_last=lru_prefetch if "lru" in phases else None)
        if "xmid1" in dbg_d and si == dbg.get("_b", 0):
            out_ops.append(dma("sp", dbg_d["xmid1"].rearrange("(c p) t -> p c t", p=128), xT[:, :, :]))
        if "lru" in phases:
            if "moe0" not in phases:
                lru_prefetch()
            lru_layer(b, after_gate=(lambda: moe_load(1, 0, SLOTS[0], ("g", "u"))) if "moe1" in phases else None)
            if "moe1" in phases:
                moe_load(1, 0, SLOTS[0], ("d",))
        if "xmid2" in dbg_d and si == dbg.get("_b", 0):
            out_ops.append(dma("sp", dbg_d["xmid2"].rearrange("(c p) t -> p c t", p=128), xT[:, :, :]))
        if "moe1" in phases:
            if "lru" not in phases:
                moe_load(1, 0, SLOTS[0])
            moe_layer(b, 1, pre_last=gla_prefetch if (si + 1 < n_seq and "gla" in phases) else None)
        if "final" in phases:
            out_ops += final_store(b, si)

    Sd.add("sp", lambda e: e.nop(), reads=[], writes=[]).deps.extend(
        [o for o in Sd.dma_ops["sp"]])

    Sd.finalize()

    sem_stack = contextlib.ExitStack()
    csem = {e: sem_stack.enter_context(nc.semaphore("c_" + e)) for e in Sched.ENGS}
    dsems = {e: [sem_stack.enter_context(nc.semaphore("d_%s_%d" % (e, i))) for i in range(Sched.NDS)]
             for e in ("sp", "pool")}
    with nc.Block() as block:
        @block.tensor
        def _(e):
            Sd.emit("pe", e, csem, dsems)

        @block.scalar
        def _(e):
            Sd.emit("act", e, csem, dsems)

        @block.vector
        def _(e):
            Sd.emit("dve", e, csem, dsems)

        @block.gpsimd
        def _(e):
            Sd.emit("pool", e, csem, dsems)

        @block.sync
        def _(e):
            Sd.emit("sp", e, csem, dsems)
    sem_stack.close()
    stack.close()
    return nc, Sd


V_NMG = 0
V_NFG = 16
V_FNG = 32
V_GNG = 40
V_CW = 42
V_CB = 74
V_BR = 82
V_BI = 90
V_LAM = 98
NV = 106


def _fm(v):
    v = np.asarray(v, np.float32).reshape(-1, 128)
    return np.ascontiguousarray(v.T)


def prep_inputs(inp):
    f = lambda a: np.ascontiguousarray(np.asarray(a, dtype=np.float32))
    x = f(inp["x"])
    c = f(inp["c"])
    vec = np.zeros((128, NV), np.float32)
    for l in range(2):
        vec[:, V_NMG + l * 8:V_NMG + (l + 1) * 8] = _fm(inp["norm_mix_g"][l])
        vec[:, V_NFG + l * 8:V_NFG + (l + 1) * 8] = _fm(inp["norm_ffn_g"][l])
    vec[:, V_FNG:V_FNG + 8] = _fm(inp["final_norm_g"])
    vec[:, V_GNG:V_GNG + 2] = _fm(inp["gla_norm_g"][0])
    for j in range(4):
        vec[:, V_CW + j * 8:V_CW + (j + 1) * 8] = _fm(inp["lru_conv_w"][0, j])
    vec[:, V_CB:V_CB + 8] = _fm(inp["lru_conv_b"][0])
    vec[:, V_BR:V_BR + 8] = _fm(np.asarray(inp["lru_b_r"][0]).reshape(-1))
    vec[:, V_BI:V_BI + 8] = _fm(np.asarray(inp["lru_b_i"][0]).reshape(-1))
    vec[:, V_LAM:V_LAM + 8] = _fm(inp["lru_lambda"][0])
    ada_b = np.concatenate([_fm(inp["ada_b"][0]), _fm(inp["ada_b"][1])], axis=1)
    wgu = np.concatenate([f(inp["gla_w_gate_up"][0]), f(inp["gla_b_gate"][0])[None, :]], axis=0)

    def blk(w):
        w = f(w).reshape(4, 2, 128, 256).transpose(2, 0, 1, 3)
        return np.ascontiguousarray(w).reshape(128, 8 * 256)

    rwl = f(inp["router_w"]).reshape(8, 128, 16).transpose(1, 0, 2).reshape(128, 128)
    shared = {
        "ada_w": f(inp["ada_w"]), "ada_b": ada_b, "vecs": vec,
        "gla_w_in": f(inp["gla_w_in"][0]), "gla_w_out": f(inp["gla_w_out"][0]), "wgu": wgu,
        "lru_w_in": f(inp["lru_w_in"][0]), "lru_w_out": f(inp["lru_w_out"][0]),
        "lru_wr": blk(inp["lru_w_r"][0]), "lru_wi": blk(inp["lru_w_i"][0]),
        "rw": np.ascontiguousarray(rwl), "rb": np.ascontiguousarray(np.tile(f(inp["router_bias"])[None, :], (128, 1))),
        "moe_wg": f(inp["moe_w_gate"]), "moe_wu": f(inp["moe_w_up"]), "moe_wd": f(inp["moe_w_down"]),
        "ident": np.eye(128, dtype=np.float32),
    }
    s_i = np.arange(128)[:, None]
    c_i = np.arange(128)[None, :]
    shared["mtri"] = (((s_i > c_i) & (s_i // 64 == c_i // 64)).astype(np.float32) / 16.0)
    shared["ind"] = ((s_i // 64) == np.arange(2)[None, :]).astype(np.float32) / 16.0
    shared["i16"] = np.eye(16, dtype=np.float32)
    in_maps = []
    for i in range(N_CORES):
        m = dict(shared)
        m["xT"] = np.ascontiguousarray(x[2 * i:2 * i + 2].transpose(0, 2, 1))
        cc = c[2 * i:2 * i + 2]
        m["cT"] = np.ascontiguousarray(cc.reshape(2, 8, 128).transpose(2, 1, 0).reshape(128, 16))
        in_maps.append(m)
    return in_maps


_CACHE = {}


def kernel(**inputs):
    in_maps = prep_inputs(inputs)
    if "nc" not in _CACHE:
        _CACHE["nc"] = build_program()[0]
    nc = _CACHE["nc"]
    res = run_bass_kernel_spmd(nc, in_maps, core_ids=list(range(N_CORES)))
    out = np.empty((16, S, D), np.float32)
    for i in range(N_CORES):
        o = res.results[i]["outT"]
        out[2 * i:2 * i + 2] = o.transpose(0, 2, 1)
    return out
```

```python
import numpy as np
import concourse.bass as bass
import concourse.mybir as mybir
from concourse.bass_utils import run_bass_kernel_spmd

F32 = mybir.dt.float32
BF16 = mybir.dt.bfloat16
AF = mybir.ActivationFunctionType
ALU = mybir.AluOpType
ESZ = {F32: 4, BF16: 2}

D = 1024
S = 2048
NB = 4
TB = 512
NE = 16
DE = 512
EPS = 1e-6
GRAN = 512
N_CORES = 8


class Op:
    __slots__ = ("eng", "idx", "fn", "deps", "dma", "dsem", "dval", "sig", "sigval", "waits", "gidx")


class Sched:
    ENGS = ("pe", "act", "dve", "pool", "sp")
    NDS = 12

    def __init__(self, nc):
        self.nc = nc
        self.ops = {e: [] for e in self.ENGS}
        self.last_w = {}
        self.readers = {}
        self.ndma = {e: 0 for e in self.ENGS}
        self.dma_ops = {e: [] for e in self.ENGS}
        self.tok_cache = {}
        self.gcount = 0

    def tokens(self, ap):
        sp = str(ap.space)
        if "SB" not in sp and "PSUM" not in sp:
            return ()
        if "PSUM" in sp:
            return ((ap.tensor.name, 0),)
        key = (ap.tensor.name, ap.offset, ap.ap, ap.dtype)
        t = self.tok_cache.get(key)
        if t is None:
            es = ESZ[ap.dtype]
            pstride = ap.ap[0][0]
            off = ap.offset % pstride if pstride > 0 else ap.offset
            name = ap.tensor.name
            dims = [(abs(st), cnt) for st, cnt in ap.ap[1:] if cnt > 1 and st != 0]
            dims.sort(reverse=True)
            outer = dims[:-1] if dims else []
            n_outer = 1
            for _, cnt in outer:
                n_outer *= cnt
            gs = set()
            if dims and n_outer <= 512:
                lst, lcnt = dims[-1]
                bases = [off]
                for st, cnt in outer:
                    bases = [b0 + i * st for b0 in bases for i in range(cnt)]
                for b0 in bases:
                    lo = b0 * es
                    hi = (b0 + (lcnt - 1) * lst + 1) * es
                    gs.update(range(lo // GRAN, (hi - 1) // GRAN + 1))
            else:
                span = 0
                for st, cnt in dims:
                    span += (cnt - 1) * st
                lo = off * es
                hi = (off + span + 1) * es
                gs.update(range(lo // GRAN, (hi - 1) // GRAN + 1))
            t = tuple((name, g) for g in sorted(gs))
            self.tok_cache[key] = t
        return t

    def add(self, eng, fn, reads=(), writes=(), dma=False):
        op = Op()
        op.eng = eng
        op.idx = len(self.ops[eng])
        op.fn = fn
        op.dma = dma
        op.sig = False
        op.sigval = 0
        op.gidx = self.gcount
        self.gcount += 1
        deps = {}
        rt = []
        for ap in reads:
            rt.extend(self.tokens(ap))
        wt = []
        for ap in writes:
            wt.extend(self.tokens(ap))
        for r in rt:
            w = self.last_w.get(r)
            if w is not None:
                deps[id(w)] = w
        for r in wt:
            w = self.last_w.get(r)
            if w is not None:
                deps[id(w)] = w
            rd = self.readers.get(r)
            if rd:
                for o in rd.values():
                    deps[id(o)] = o
        deps.pop(id(op), None)
        op.deps = list(deps.values())
        for r in rt:
            d = self.readers.get(r)
            if d is None:
                d = {}
                self.readers[r] = d
            if dma:
                d[(eng, op.idx)] = op
            else:
                d[eng] = op
        for r in wt:
            self.last_w[r] = op
            self.readers[r] = {}
        if dma:
            i = self.ndma[eng]
            self.ndma[eng] = i + 1
            op.dsem = i % self.NDS
            op.dval = 16 * (i // self.NDS + 1)
            if i >= self.NDS:
                op.deps.append(self.dma_ops[eng][i - self.NDS])
            self.dma_ops[eng].append(op)
        self.ops[eng].append(op)
        return op

    def finalize(self):
        for eng in self.ENGS:
            known = {}
            for op in self.ops[eng]:
                need = {}
                for d in op.deps:
                    if d.dma:
                        k = ("d", d.eng, d.dsem)
                        v = d.dval
                        if v > need.get(k, (0, None))[0]:
                            need[k] = (v, d)
                    else:
                        if d.eng == "pe" and eng == "pe" and not op.dma:
                            continue
                        k = ("c", d.eng)
                        v = d.idx + 1
                        if v > need.get(k, (0, None))[0]:
                            need[k] = (v, d)
                waits = []
                for k, (v, d) in need.items():
                    if known.get(k, 0) >= v:
                        continue
                    known[k] = v
                    waits.append(d)
                    if not d.dma:
                        d.sig = True
                op.waits = waits
        for eng in self.ENGS:
            c = 0
            for op in self.ops[eng]:
                if op.sig and not op.dma:
                    c += 1
                    op.sigval = c

    def emit(self, eng, e, csem, dsems):
        for op in self.ops[eng]:
            for d in op.waits:
                if d.dma:
                    e.wait_ge(dsems[d.eng][d.dsem], d.dval)
                else:
                    e.wait_ge(csem[d.eng], d.sigval)
            ins = op.fn(e)
            if op.dma:
                ins.then_inc(dsems[eng][op.dsem], 16)
            elif op.sig:
                ins.then_inc(csem[eng], 1)


def build_program(n_seq=2, dbg=None):
    dbg = dbg or {}
    nc = bass.Bass("TRN2", target_bir_lowering=False)
    Sd = Sched(nc)

    def din(name, shape):
        return nc.dram_tensor(name, list(shape), F32, kind="ExternalInput").ap()

    xT_d = din("xT", [2, D, S])
    cT_d = din("cT", [128, 16])
    ada_w_d = din("ada_w", [2, D, 6 * D])
    ada_b_d = din("ada_b", [128, 96])
    vecs_d = din("vecs", [128, NV])
    gla_w_in_d = din("gla_w_in", [D, 3088])
    gla_w_out_d = din("gla_w_out", [D, D])
    wgu_d = din("wgu", [17, 512])
    lru_w_in_d = din("lru_w_in", [D, 2 * D])
    lru_w_out_d = din("lru_w_out", [D, D])
    lru_wr_d = din("lru_wr", [128, 8 * 256])
    lru_wi_d = din("lru_wi", [128, 8 * 256])
    rw_d = din("rw", [128, 8 * 16])
    rb_d = din("rb", [128, 16])
    wg_d = din("moe_wg", [2, NE, D, DE])
    wu_d = din("moe_wu", [2, NE, D, DE])
    wd_d = din("moe_wd", [2, NE, DE, D])
    ident_d = din("ident", [128, 128])
    mtri_d = din("mtri", [128, 128])
    ind_d = din("ind", [128, 2])
    i16_d = din("i16", [16, 16])
    outT_d = nc.dram_tensor("outT", [2, D, S], F32, kind="ExternalOutput").ap()
    dbg_d = {}
    for k, shp in dbg.items():
        if k.startswith("_"):
            continue
        dbg_d[k] = nc.dram_tensor("dbg_" + k, list(shp), F32, kind="ExternalOutput").ap()

    import contextlib
    stack = contextlib.ExitStack()

    def SB(name, shape, dt=F32):
        return stack.enter_context(nc.sbuf_tensor("s_" + name, list(shape), dt))

    def PS(name):
        return stack.enter_context(nc.psum_tensor(name, [128, 512], F32))

    xT = SB("xT", [128, 8, S])
    hbuf = SB("hbuf", [128, 16384], BF16)
    W = SB("W", [128, 36864], BF16)
    scr = SB("scr", [128, 10240], BF16)
    cTr = SB("cTr", [128, 2048])
    ident = SB("ident", [128, 128])
    mtri = SB("mtri", [128, 128])
    ind = SB("ind", [128, 2])
    i32f = SB("i32f", [32, 16])
    i32b = SB("i32b", [32, 16], BF16)
    ones_d = SB("ones_d", [128, 128], BF16)
    ones_v = SB("ones_v", [128, 128], BF16)
    vecs = SB("vecs", [128, NV])
    cT_s = SB("cT_s", [128, 16])
    cond = SB("cond", [128, 16], BF16)
    ada_b = SB("ada_b", [128, 96])
    ada = SB("ada", [128, 2, 48, 2])
    mod = SB("mod", [128, 2, 4, 8, 2])
    rw = SB("rw", [128, 8, 16], BF16)
    rb = SB("rb", [128, 16])
    wgu = SB("wgu", [32, 512])
    smallw = SB("smallw", [128, 256])
    halo = SB("halo", [128, 8, 4])
    carry = SB("carry", [128, 8])
    rt = scr[:, 0:5120].bitcast(F32)

    ps = [PS("ps%d" % i) for i in range(8)]

    def mm(out, lhsT, rhs, start=True, stop=True):
        return Sd.add("pe", lambda e: e.matmul(out, lhsT, rhs, start=start, stop=stop),
                      reads=[lhsT, rhs], writes=[out])

    def tr(out, in_, idn):
        return Sd.add("pe", lambda e: e.transpose(out, in_, idn), reads=[in_, idn], writes=[out])

    def act(out, in_, func, bias=None, scale=None, eng="act"):
        rd = [in_]
        kw = {}
        if bias is not None:
            kw["bias"] = bias
            if not isinstance(bias, (int, float)):
                rd.append(bias)
        if scale is not None:
            kw["scale"] = scale
            if not isinstance(scale, (int, float)):
                rd.append(scale)
        return Sd.add("act", lambda e: e.activation(out, in_, func, **kw), reads=rd, writes=[out])

    def stt(out, in0, scalar, in1, op0, op1, eng="dve"):
        rd = [in0, in1]
        if not isinstance(scalar, (int, float)):
            rd.append(scalar)
        return Sd.add(eng, lambda e: e.scalar_tensor_tensor(out, in0, scalar, in1, op0, op1),
                      reads=rd, writes=[out])

    def ts(out, in0, s1, s2, op0, op1=None, eng="dve"):
        rd = [in0]
        for s in (s1, s2):
            if s is not None and not isinstance(s, (int, float)):
                rd.append(s)
        if op1 is None:
            return Sd.add(eng, lambda e: e.tensor_scalar(out, in0, s1, None, op0), reads=rd, writes=[out])
        return Sd.add(eng, lambda e: e.tensor_scalar(out, in0, s1, s2, op0, op1), reads=rd, writes=[out])

    def tt(out, in0, in1, op, eng="dve"):
        return Sd.add(eng, lambda e: e.tensor_tensor(out, in0, in1, op), reads=[in0, in1], writes=[out])

    def cp(out, in_, eng="dve"):
        return Sd.add(eng, lambda e: e.tensor_copy(out, in_), reads=[in_], writes=[out])

    def recip(out, in_):
        return Sd.add("dve", lambda e: e.reciprocal(out, in_), reads=[in_], writes=[out])

    def red(out, in_, op, eng="dve"):
        return Sd.add(eng, lambda e: e.tensor_reduce(out, in_, mybir.AxisListType.X, op),
                      reads=[in_], writes=[out])

    def scan(out, d0, d1, init, op0, op1):
        rd = [d0, d1]
        if not isinstance(init, (int, float)):
            rd.append(init)
        return Sd.add("dve", lambda e: e.tensor_tensor_scan(out, d0, d1, init, op0, op1),
                      reads=rd, writes=[out])

    def memset(ap, val, eng="dve"):
        return Sd.add(eng, lambda e: e.memset(ap, val), writes=[ap])

    def dma(q, out, in_, **kw):
        return Sd.add(q, lambda e: e.dma_start(out=out, in_=in_, **kw), reads=[in_], writes=[out], dma=True)

    def f32v(t, off_b, shape):
        n = int(np.prod(shape[1:]))
        v = t[0:shape[0], off_b // 2: off_b // 2 + 2 * n].bitcast(F32)
        if len(shape) == 3:
            v = v.rearrange("p (a b) -> p a b", a=shape[1])
        elif len(shape) == 4:
            v = v.rearrange("p (a b c) -> p a b c", a=shape[1], b=shape[2])
        return v

    def b16v(t, off_b, shape):
        n = int(np.prod(shape[1:]))
        v = t[0:shape[0], off_b // 2: off_b // 2 + n]
        if len(shape) == 3:
            v = v.rearrange("p (a b) -> p a b", a=shape[1])
        elif len(shape) == 4:
            v = v.rearrange("p (a b c) -> p a b c", a=shape[1], b=shape[2])
        return v

    def vcol(i):
        return vecs[:, i:i + 1]

    dma("sp", ident[:], ident_d)
    dma("sp", mtri[:], mtri_d)
    dma("sp", ind[:], ind_d)
    dma("sp", i32f[0:16, :], i16_d)
    dma("sp", i32f[16:32, :], i16_d)
    cp(i32b[:], i32f[:], eng="dve")
    dma("sp", vecs[:], vecs_d)
    dma("sp", cT_s[:], cT_d)
    dma("sp", ada_b[:], ada_b_d)
    dma("sp", rb[:], rb_d)
    dma("sp", wgu[0:17, :], wgu_d)
    dma("pool", rw[:].rearrange("p a b -> p (a b)"), rw_d)
    memset(ones_d[:], 1.0 / 1024.0)
    memset(ones_v[:], 1.0 / 256.0)
    act(cond[:], cT_s[:], AF.Silu)

    NPIECE = 12
    for l in range(2):
        for pc in range(NPIECE):
            slot = (l * NPIECE + pc) % 3
            wv = b16v(hbuf, slot * 8192, [128, 8, 512])
            dma("pool", wv, ada_w_d[l, :, pc * 512:(pc + 1) * 512].rearrange("(c p) n -> p c n", p=128))
            for oc4 in range(4):
                oc = pc * 4 + oc4
                for kc in range(8):
                    mm(ps[7][:, (l * 48 + oc) * 2:(l * 48 + oc) * 2 + 2],
                       wv[:, kc, oc4 * 128:(oc4 + 1) * 128],
                       cond[:, kc * 2:kc * 2 + 2], start=(kc == 0), stop=(kc == 7))
    for l in range(2):
        tt(ada[:, l, :, :], ps[7][:, l * 96:(l + 1) * 96].rearrange("p (a b) -> p a b", b=2),
           ada_b[:, l * 48:(l + 1) * 48].unsqueeze(2).to_broadcast([128, 48, 2]), ALU.add)
    for l in range(2):
        for j, (which, gcol) in enumerate(((1, V_NMG + l * 8), (4, V_NFG + l * 8))):
            ts(mod[:, l, j, :, :], ada[:, l, which * 8:(which + 1) * 8, :], 1.0, None, ALU.add)
            tt(mod[:, l, j, :, :], mod[:, l, j, :, :],
               vecs[:, gcol:gcol + 8].unsqueeze(2).to_broadcast([128, 8, 2]), ALU.mult)

    def A_of(l, j, c, b):
        return mod[:, l, j, c, b:b + 1]

    def ada_col(l, which, c, b):
        return ada[:, l, which * 8 + c, b:b + 1]

    lam = vecs[:, V_LAM:V_LAM + 8]
    sw_a = smallw[:, 0:8]
    sw_b = smallw[:, 8:16]
    cneg = smallw[:, 16:24]
    cneg2 = smallw[:, 24:32]
    ts(sw_b, lam, 0.0, None, ALU.min)
    stt(sw_a, sw_b, 2.0, lam, ALU.mult, ALU.subtract)
    act(sw_a, sw_a, AF.Exp)
    act(sw_a, sw_a, AF.Ln, bias=1.0)
    tt(sw_a, sw_a, sw_b, ALU.subtract)
    ts(cneg, sw_a, -8.0, None, ALU.mult)
    ts(cneg2, sw_a, -16.0, None, ALU.mult)

    def norm_a0(jb):
        t0 = jb * TB
        sq = b16v(scr, 0, [128, 8, 512])
        act(sq[:, :, :], xT[:, :, t0:t0 + TB], AF.Square)

    def norm_a(jb, b, l, j, npre=2, do_sq=True):
        t0 = jb * TB
        sq = b16v(scr, 0, [128, 8, 512])
        rstd = f32v(scr, 8192, [128, 512])
        tmp = [f32v(scr, 10240, [128, 512]), f32v(scr, 12288, [128, 512])]
        if do_sq:
            norm_a0(jb)
        for c in range(8):
            mm(ps[6][:], ones_d[:], sq[:, c, :], start=(c == 0), stop=(c == 7))
        act(rstd, ps[6][:], AF.Ln, bias=EPS)
        act(rstd, rstd, AF.Exp, scale=-0.5)
        for c in range(npre):
            stt(tmp[c % 2], xT[:, c, t0:t0 + TB], A_of(l, j, c, b), rstd, ALU.mult, ALU.mult)

    def norm_b(jb, b, l, j, shift_which, dst, npre=2):
        t0 = jb * TB
        rstd = f32v(scr, 8192, [128, 512])
        tmp = [f32v(scr, 10240, [128, 512]), f32v(scr, 12288, [128, 512])]
        for c in range(8):
            tm = tmp[c % 2]
            if c >= npre:
                stt(tm, xT[:, c, t0:t0 + TB], A_of(l, j, c, b), rstd, ALU.mult, ALU.mult)
            act(dst[:, c, :], tm, AF.Identity, bias=ada_col(l, shift_which, c, b))

    def norm_block(jb, b, l, j, shift_which, dst):
        norm_a(jb, b, l, j, npre=0)
        norm_b(jb, b, l, j, shift_which, dst, npre=0)

    def resid_update(pbank, m, jb, gcol):
        t0 = jb * TB
        stt(xT[:, m, t0:t0 + TB], pbank, gcol, xT[:, m, t0:t0 + TB], ALU.mult, ALU.add)

    wa = SB("wa", [128, 8, 128], BF16)
    memset(wa[:], 0.0)

    def gla_views():
        return dict(
            wout=b16v(W, 0, [128, 8, 1024]),
            Sst=f32v(W, 16384, [128, 4, 256]),
            Sb=b16v(W, 20480, [128, 4, 256]),
            gam=f32v(W, 22528, [128, 4, 32]),
            wq=b16v(W, 24576, [128, 8, 512]),
            wk=b16v(W, 32768, [128, 8, 512]),
            wv=b16v(W, 40960, [128, 8, 1024]),
            wg=b16v(W, 57344, [128, 8, 1024]),
        )

    def gla_prefetch():
        v = gla_views()
        src = gla_w_in_d.rearrange("(c p) n -> p c n", p=128)
        dma("pool", wa[:, :, 0:16], src[:, :, 3072:3088])
        dma("pool", v["wq"], src[:, :, 0:512])
        dma("pool", v["wg"], src[:, :, 2048:3072])
        dma("pool", v["wk"], src[:, :, 512:1024])
        dma("pool", v["wv"], src[:, :, 1024:2048])

    def gla_layer(b, l=0, after_proj=None):
        V = gla_views()
        wout, Sst, Sb, gam, wq, wk, wv, wg = (V[k] for k in ("wout", "Sst", "Sb", "gam", "wq", "wk", "wv", "wg"))
        dma("pool", wout, gla_w_out_d.rearrange("(c p) n -> p c n", p=128))
        memset(Sst, 0.0)
        hT = b16v(hbuf, 0, [128, 8, 512])
        qT = b16v(hbuf, 8192, [128, 4, 512])
        kdec = b16v(hbuf, 12288, [128, 4, 512])
        vv = b16v(hbuf, 16384, [128, 4, 1024])
        sgT = b16v(hbuf, 24576, [128, 8, 512])
        oT = f32v(scr, 0, [128, 8, 512])
        m4 = f32v(scr, 0, [128, 4, 512])
        u4 = f32v(scr, 8192, [128, 4, 512])
        alrT = f32v(scr, 16384, [32, 512])
        expD = f32v(scr, 18432, [128, 512])
        la = cTr[:, :].rearrange("p (a b) -> p a b", a=4)
        yT = hT
        rstd4 = f32v(hbuf, 8192, [128, 4, 512])
        sq2 = b16v(hbuf, 16384, [128, 8, 512])
        tn = [f32v(scr, 16384, [128, 512]), f32v(scr, 18432, [128, 512])]
        QS = 128.0 ** -0.5
        norm_a(0, b, l, 0)
        for jb in range(NB):
            t0 = jb * TB
            norm_b(jb, b, l, 0, 0, hT)
            for kc in range(8):
                mm(ps[2][:], wa[:, kc, :], hT[:, kc, :], start=(kc == 0), stop=(kc == 7))
            memset(alrT, 1.0)
            cp(alrT[0:16, :], ps[2][0:16, :], eng="dve")
            for tt_ in range(4):
                pz = ps[3 + tt_ % 2]
                mm(pz[:], alrT[0:17, tt_ * 128:(tt_ + 1) * 128], wgu[0:17, :])
                ts(m4[:, tt_, :], pz[:], 0.0, None, ALU.min)
                stt(u4[:, tt_, :], m4[:, tt_, :], 2.0, pz[:], ALU.mult, ALU.subtract)
            for hd in range(4):
                pb = ps[hd % 2]
                for kc in range(8):
                    mm(pb[:], wq[:, kc, hd * 128:(hd + 1) * 128], hT[:, kc, :], start=(kc == 0), stop=(kc == 7))
                act(qT[:, hd, :], pb[:], AF.Copy, scale=QS)
            act(u4[:, :, :], u4[:, :, :], AF.Exp)
            act(u4[:, :, :], u4[:, :, :], AF.Ln, bias=1.0)
            tt(la[:, :, :], m4[:, :, :], u4[:, :, :], ALU.subtract)
            for gc in range(8):
                pb = ps[gc % 2]
                for kc in range(8):
                    mm(pb[:], wg[:, kc, gc * 128:(gc + 1) * 128], hT[:, kc, :], start=(kc == 0), stop=(kc == 7))
                act(sgT[:, gc, :], pb[:], AF.Silu)
            for tt_ in range(4):
                mm(ps[3][:], mtri[:], la[:, tt_, :])
                for hd in range(4):
                    mm(ps[7][:, hd * 2:hd * 2 + 2], la[:, tt_, hd * 128:(hd + 1) * 128], ind[:])
                for kc in range(8):
                    mm(ps[4 + tt_ % 2][:], hT[:, kc, tt_ * 128:(tt_ + 1) * 128], wk[:, kc, :],
                       start=(kc == 0), stop=(kc == 7))
                act(expD, ps[3][:], AF.Exp)
                n0 = jb * 8 + tt_ * 2
                act(gam[:, :, n0:n0 + 2], ps[7][:, 0:8].rearrange("p (a b) -> p a b", b=2), AF.Exp)
                tt(kdec[:, tt_, :], ps[4 + tt_ % 2][:], expD, ALU.mult)
                for vh in range(2):
                    pb = ps[vh]
                    for kc in range(8):
                        mm(pb[:], hT[:, kc, tt_ * 128:(tt_ + 1) * 128], wv[:, kc, vh * 512:(vh + 1) * 512],
                           start=(kc == 0), stop=(kc == 7))
                    act(vv[:, tt_, vh * 512:(vh + 1) * 512], pb[:], AF.Copy)
            if jb == NB - 1 and after_proj is not None:
                after_proj()
            def kv_mm(n):
                tt_ = n // 2
                p0 = (n % 2) * 64
                kvb = (ps[0], ps[1]) if n % 2 == 0 else (ps[4], ps[5])
                for hd in range(4):
                    mm(kvb[hd // 2][:, (hd % 2) * 256:(hd % 2 + 1) * 256],
                       kdec[p0:p0 + 64, tt_, hd * 128:(hd + 1) * 128],
                       vv[p0:p0 + 64, tt_, hd * 256:(hd + 1) * 256])
            kv_mm(0)
            for n in range(8):
                ng = jb * 8 + n
                kvb = (ps[0], ps[1]) if n % 2 == 0 else (ps[4], ps[5])
                if n + 1 < 8:
                    kv_mm(n + 1)
                for hd in range(4):
                    stt(Sst[:, hd, :], Sst[:, hd, :], gam[:, hd, ng:ng + 1],
                        kvb[hd // 2][:, (hd % 2) * 256:(hd % 2 + 1) * 256], ALU.mult, ALU.add)
                    act(Sb[:, hd, :], Sst[:, hd, :], AF.Copy)
                    for dvc in range(2):
                        ch = hd * 2 + dvc
                        mm(ps[2 + n % 2][:, ch * 64:(ch + 1) * 64], Sb[:, hd, dvc * 128:(dvc + 1) * 128],
                           qT[:, hd, n * 64:(n + 1) * 64])
                if n >= 1:
                    cp(oT[:, :, (n - 1) * 64:n * 64], ps[2 + (n - 1) % 2][:].rearrange("p (a b) -> p a b", a=8),
                       eng="dve")
            cp(oT[:, :, 7 * 64:8 * 64], ps[2 + 7 % 2][:].rearrange("p (a b) -> p a b", a=8), eng="dve")
            for hd in range(4):
                act(sq2[:, 2 * hd:2 * hd + 2, :], oT[:, 2 * hd:2 * hd + 2, :], AF.Square)
                pn = ps[6 + hd % 2]
                for dvc in range(2):
                    mm(pn[:], ones_v[:], sq2[:, hd * 2 + dvc, :], start=(dvc == 0), stop=(dvc == 1))
                act(rstd4[:, hd, :], pn[:], AF.Ln, bias=EPS)
                act(rstd4[:, hd, :], rstd4[:, hd, :], AF.Exp, scale=-0.5)
            for hd in range(4):
                for dvc in range(2):
                    ch = hd * 2 + dvc
                    stt(tn[dvc], oT[:, ch, :], vcol(V_GNG + dvc), rstd4[:, hd, :], ALU.mult, ALU.mult)
                    tt(yT[:, ch, :], tn[dvc], sgT[:, ch, :], ALU.mult)
            if jb + 1 < NB:
                norm_a0(jb + 1)
            for m in range(8):
                if m == 4 and jb + 1 < NB:
                    norm_a(jb + 1, b, l, 0, do_sq=False)
                pb = ps[m % 2]
                for kc in range(8):
                    mm(pb[:], wout[:, kc, m * 128:(m + 1) * 128], yT[:, kc, :], start=(kc == 0), stop=(kc == 7))
                resid_update(pb[:], m, jb, ada_col(l, 2, m, b))

    def lru_views():
        return dict(
            wout=b16v(W, 0, [128, 8, 1024]),
            wgt=b16v(W, 24576, [128, 8, 1024]),
            wx=b16v(W, 40960, [128, 8, 1024]),
            wr=b16v(W, 57344, [128, 8, 256]),
            wi=b16v(W, 61440, [128, 8, 256]),
        )

    def lru_prefetch():
        v = lru_views()
        src = lru_w_in_d.rearrange("(c p) n -> p c n", p=128)
        dma("pool", v["wgt"], src[:, :, 0:1024])
        dma("pool", v["wx"], src[:, :, 1024:2048])
        dma("pool", v["wr"], lru_wr_d.rearrange("p (a b) -> p a b", a=8))
        dma("pool", v["wi"], lru_wi_d.rearrange("p (a b) -> p a b", a=8))

    hbr = smallw[:, 32:40]
    hbi = smallw[:, 40:48]
    cnh = smallw[:, 48:56]
    ts(hbr, vecs[:, V_BR:V_BR + 8], 0.5, None, ALU.mult)
    ts(hbi, vecs[:, V_BI:V_BI + 8], 0.5, None, ALU.mult)
    ts(cnh, cneg, 0.5, None, ALU.mult)

    def lru_layer(b, l=1, after_gate=None):
        V = lru_views()
        wout, wgt, wx, wr, wi = (V[k] for k in ("wout", "wgt", "wx", "wr", "wi"))
        dma("pool", wout, lru_w_out_d.rearrange("(c p) n -> p c n", p=128))
        hT = b16v(hbuf, 0, [128, 8, 512])
        yT = b16v(hbuf, 8192, [128, 8, 512])
        xb2 = [f32v(hbuf, 16384, [128, 2, 516]), f32v(hbuf, 27136, [128, 2, 516])]
        xc2 = [f32v(hbuf, 20992, [128, 2, 512]), cTr[:, 0:1024].rearrange("p (a b) -> p a b", a=2)]
        xcb = [b16v(hbuf, 25088, [128, 2, 512]),
               cTr[:, 1024:1536].bitcast(BF16).rearrange("p (a b) -> p a b", a=2)]
        TR = [f32v(scr, 0, [128, 2, 512]), f32v(W, 16384, [128, 2, 512])]
        TI = [f32v(scr, 4096, [128, 2, 512]), f32v(W, 20480, [128, 2, 512])]
        AA = [f32v(scr, 8192, [128, 2, 512]), f32v(W, 65536, [128, 2, 512])]
        MM = [f32v(scr, 12288, [128, 2, 512]), f32v(W, 69632, [128, 2, 512])]
        memset(halo[:], 0.0)
        memset(carry[:], 0.0)
        norm_a(0, b, l, 0)
        for jb in range(NB):
            t0 = jb * TB
            norm_b(jb, b, l, 0, 0, hT)
            def Gmm(hb):
                for cc in range(2):
                    c = hb * 2 + cc
                    for kc in range(8):
                        mm(ps[cc][:], wgt[:, kc, c * 128:(c + 1) * 128], hT[:, kc, :],
                           start=(kc == 0), stop=(kc == 7))

            def A1mm(hb):
                for cc in range(2):
                    c = hb * 2 + cc
                    pb = ps[2 + cc]
                    for kc in range(8):
                        mm(pb[:], wx[:, kc, c * 128:(c + 1) * 128], hT[:, kc, :], start=(kc == 0), stop=(kc == 7))

            def A1(hb):
                X, XC, XB = xb2[hb % 2], xc2[hb % 2], xcb[hb % 2]
                for cc in range(2):
                    c = hb * 2 + cc
                    act(X[:, cc, 0:3], halo[:, c, 0:3], AF.Copy)
                for cc in range(2):
                    act(X[:, cc, 3:515], ps[2 + cc][:], AF.Copy)
                for cc in range(2):
                    c = hb * 2 + cc
                    cp(halo[:, c, 0:3], X[:, cc, 512:515], eng="dve")
                    ts(XC[:, cc, :], X[:, cc, 0:512], vcol(V_CW + 0 * 8 + c), vcol(V_CB + c), ALU.mult, ALU.add)
                    for j in range(1, 4):
                        stt(XC[:, cc, :], X[:, cc, j:j + 512], vcol(V_CW + j * 8 + c), XC[:, cc, :],
                            ALU.mult, ALU.add)

            def A2(hb):
                X, XC, XB = xb2[hb % 2], xc2[hb % 2], xcb[hb % 2]
                for cc in range(2):
                    act(XB[:, cc, :], XC[:, cc, :], AF.Copy)
                for oc in range(2):
                    for kk in range(2):
                        mm(ps[4 + 2 * oc][:], wr[:, hb * 2 + kk, oc * 128:(oc + 1) * 128], XB[:, kk, :],
                           start=(kk == 0), stop=(kk == 1))
                    for kk in range(2):
                        mm(ps[5 + 2 * oc][:], wi[:, hb * 2 + kk, oc * 128:(oc + 1) * 128], XB[:, kk, :],
                           start=(kk == 0), stop=(kk == 1))

            def B1(hb):
                t_r, t_i, a_, m_ = TR[hb % 2], TI[hb % 2], AA[hb % 2], MM[hb % 2]
                for cc in range(2):
                    act(yT[:, hb * 2 + cc, :], ps[cc][:], AF.Gelu_apprx_tanh)
                for oc in range(2):
                    c = hb * 2 + oc
                    act(t_r[:, oc, :], ps[4 + 2 * oc][:], AF.Tanh, bias=hbr[:, c:c + 1], scale=0.5)
                    act(t_i[:, oc, :], ps[5 + 2 * oc][:], AF.Tanh, bias=hbi[:, c:c + 1], scale=0.5)
                for oc in range(2):
                    c = hb * 2 + oc
                    act(a_[:, oc, :], t_r[:, oc, :], AF.Exp, bias=cnh[:, c:c + 1], scale=cnh[:, c:c + 1])
                    act(m_[:, oc, :], t_r[:, oc, :], AF.Exp, bias=cneg[:, c:c + 1], scale=cneg[:, c:c + 1])
                for oc in range(2):
                    act(m_[:, oc, :], m_[:, oc, :], AF.Sqrt, bias=0.25, scale=-0.25)

            def B2(hb):
                t_r, t_i, a_, m_ = TR[hb % 2], TI[hb % 2], AA[hb % 2], MM[hb % 2]
                hs = t_r
                XC = xc2[hb % 2]
                for oc in range(2):
                    c = hb * 2 + oc
                    stt(t_i[:, oc, :], t_i[:, oc, :], 1.0, XC[:, oc, :], ALU.add, ALU.mult)
                    tt(m_[:, oc, :], m_[:, oc, :], t_i[:, oc, :], ALU.mult)
                    scan(hs[:, oc, :], a_[:, oc, :], m_[:, oc, :], carry[:, c:c + 1], ALU.mult, ALU.add)
                    cp(carry[:, c:c + 1], hs[:, oc, 511:512], eng="dve")
                    tt(yT[:, c, :], hs[:, oc, :], yT[:, c, :], ALU.mult)

            Gmm(0); A1mm(0); A1(0); A1mm(1); A2(0)
            for hb in range(4):
                if hb + 1 < 4:
                    A1(hb + 1)
                if hb + 2 < 4:
                    A1mm(hb + 2)
                B1(hb)
                if hb + 1 < 4:
                    Gmm(hb + 1)
                    A2(hb + 1)
                elif jb == NB - 1 and after_gate is not None:
                    after_gate()
                B2(hb)
            if jb + 1 < NB:
                norm_a0(jb + 1)
            for m in range(8):
                if m == 4 and jb + 1 < NB:
                    norm_a(jb + 1, b, l, 0, do_sq=False)
                pb = ps[m % 2]
                for kc in range(8):
                    mm(pb[:], wout[:, kc, m * 128:(m + 1) * 128], yT[:, kc, :], start=(kc == 0), stop=(kc == 7))
                resid_update(pb[:], m, jb, ada_col(l, 2, m, b))

    def moe_wviews(slot):
        base = slot * 24576
        return (b16v(W, base, [128, 8, 512]), b16v(W, base + 8192, [128, 8, 512]),
                b16v(W, base + 16384, [128, 4, 1024]))

    moe_state = {"slot": 0}

    def moe_load(l, e, slot, parts=("g", "u", "d")):
        wg, wu, wd = moe_wviews(slot)
        if "g" in parts:
            dma("pool", wg, wg_d[l, e].rearrange("(c p) n -> p c n", p=128))
        if "u" in parts:
            dma("pool", wu, wu_d[l, e].rearrange("(c p) n -> p c n", p=128))
        if "d" in parts:
            dma("pool", wd, wd_d[l, e].rearrange("(c p) n -> p c n", p=128))

    SLOTS = [1, 2, 0, 1, 2, 0, 1, 2, 0, 1, 2, 0, 1, 2, 1, 0]

    def moe_layer(b, l, pre_last=None):
        hT = b16v(hbuf, 0, [128, 8, S])
        for jb in range(NB):
            norm_block(jb, b, l, 1, 3, hT[:, :, jb * TB:(jb + 1) * TB])
        for t in range(16):
            for kc in range(8):
                mm(ps[6][:, t * 16:(t + 1) * 16], hT[:, kc, t * 128:(t + 1) * 128], rw[:, kc, :],
                   start=(kc == 0), stop=(kc == 7))
        R = lambda i: rt[:, i * 256:(i + 1) * 256]
        R3 = lambda i: rt[:, i * 256:(i + 1) * 256].rearrange("p (t e) -> p t e", e=16)
        R4 = lambda i: rt[:, i * 256:(i + 1) * 256].rearrange("p (t g j) -> p t g j", g=4, j=4)
        G = lambda i: rt[:, 2304 + i * 64:2304 + (i + 1) * 64]
        G3 = lambda i: rt[:, 2304 + i * 64:2304 + (i + 1) * 64].rearrange("p (t g) -> p t g", g=4)
        sc_, sel_, msk, tmp_ = 0, 1, 2, 3
        act(R(sc_), ps[6][:, 0:256], AF.Sigmoid)
        tt(R3(sel_), R3(sc_), rb[:, :].unsqueeze(1).to_broadcast([128, 16, 16]), ALU.add)
        red(G(0), R4(sel_), ALU.max)
        tt(R4(msk), R4(sel_), G3(0).unsqueeze(3).to_broadcast([128, 16, 4, 4]), ALU.is_ge)
        stt(R(tmp_), R(msk), -1e30, R(sel_), ALU.mult, ALU.add)
        red(G(1), R4(tmp_), ALU.max)
        tt(G(2), G(0), G(1), ALU.add)
        red(rt[:, 1664:1680], G3(2), ALU.max)
        tt(G3(3), G3(2), rt[:, 1664:1680].unsqueeze(2).to_broadcast([128, 16, 4]), ALU.is_ge)
        tt(R4(msk), R4(sel_), G3(1).unsqueeze(3).to_broadcast([128, 16, 4, 4]), ALU.is_ge)
        tt(R4(msk), R4(msk), G3(3).unsqueeze(3).to_broadcast([128, 16, 4, 4]), ALU.mult)
        tt(R(tmp_), R(msk), R(sc_), ALU.mult)
        red(rt[:, 1680:1696], R3(tmp_), ALU.add)
        recip(rt[:, 1680:1696], rt[:, 1680:1696])
        tt(R3(tmp_), R3(tmp_), rt[:, 1680:1696].unsqueeze(2).to_broadcast([128, 16, 16]), ALU.mult)
        pk = rt[:, 1024:1536].rearrange("p (t e) -> p t e", e=32)
        hib = rt[:, 1536:1664].bitcast(BF16).rearrange("p (t e) -> p t e", e=16)
        cp(hib, R3(tmp_), eng="dve")
        cp(pk[:, :, 0:16], hib, eng="dve")
        tt(pk[:, :, 16:32], R3(tmp_), pk[:, :, 0:16], ALU.subtract)
        combT = cTr[0:32, 0:1024].bitcast(BF16)
        for t4 in range(4):
            for q in range(4):
                t = t4 * 4 + q
                tr(ps[7][0:32, q * 128:(q + 1) * 128], pk[:, t, :], ident[:])
            act(combT[:, t4 * 512:(t4 + 1) * 512], ps[7][0:32, :], AF.Copy)
        if "comb" in dbg_d and b == dbg.get("_b", 0) and l == dbg.get("_l", 0):
            dma("sp", dbg_d["comb"], combT)
        he = [b16v(scr, 0, [128, 4, 512]), b16v(scr, 4096, [128, 4, 512])]
        sg = [f32v(scr, 8192, [128, 512]), f32v(scr, 10240, [128, 512])]
        tb = [f32v(scr, 12288, [128, 512]), f32v(scr, 14336, [128, 512])]
        cbs = [f32v(scr, 16384, [128, 512]), f32v(scr, 18432, [128, 512])]
        it = 0
        for e in range(NE):
            slot = SLOTS[e]
            if e + 1 < NE:
                moe_load(l, e + 1, SLOTS[e + 1])
            elif pre_last is not None:
                pre_last()
            wg, wu, wd = moe_wviews(slot)
            for jb in range(NB):
                t0 = jb * TB
                k2 = it % 2
                it += 1
                mm(ps[6][:], i32b[:, e:e + 1].to_broadcast([32, 128]), combT[:, t0:t0 + TB])
                act(cbs[k2], ps[6][:], AF.Copy)
                for c in range(4):
                    pg = ps[0 + c % 2]
                    pu = ps[2 + c % 2]
                    for kc in range(8):
                        mm(pg[:], wg[:, kc, c * 128:(c + 1) * 128], hT[:, kc, t0:t0 + TB],
                           start=(kc == 0), stop=(kc == 7))
                    for kc in range(8):
                        mm(pu[:], wu[:, kc, c * 128:(c + 1) * 128], hT[:, kc, t0:t0 + TB],
                           start=(kc == 0), stop=(kc == 7))
                    act(sg[c % 2], pg[:], AF.Silu)
                    tt(tb[c % 2], pu[:], sg[c % 2], ALU.mult)
                    tt(he[k2][:, c, :], tb[c % 2], cbs[k2], ALU.mult)
                for m in range(8):
                    pd = ps[4 + m % 2]
                    for kc in range(4):
                        mm(pd[:], wd[:, kc, m * 128:(m + 1) * 128], he[k2][:, kc, :],
                           start=(kc == 0), stop=(kc == 3))
                    resid_update(pd[:], m, jb, ada_col(l, 5, m, b))

    def final_store(b, si):
        sq = b16v(scr, 0, [128, 8, 512])
        rstd = f32v(scr, 8192, [128, 512])
        ob = [f32v(hbuf, 0, [128, 8, 512]), f32v(hbuf, 16384, [128, 8, 512])]
        outs = []
        for jb in range(NB):
            t0 = jb * TB
            act(sq[:, :, :], xT[:, :, t0:t0 + TB], AF.Square)
            for c in range(8):
                mm(ps[6][:], ones_d[:], sq[:, c, :], start=(c == 0), stop=(c == 7))
            act(rstd, ps[6][:], AF.Ln, bias=EPS)
            act(rstd, rstd, AF.Exp, scale=-0.5)
            o = ob[jb % 2]
            for c in range(8):
                stt(o[:, c, :], xT[:, c, t0:t0 + TB], vcol(V_FNG + c), rstd, ALU.mult, ALU.mult)
            outs.append(dma("sp", outT_d[si, :, t0:t0 + TB].rearrange("(c p) t -> p c t", p=128), o))
        return outs

    out_ops = []
    phases = dbg.get("_phases", ("gla", "moe0", "lru", "moe1", "final"))
    for si in range(n_seq):
        b = si
        for jb in range(NB):
            dma("sp", xT[:, :, jb * TB:(jb + 1) * TB],
                xT_d[si, :, jb * TB:(jb + 1) * TB].rearrange("(c p) t -> p c t", p=128))
        if si == 0 and "gla" in phases:
            gla_prefetch()
        if "gla" in phases:
            gla_layer(b, after_proj=(lambda: moe_load(0, 0, SLOTS[0])) if "moe0" in phases else None)
        if "xmid0" in dbg_d and si == dbg.get("_b", 0):
            out_ops.append(dma("sp", dbg_d["xmid0"].rearrange("(c p) t -> p c t", p=128), xT[:, :, :]))
        if "moe0" in phases:
            if "gla" not in phases:
                moe_load(0, 0, SLOTS[0])
            moe_layer(b, 0, pre_last=lru_prefetch if "lru" in phases else None)
        if "xmid1" in dbg_d and si == dbg.get("_b", 0):
            out_ops.append(dma("sp", dbg_d["xmid1"].rearrange("(c p) t -> p c t", p=128), xT[:, :, :]))
        if "lru" in phases:
            if "moe0" not in phases:
                lru_prefetch()
            lru_layer(b, after_gate=(lambda: moe_load(1, 0, SLOTS[0], ("g", "u"))) if "moe1" in phases else None)
            if "moe1" in phases:
                moe_load(1, 0, SLOTS[0], ("d",))
        if "xmid2" in dbg_d and si == dbg.get("_b", 0):
            out_ops.append(dma("sp", dbg_d["xmid2"].rearrange("(c p) t -> p c t", p=128), xT[:, :, :]))
        if "moe1" in phases:
            if "lru" not in phases:
                moe_load(1, 0, SLOTS[0])
            moe_layer(b, 1, pre_last=gla_prefetch if (si + 1 < n_seq and "gla" in phases) else None)
        if "final" in phases:
            out_ops += final_store(b, si)

    Sd.add("sp", lambda e: e.nop(), reads=[], writes=[]).deps.extend(
        [o for o in Sd.dma_ops["sp"]])

    Sd.finalize()

    sem_stack = contextlib.ExitStack()
    csem = {e: sem_stack.enter_context(nc.semaphore("c_" + e)) for e in Sched.ENGS}
    dsems = {e: [sem_stack.enter_context(nc.semaphore("d_%s_%d" % (e, i))) for i in range(Sched.NDS)]
             for e in ("sp", "pool")}
    with nc.Block() as block:
        @block.tensor
        def _(e):
            Sd.emit("pe", e, csem, dsems)

        @block.scalar
        def _(e):
            Sd.emit("act", e, csem, dsems)

        @block.vector
        def _(e):
            Sd.emit("dve", e, csem, dsems)

        @block.gpsimd
        def _(e):
            Sd.emit("pool", e, csem, dsems)

        @block.sync
        def _(e):
            Sd.emit("sp", e, csem, dsems)
    sem_stack.close()
    stack.close()
    return nc, Sd


V_NMG = 0
V_NFG = 16
V_FNG = 32
V_GNG = 40
V_CW = 42
V_CB = 74
V_BR = 82
V_BI = 90
V_LAM = 98
NV = 106


def _fm(v):
    v = np.asarray(v, np.float32).reshape(-1, 128)
    return np.ascontiguousarray(v.T)


def prep_inputs(inp):
    f = lambda a: np.ascontiguousarray(np.asarray(a, dtype=np.float32))
    x = f(inp["x"])
    c = f(inp["c"])
    vec = np.zeros((128, NV), np.float32)
    for l in range(2):
        vec[:, V_NMG + l * 8:V_NMG + (l + 1) * 8] = _fm(inp["norm_mix_g"][l])
        vec[:, V_NFG + l * 8:V_NFG + (l + 1) * 8] = _fm(inp["norm_ffn_g"][l])
    vec[:, V_FNG:V_FNG + 8] = _fm(inp["final_norm_g"])
    vec[:, V_GNG:V_GNG + 2] = _fm(inp["gla_norm_g"][0])
    for j in range(4):
        vec[:, V_CW + j * 8:V_CW + (j + 1) * 8] = _fm(inp["lru_conv_w"][0, j])
    vec[:, V_CB:V_CB + 8] = _fm(inp["lru_conv_b"][0])
    vec[:, V_BR:V_BR + 8] = _fm(np.asarray(inp["lru_b_r"][0]).reshape(-1))
    vec[:, V_BI:V_BI + 8] = _fm(np.asarray(inp["lru_b_i"][0]).reshape(-1))
    vec[:, V_LAM:V_LAM + 8] = _fm(inp["lru_lambda"][0])
    ada_b = np.concatenate([_fm(inp["ada_b"][0]), _fm(inp["ada_b"][1])], axis=1)
    wgu = np.concatenate([f(inp["gla_w_gate_up"][0]), f(inp["gla_b_gate"][0])[None, :]], axis=0)

    def blk(w):
        w = f(w).reshape(4, 2, 128, 256).transpose(2, 0, 1, 3)
        return np.ascontiguousarray(w).reshape(128, 8 * 256)

    rwl = f(inp["router_w"]).reshape(8, 128, 16).transpose(1, 0, 2).reshape(128, 128)
    shared = {
        "ada_w": f(inp["ada_w"]), "ada_b": ada_b, "vecs": vec,
        "gla_w_in": f(inp["gla_w_in"][0]), "gla_w_out": f(inp["gla_w_out"][0]), "wgu": wgu,
        "lru_w_in": f(inp["lru_w_in"][0]), "lru_w_out": f(inp["lru_w_out"][0]),
        "lru_wr": blk(inp["lru_w_r"][0]), "lru_wi": blk(inp["lru_w_i"][0]),
        "rw": np.ascontiguousarray(rwl), "rb": np.ascontiguousarray(np.tile(f(inp["router_bias"])[None, :], (128, 1))),
        "moe_wg": f(inp["moe_w_gate"]), "moe_wu": f(inp["moe_w_up"]), "moe_wd": f(inp["moe_w_down"]),
        "ident": np.eye(128, dtype=np.float32),
    }
    s_i = np.arange(128)[:, None]
    c_i = np.arange(128)[None, :]
    shared["mtri"] = (((s_i > c_i) & (s_i // 64 == c_i // 64)).astype(np.float32) / 16.0)
    shared["ind"] = ((s_i // 64) == np.arange(2)[None, :]).astype(np.float32) / 16.0
    shared["i16"] = np.eye(16, dtype=np.float32)
    in_maps = []
    for i in range(N_CORES):
        m = dict(shared)
        m["xT"] = np.ascontiguousarray(x[2 * i:2 * i + 2].transpose(0, 2, 1))
        cc = c[2 * i:2 * i + 2]
        m["cT"] = np.ascontiguousarray(cc.reshape(2, 8, 128).transpose(2, 1, 0).reshape(128, 16))
        in_maps.append(m)
    return in_maps


_CACHE = {}


def kernel(**inputs):
    in_maps = prep_inputs(inputs)
    if "nc" not in _CACHE:
        _CACHE["nc"] = build_program()[0]
    nc = _CACHE["nc"]
    res = run_bass_kernel_spmd(nc, in_maps, core_ids=list(range(N_CORES)))
    out = np.empty((16, S, D), np.float32)
    for i in range(N_CORES):
        o = res.results[i]["outT"]
        out[2 * i:2 * i + 2] = o.transpose(0, 2, 1)
    return out
```

```python
import numpy as np
import concourse.bass as bass
import concourse.mybir as mybir
from concourse.bass_utils import run_bass_kernel_spmd

F32 = mybir.dt.float32
BF16 = mybir.dt.bfloat16
AF = mybir.ActivationFunctionType
ALU = mybir.AluOpType
ESZ = {F32: 4, BF16: 2}

D = 1024
S = 2048
NB = 4
TB = 512
NE = 16
DE = 512
EPS = 1e-6
GRAN = 512
N_CORES = 8


class Op:
    __slots__ = ("eng", "idx", "fn", "deps", "dma", "dsem", "dval", "sig", "sigval", "waits", "gidx")


class Sched:
    ENGS = ("pe", "act", "dve", "pool", "sp")
    NDS = 12

    def __init__(self, nc):
        self.nc = nc
        self.ops = {e: [] for e in self.ENGS}
        self.last_w = {}
        self.readers = {}
        self.ndma = {e: 0 for e in self.ENGS}
        self.dma_ops = {e: [] for e in self.ENGS}
        self.tok_cache = {}
        self.gcount = 0

    def tokens(self, ap):
        sp = str(ap.space)
        if "SB" not in sp and "PSUM" not in sp:
            return ()
        if "PSUM" in sp:
            return ((ap.tensor.name, 0),)
        key = (ap.tensor.name, ap.offset, ap.ap, ap.dtype)
        t = self.tok_cache.get(key)
        if t is None:
            es = ESZ[ap.dtype]
            pstride = ap.ap[0][0]
            off = ap.offset % pstride if pstride > 0 else ap.offset
            name = ap.tensor.name
            dims = [(abs(st), cnt) for st, cnt in ap.ap[1:] if cnt > 1 and st != 0]
            dims.sort(reverse=True)
            outer = dims[:-1] if dims else []
            n_outer = 1
            for _, cnt in outer:
                n_outer *= cnt
            gs = set()
            if dims and n_outer <= 512:
                lst, lcnt = dims[-1]
                bases = [off]
                for st, cnt in outer:
                    bases = [b0 + i * st for b0 in bases for i in range(cnt)]
                for b0 in bases:
                    lo = b0 * es
                    hi = (b0 + (lcnt - 1) * lst + 1) * es
                    gs.update(range(lo // GRAN, (hi - 1) // GRAN + 1))
            else:
                span = 0
                for st, cnt in dims:
                    span += (cnt - 1) * st
                lo = off * es
                hi = (off + span + 1) * es
                gs.update(range(lo // GRAN, (hi - 1) // GRAN + 1))
            t = tuple((name, g) for g in sorted(gs))
            self.tok_cache[key] = t
        return t

    def add(self, eng, fn, reads=(), writes=(), dma=False):
        op = Op()
        op.eng = eng
        op.idx = len(self.ops[eng])
        op.fn = fn
        op.dma = dma
        op.sig = False
        op.sigval = 0
        op.gidx = self.gcount
        self.gcount += 1
        deps = {}
        rt = []
        for ap in reads:
            rt.extend(self.tokens(ap))
        wt = []
        for ap in writes:
            wt.extend(self.tokens(ap))
        for r in rt:
            w = self.last_w.get(r)
            if w is not None:
                deps[id(w)] = w
        for r in wt:
            w = self.last_w.get(r)
            if w is not None:
                deps[id(w)] = w
            rd = self.readers.get(r)
            if rd:
                for o in rd.values():
                    deps[id(o)] = o
        deps.pop(id(op), None)
        op.deps = list(deps.values())
        for r in rt:
            d = self.readers.get(r)
            if d is None:
                d = {}
                self.readers[r] = d
            if dma:
                d[(eng, op.idx)] = op
            else:
                d[eng] = op
        for r in wt:
            self.last_w[r] = op
            self.readers[r] = {}
        if dma:
            i = self.ndma[eng]
            self.ndma[eng] = i + 1
            op.dsem = i % self.NDS
            op.dval = 16 * (i // self.NDS + 1)
            if i >= self.NDS:
                op.deps.append(self.dma_ops[eng][i - self.NDS])
            self.dma_ops[eng].append(op)
        self.ops[eng].append(op)
        return op

    def finalize(self):
        for eng in self.ENGS:
            known = {}
            for op in self.ops[eng]:
                need = {}
                for d in op.deps:
                    if d.dma:
                        k = ("d", d.eng, d.dsem)
                        v = d.dval
                        if v > need.get(k, (0, None))[0]:
                            need[k] = (v, d)
                    else:
                        if d.eng == "pe" and eng == "pe" and not op.dma:
                            continue
                        k = ("c", d.eng)
                        v = d.idx + 1
                        if v > need.get(k, (0, None))[0]:
                            need[k] = (v, d)
                waits = []
                for k, (v, d) in need.items():
                    if known.get(k, 0) >= v:
                        continue
                    known[k] = v
                    waits.append(d)
                    if not d.dma:
                        d.sig = True
                op.waits = waits
        for eng in self.ENGS:
            c = 0
            for op in self.ops[eng]:
                if op.sig and not op.dma:
                    c += 1
                    op.sigval = c

    def emit(self, eng, e, csem, dsems):
        for op in self.ops[eng]:
            for d in op.waits:
                if d.dma:
                    e.wait_ge(dsems[d.eng][d.dsem], d.dval)
                else:
                    e.wait_ge(csem[d.eng], d.sigval)
            ins = op.fn(e)
            if op.dma:
                ins.then_inc(dsems[eng][op.dsem], 16)
            elif op.sig:
                ins.then_inc(csem[eng], 1)


def build_program(n_seq=2, dbg=None):
    dbg = dbg or {}
    nc = bass.Bass("TRN2", target_bir_lowering=False)
    Sd = Sched(nc)

    def din(name, shape):
        return nc.dram_tensor(name, list(shape), F32, kind="ExternalInput").ap()

    xT_d = din("xT", [2, D, S])
    cT_d = din("cT", [128, 16])
    ada_w_d = din("ada_w", [2, D, 6 * D])
    ada_b_d = din("ada_b", [128, 96])
    vecs_d = din("vecs", [128, NV])
    gla_w_in_d = din("gla_w_in", [D, 3088])
    gla_w_out_d = din("gla_w_out", [D, D])
    wgu_d = din("wgu", [17, 512])
    lru_w_in_d = din("lru_w_in", [D, 2 * D])
    lru_w_out_d = din("lru_w_out", [D, D])
    lru_wr_d = din("lru_wr", [128, 8 * 256])
    lru_wi_d = din("lru_wi", [128, 8 * 256])
    rw_d = din("rw", [128, 8 * 16])
    rb_d = din("rb", [128, 16])
    wg_d = din("moe_wg", [2, NE, D, DE])
    wu_d = din("moe_wu", [2, NE, D, DE])
    wd_d = din("moe_wd", [2, NE, DE, D])
    ident_d = din("ident", [128, 128])
    mtri_d = din("mtri", [128, 128])
    ind_d = din("ind", [128, 2])
    i16_d = din("i16", [16, 16])
    outT_d = nc.dram_tensor("outT", [2, D, S], F32, kind="ExternalOutput").ap()
    dbg_d = {}
    for k, shp in dbg.items():
        if k.startswith("_"):
            continue
        dbg_d[k] = nc.dram_tensor("dbg_" + k, list(shp), F32, kind="ExternalOutput").ap()

    import contextlib
    stack = contextlib.ExitStack()

    def SB(name, shape, dt=F32):
        return stack.enter_context(nc.sbuf_tensor("s_" + name, list(shape), dt))

    def PS(name):
        return stack.enter_context(nc.psum_tensor(name, [128, 512], F32))

    xT = SB("xT", [128, 8, S])
    hbuf = SB("hbuf", [128, 16384], BF16)
    W = SB("W", [128, 36864], BF16)
    scr = SB("scr", [128, 10240], BF16)
    cTr = SB("cTr", [128, 2048])
    ident = SB("ident", [128, 128])
    mtri = SB("mtri", [128, 128])
    ind = SB("ind", [128, 2])
    i32f = SB("i32f", [32, 16])
    i32b = SB("i32b", [32, 16], BF16)
    ones_d = SB("ones_d", [128, 128], BF16)
    ones_v = SB("ones_v", [128, 128], BF16)
    vecs = SB("vecs", [128, NV])
    cT_s = SB("cT_s", [128, 16])
    cond = SB("cond", [128, 16], BF16)
    ada_b = SB("ada_b", [128, 96])
    ada = SB("ada", [128, 2, 48, 2])
    mod = SB("mod", [128, 2, 4, 8, 2])
    rw = SB("rw", [128, 8, 16], BF16)
    rb = SB("rb", [128, 16])
    wgu = SB("wgu", [32, 512])
    smallw = SB("smallw", [128, 256])
    halo = SB("halo", [128, 8, 4])
    carry = SB("carry", [128, 8])
    rt = scr[:, 0:5120].bitcast(F32)

    ps = [PS("ps%d" % i) for i in range(8)]

    def mm(out, lhsT, rhs, start=True, stop=True):
        return Sd.add("pe", lambda e: e.matmul(out, lhsT, rhs, start=start, stop=stop),
                      reads=[lhsT, rhs], writes=[out])

    def tr(out, in_, idn):
        return Sd.add("pe", lambda e: e.transpose(out, in_, idn), reads=[in_, idn], writes=[out])

    def act(out, in_, func, bias=None, scale=None, eng="act"):
        rd = [in_]
        kw = {}
        if bias is not None:
            kw["bias"] = bias
            if not isinstance(bias, (int, float)):
                rd.append(bias)
        if scale is not None:
            kw["scale"] = scale
            if not isinstance(scale, (int, float)):
                rd.append(scale)
        return Sd.add("act", lambda e: e.activation(out, in_, func, **kw), reads=rd, writes=[out])

    def stt(out, in0, scalar, in1, op0, op1, eng="dve"):
        rd = [in0, in1]
        if not isinstance(scalar, (int, float)):
            rd.append(scalar)
        return Sd.add(eng, lambda e: e.scalar_tensor_tensor(out, in0, scalar, in1, op0, op1),
                      reads=rd, writes=[out])

    def ts(out, in0, s1, s2, op0, op1=None, eng="dve"):
        rd = [in0]
        for s in (s1, s2):
            if s is not None and not isinstance(s, (int, float)):
                rd.append(s)
        if op1 is None:
            return Sd.add(eng, lambda e: e.tensor_scalar(out, in0, s1, None, op0), reads=rd, writes=[out])
        return Sd.add(eng, lambda e: e.tensor_scalar(out, in0, s1, s2, op0, op1), reads=rd, writes=[out])

    def tt(out, in0, in1, op, eng="dve"):
        return Sd.add(eng, lambda e: e.tensor_tensor(out, in0, in1, op), reads=[in0, in1], writes=[out])

    def cp(out, in_, eng="dve"):
        return Sd.add(eng, lambda e: e.tensor_copy(out, in_), reads=[in_], writes=[out])

    def recip(out, in_):
        return Sd.add("dve", lambda e: e.reciprocal(out, in_), reads=[in_], writes=[out])

    def red(out, in_, op, eng="dve"):
        return Sd.add(eng, lambda e: e.tensor_reduce(out, in_, mybir.AxisListType.X, op),
                      reads=[in_], writes=[out])

    def scan(out, d0, d1, init, op0, op1):
        rd = [d0, d1]
        if not isinstance(init, (int, float)):
            rd.append(init)
        return Sd.add("dve", lambda e: e.tensor_tensor_scan(out, d0, d1, init, op0, op1),
                      reads=rd, writes=[out])

    def memset(ap, val, eng="dve"):
        return Sd.add(eng, lambda e: e.memset(ap, val), writes=[ap])

    def dma(q, out, in_, **kw):
        return Sd.add(q, lambda e: e.dma_start(out=out, in_=in_, **kw), reads=[in_], writes=[out], dma=True)

    def f32v(t, off_b, shape):
        n = int(np.prod(shape[1:]))
        v = t[0:shape[0], off_b // 2: off_b // 2 + 2 * n].bitcast(F32)
        if len(shape) == 3:
            v = v.rearrange("p (a b) -> p a b", a=shape[1])
        elif len(shape) == 4:
            v = v.rearrange("p (a b c) -> p a b c", a=shape[1], b=shape[2])
        return v

    def b16v(t, off_b, shape):
        n = int(np.prod(shape[1:]))
        v = t[0:shape[0], off_b // 2: off_b // 2 + n]
        if len(shape) == 3:
            v = v.rearrange("p (a b) -> p a b", a=shape[1])
        elif len(shape) == 4:
            v = v.rearrange("p (a b c) -> p a b c", a=shape[1], b=shape[2])
        return v

    def vcol(i):
        return vecs[:, i:i + 1]

    dma("sp", ident[:], ident_d)
    dma("sp", mtri[:], mtri_d)
    dma("sp", ind[:], ind_d)
    dma("sp", i32f[0:16, :], i16_d)
    dma("sp", i32f[16:32, :], i16_d)
    cp(i32b[:], i32f[:], eng="dve")
    dma("sp", vecs[:], vecs_d)
    dma("sp", cT_s[:], cT_d)
    dma("sp", ada_b[:], ada_b_d)
    dma("sp", rb[:], rb_d)
    dma("sp", wgu[0:17, :], wgu_d)
    dma("pool", rw[:].rearrange("p a b -> p (a b)"), rw_d)
    memset(ones_d[:], 1.0 / 1024.0)
    memset(ones_v[:], 1.0 / 256.0)
    act(cond[:], cT_s[:], AF.Silu)

    NPIECE = 12
    for l in range(2):
        for pc in range(NPIECE):
            slot = (l * NPIECE + pc) % 3
            wv = b16v(hbuf, slot * 8192, [128, 8, 512])
            dma("pool", wv, ada_w_d[l, :, pc * 512:(pc + 1) * 512].rearrange("(c p) n -> p c n", p=128))
            for oc4 in range(4):
                oc = pc * 4 + oc4
                for kc in range(8):
                    mm(ps[7][:, (l * 48 + oc) * 2:(l * 48 + oc) * 2 + 2],
                       wv[:, kc, oc4 * 128:(oc4 + 1) * 128],
                       cond[:, kc * 2:kc * 2 + 2], start=(kc == 0), stop=(kc == 7))
    for l in range(2):
        tt(ada[:, l, :, :], ps[7][:, l * 96:(l + 1) * 96].rearrange("p (a b) -> p a b", b=2),
           ada_b[:, l * 48:(l + 1) * 48].unsqueeze(2).to_broadcast([128, 48, 2]), ALU.add)
    for l in range(2):
        for j, (which, gcol) in enumerate(((1, V_NMG + l * 8), (4, V_NFG + l * 8))):
            ts(mod[:, l, j, :, :], ada[:, l, which * 8:(which + 1) * 8, :], 1.0, None, ALU.add)
            tt(mod[:, l, j, :, :], mod[:, l, j, :, :],
               vecs[:, gcol:gcol + 8].unsqueeze(2).to_broadcast([128, 8, 2]), ALU.mult)

    def A_of(l, j, c, b):
        return mod[:, l, j, c, b:b + 1]

    def ada_col(l, which, c, b):
        return ada[:, l, which * 8 + c, b:b + 1]

    lam = vecs[:, V_LAM:V_LAM + 8]
    sw_a = smallw[:, 0:8]
    sw_b = smallw[:, 8:16]
    cneg = smallw[:, 16:24]
    cneg2 = smallw[:, 24:32]
    ts(sw_b, lam, 0.0, None, ALU.min)
    stt(sw_a, sw_b, 2.0, lam, ALU.mult, ALU.subtract)
    act(sw_a, sw_a, AF.Exp)
    act(sw_a, sw_a, AF.Ln, bias=1.0)
    tt(sw_a, sw_a, sw_b, ALU.subtract)
    ts(cneg, sw_a, -8.0, None, ALU.mult)
    ts(cneg2, sw_a, -16.0, None, ALU.mult)

    def norm_a0(jb):
        t0 = jb * TB
        sq = b16v(scr, 0, [128, 8, 512])
        act(sq[:, :, :], xT[:, :, t0:t0 + TB], AF.Square)

    def norm_a(jb, b, l, j, npre=2, do_sq=True):
        t0 = jb * TB
        sq = b16v(scr, 0, [128, 8, 512])
        rstd = f32v(scr, 8192, [128, 512])
        tmp = [f32v(scr, 10240, [128, 512]), f32v(scr, 12288, [128, 512])]
        if do_sq:
            norm_a0(jb)
        for c in range(8):
            mm(ps[6][:], ones_d[:], sq[:, c, :], start=(c == 0), stop=(c == 7))
        act(rstd, ps[6][:], AF.Ln, bias=EPS)
        act(rstd, rstd, AF.Exp, scale=-0.5)
        for c in range(npre):
            stt(tmp[c % 2], xT[:, c, t0:t0 + TB], A_of(l, j, c, b), rstd, ALU.mult, ALU.mult)

    def norm_b(jb, b, l, j, shift_which, dst, npre=2):
        t0 = jb * TB
        rstd = f32v(scr, 8192, [128, 512])
        tmp = [f32v(scr, 10240, [128, 512]), f32v(scr, 12288, [128, 512])]
        for c in range(8):
            tm = tmp[c % 2]
            if c >= npre:
                stt(tm, xT[:, c, t0:t0 + TB], A_of(l, j, c, b), rstd, ALU.mult, ALU.mult)
            act(dst[:, c, :], tm, AF.Identity, bias=ada_col(l, shift_which, c, b))

    def norm_block(jb, b, l, j, shift_which, dst):
        norm_a(jb, b, l, j, npre=0)
        norm_b(jb, b, l, j, shift_which, dst, npre=0)

    def resid_update(pbank, m, jb, gcol):
        t0 = jb * TB
        stt(xT[:, m, t0:t0 + TB], pbank, gcol, xT[:, m, t0:t0 + TB], ALU.mult, ALU.add)

    wa = SB("wa", [128, 8, 128], BF16)
    memset(wa[:], 0.0)

    def gla_views():
        return dict(
            wout=b16v(W, 0, [128, 8, 1024]),
            Sst=f32v(W, 16384, [128, 4, 256]),
            Sb=b16v(W, 20480, [128, 4, 256]),
            gam=f32v(W, 22528, [128, 4, 32]),
            wq=b16v(W, 24576, [128, 8, 512]),
            wk=b16v(W, 32768, [128, 8, 512]),
            wv=b16v(W, 40960, [128, 8, 1024]),
            wg=b16v(W, 57344, [128, 8, 1024]),
        )

    def gla_prefetch():
        v = gla_views()
        src = gla_w_in_d.rearrange("(c p) n -> p c n", p=128)
        dma("pool", wa[:, :, 0:16], src[:, :, 3072:3088])
        dma("pool", v["wq"], src[:, :, 0:512])
        dma("pool", v["wg"], src[:, :, 2048:3072])
        dma("pool", v["wk"], src[:, :, 512:1024])
        dma("pool", v["wv"], src[:, :, 1024:2048])

    def gla_layer(b, l=0, after_proj=None):
        V = gla_views()
        wout, Sst, Sb, gam, wq, wk, wv, wg = (V[k] for k in ("wout", "Sst", "Sb", "gam", "wq", "wk", "wv", "wg"))
        dma("pool", wout, gla_w_out_d.rearrange("(c p) n -> p c n", p=128))
        memset(Sst, 0.0)
        hT = b16v(hbuf, 0, [128, 8, 512])
        qT = b16v(hbuf, 8192, [128, 4, 512])
        kdec = b16v(hbuf, 12288, [128, 4, 512])
        vv = b16v(hbuf, 16384, [128, 4, 1024])
        sgT = b16v(hbuf, 24576, [128, 8, 512])
        oT = f32v(scr, 0, [128, 8, 512])
        m4 = f32v(scr, 0, [128, 4, 512])
        u4 = f32v(scr, 8192, [128, 4, 512])
        alrT = f32v(scr, 16384, [32, 512])
        expD = f32v(scr, 18432, [128, 512])
        la = cTr[:, :].rearrange("p (a b) -> p a b", a=4)
        yT = hT
        rstd4 = f32v(hbuf, 8192, [128, 4, 512])
        sq2 = b16v(hbuf, 16384, [128, 8, 512])
        tn = [f32v(scr, 16384, [128, 512]), f32v(scr, 18432, [128, 512])]
        QS = 128.0 ** -0.5
        norm_a(0, b, l, 0)
        for jb in range(NB):
            t0 = jb * TB
            norm_b(jb, b, l, 0, 0, hT)
            for kc in range(8):
                mm(ps[2][:], wa[:, kc, :], hT[:, kc, :], start=(kc == 0), stop=(kc == 7))
            memset(alrT, 1.0)
            cp(alrT[0:16, :], ps[2][0:16, :], eng="dve")
            for tt_ in range(4):
                pz = ps[3 + tt_ % 2]
                mm(pz[:], alrT[0:17, tt_ * 128:(tt_ + 1) * 128], wgu[0:17, :])
                ts(m4[:, tt_, :], pz[:], 0.0, None, ALU.min)
                stt(u4[:, tt_, :], m4[:, tt_, :], 2.0, pz[:], ALU.mult, ALU.subtract)
            for hd in range(4):
                pb = ps[hd % 2]
                for kc in range(8):
                    mm(pb[:], wq[:, kc, hd * 128:(hd + 1) * 128], hT[:, kc, :], start=(kc == 0), stop=(kc == 7))
                act(qT[:, hd, :], pb[:], AF.Copy, scale=QS)
            act(u4[:, :, :], u4[:, :, :], AF.Exp)
            act(u4[:, :, :], u4[:, :, :], AF.Ln, bias=1.0)
            tt(la[:, :, :], m4[:, :, :], u4[:, :, :], ALU.subtract)
            for gc in range(8):
                pb = ps[gc % 2]
                for kc in range(8):
                    mm(pb[:], wg[:, kc, gc * 128:(gc + 1) * 128], hT[:, kc, :], start=(kc == 0), stop=(kc == 7))
                act(sgT[:, gc, :], pb[:], AF.Silu)
            for tt_ in range(4):
                mm(ps[3][:], mtri[:], la[:, tt_, :])
                for hd in range(4):
                    mm(ps[7][:, hd * 2:hd * 2 + 2], la[:, tt_, hd * 128:(hd + 1) * 128], ind[:])
                for kc in range(8):
                    mm(ps[4 + tt_ % 2][:], hT[:, kc, tt_ * 128:(tt_ + 1) * 128], wk[:, kc, :],
                       start=(kc == 0), stop=(kc == 7))
                act(expD, ps[3][:], AF.Exp)
                n0 = jb * 8 + tt_ * 2
                act(gam[:, :, n0:n0 + 2], ps[7][:, 0:8].rearrange("p (a b) -> p a b", b=2), AF.Exp)
                tt(kdec[:, tt_, :], ps[4 + tt_ % 2][:], expD, ALU.mult)
                for vh in range(2):
                    pb = ps[vh]
                    for kc in range(8):
                        mm(pb[:], hT[:, kc, tt_ * 128:(tt_ + 1) * 128], wv[:, kc, vh * 512:(vh + 1) * 512],
                           start=(kc == 0), stop=(kc == 7))
                    act(vv[:, tt_, vh * 512:(vh + 1) * 512], pb[:], AF.Copy)
            if jb == NB - 1 and after_proj is not None:
                after_proj()
            def kv_mm(n):
                tt_ = n // 2
                p0 = (n % 2) * 64
                kvb = (ps[0], ps[1]) if n % 2 == 0 else (ps[4], ps[5])
                for hd in range(4):
                    mm(kvb[hd // 2][:, (hd % 2) * 256:(hd % 2 + 1) * 256],
                       kdec[p0:p0 + 64, tt_, hd * 128:(hd + 1) * 128],
                       vv[p0:p0 + 64, tt_, hd * 256:(hd + 1) * 256])
            kv_mm(0)
            for n in range(8):
                ng = jb * 8 + n
                kvb = (ps[0], ps[1]) if n % 2 == 0 else (ps[4], ps[5])
                if n + 1 < 8:
                    kv_mm(n + 1)
                for hd in range(4):
                    stt(Sst[:, hd, :], Sst[:, hd, :], gam[:, hd, ng:ng + 1],
                        kvb[hd // 2][:, (hd % 2) * 256:(hd % 2 + 1) * 256], ALU.mult, ALU.add)
                    act(Sb[:, hd, :], Sst[:, hd, :], AF.Copy)
                    for dvc in range(2):
                        ch = hd * 2 + dvc
                        mm(ps[2 + n % 2][:, ch * 64:(ch + 1) * 64], Sb[:, hd, dvc * 128:(dvc + 1) * 128],
                           qT[:, hd, n * 64:(n + 1) * 64])
                if n >= 1:
                    cp(oT[:, :, (n - 1) * 64:n * 64], ps[2 + (n - 1) % 2][:].rearrange("p (a b) -> p a b", a=8),
                       eng="dve")
            cp(oT[:, :, 7 * 64:8 * 64], ps[2 + 7 % 2][:].rearrange("p (a b) -> p a b", a=8), eng="dve")
            for hd in range(4):
                act(sq2[:, 2 * hd:2 * hd + 2, :], oT[:, 2 * hd:2 * hd + 2, :], AF.Square)
                pn = ps[6 + hd % 2]
                for dvc in range(2):
                    mm(pn[:], ones_v[:], sq2[:, hd * 2 + dvc, :], start=(dvc == 0), stop=(dvc == 1))
                act(rstd4[:, hd, :], pn[:], AF.Ln, bias=EPS)
                act(rstd4[:, hd, :], rstd4[:, hd, :], AF.Exp, scale=-0.5)
            for hd in range(4):
                for dvc in range(2):
                    ch = hd * 2 + dvc
                    stt(tn[dvc], oT[:, ch, :], vcol(V_GNG + dvc), rstd4[:, hd, :], ALU.mult, ALU.mult)
                    tt(yT[:, ch, :], tn[dvc], sgT[:, ch, :], ALU.mult)
            if jb + 1 < NB:
                norm_a0(jb + 1)
            for m in range(8):
                if m == 4 and jb + 1 < NB:
                    norm_a(jb + 1, b, l, 0, do_sq=False)
                pb = ps[m % 2]
                for kc in range(8):
                    mm(pb[:], wout[:, kc, m * 128:(m + 1) * 128], yT[:, kc, :], start=(kc == 0), stop=(kc == 7))
                resid_update(pb[:], m, jb, ada_col(l, 2, m, b))

    def lru_views():
        return dict(
            wout=b16v(W, 0, [128, 8, 1024]),
            wgt=b16v(W, 24576, [128, 8, 1024]),
            wx=b16v(W, 40960, [128, 8, 1024]),
            wr=b16v(W, 57344, [128, 8, 256]),
            wi=b16v(W, 61440, [128, 8, 256]),
        )

    def lru_prefetch():
        v = lru_views()
        src = lru_w_in_d.rearrange("(c p) n -> p c n", p=128)
        dma("pool", v["wgt"], src[:, :, 0:1024])
        dma("pool", v["wx"], src[:, :, 1024:2048])
        dma("pool", v["wr"], lru_wr_d.rearrange("p (a b) -> p a b", a=8))
        dma("pool", v["wi"], lru_wi_d.rearrange("p (a b) -> p a b", a=8))

    hbr = smallw[:, 32:40]
    hbi = smallw[:, 40:48]
    cnh = smallw[:, 48:56]
    ts(hbr, vecs[:, V_BR:V_BR + 8], 0.5, None, ALU.mult)
    ts(hbi, vecs[:, V_BI:V_BI + 8], 0.5, None, ALU.mult)
    ts(cnh, cneg, 0.5, None, ALU.mult)

    def lru_layer(b, l=1, after_gate=None):
        V = lru_views()
        wout, wgt, wx, wr, wi = (V[k] for k in ("wout", "wgt", "wx", "wr", "wi"))
        dma("pool", wout, lru_w_out_d.rearrange("(c p) n -> p c n", p=128))
        hT = b16v(hbuf, 0, [128, 8, 512])
        yT = b16v(hbuf, 8192, [128, 8, 512])
        xb2 = [f32v(hbuf, 16384, [128, 2, 516]), f32v(hbuf, 27136, [128, 2, 516])]
        xc2 = [f32v(hbuf, 20992, [128, 2, 512]), cTr[:, 0:1024].rearrange("p (a b) -> p a b", a=2)]
        xcb = [b16v(hbuf, 25088, [128, 2, 512]),
               cTr[:, 1024:1536].bitcast(BF16).rearrange("p (a b) -> p a b", a=2)]
        TR = [f32v(scr, 0, [128, 2, 512]), f32v(W, 16384, [128, 2, 512])]
        TI = [f32v(scr, 4096, [128, 2, 512]), f32v(W, 20480, [128, 2, 512])]
        AA = [f32v(scr, 8192, [128, 2, 512]), f32v(W, 65536, [128, 2, 512])]
        MM = [f32v(scr, 12288, [128, 2, 512]), f32v(W, 69632, [128, 2, 512])]
        memset(halo[:], 0.0)
        memset(carry[:], 0.0)
        norm_a(0, b, l, 0)
        for jb in range(NB):
            t0 = jb * TB
            norm_b(jb, b, l, 0, 0, hT)
            def Gmm(hb):
                for cc in range(2):
                    c = hb * 2 + cc
                    for kc in range(8):
                        mm(ps[cc][:], wgt[:, kc, c * 128:(c + 1) * 128], hT[:, kc, :],
                           start=(kc == 0), stop=(kc == 7))

            def A1mm(hb):
                for cc in range(2):
                    c = hb * 2 + cc
                    pb = ps[2 + cc]
                    for kc in range(8):
                        mm(pb[:], wx[:, kc, c * 128:(c + 1) * 128], hT[:, kc, :], start=(kc == 0), stop=(kc == 7))

            def A1(hb):
                X, XC, XB = xb2[hb % 2], xc2[hb % 2], xcb[hb % 2]
                for cc in range(2):
                    c = hb * 2 + cc
                    act(X[:, cc, 0:3], halo[:, c, 0:3], AF.Copy)
                for cc in range(2):
                    act(X[:, cc, 3:515], ps[2 + cc][:], AF.Copy)
                for cc in range(2):
                    c = hb * 2 + cc
                    cp(halo[:, c, 0:3], X[:, cc, 512:515], eng="dve")
                    ts(XC[:, cc, :], X[:, cc, 0:512], vcol(V_CW + 0 * 8 + c), vcol(V_CB + c), ALU.mult, ALU.add)
                    for j in range(1, 4):
                        stt(XC[:, cc, :], X[:, cc, j:j + 512], vcol(V_CW + j * 8 + c), XC[:, cc, :],
                            ALU.mult, ALU.add)

            def A2(hb):
                X, XC, XB = xb2[hb % 2], xc2[hb % 2], xcb[hb % 2]
                for cc in range(2):
                    act(XB[:, cc, :], XC[:, cc, :], AF.Copy)
                for oc in range(2):
                    for kk in range(2):
                        mm(ps[4 + 2 * oc][:], wr[:, hb * 2 + kk, oc * 128:(oc + 1) * 128], XB[:, kk, :],
                           start=(kk == 0), stop=(kk == 1))
                    for kk in range(2):
                        mm(ps[5 + 2 * oc][:], wi[:, hb * 2 + kk, oc * 128:(oc + 1) * 128], XB[:, kk, :],
                           start=(kk == 0), stop=(kk == 1))

            def B1(hb):
                t_r, t_i, a_, m_ = TR[hb % 2], TI[hb % 2], AA[hb % 2], MM[hb % 2]
                for cc in range(2):
                    act(yT[:, hb * 2 + cc, :], ps[cc][:], AF.Gelu_apprx_tanh)
                for oc in range(2):
                    c = hb * 2 + oc
                    act(t_r[:, oc, :], ps[4 + 2 * oc][:], AF.Tanh, bias=hbr[:, c:c + 1], scale=0.5)
                    act(t_i[:, oc, :], ps[5 + 2 * oc][:], AF.Tanh, bias=hbi[:, c:c + 1], scale=0.5)
                for oc in range(2):
                    c = hb * 2 + oc
                    act(a_[:, oc, :], t_r[:, oc, :], AF.Exp, bias=cnh[:, c:c + 1], scale=cnh[:, c:c + 1])
                    act(m_[:, oc, :], t_r[:, oc, :], AF.Exp, bias=cneg[:, c:c + 1], scale=cneg[:, c:c + 1])
                for oc in range(2):
                    act(m_[:, oc, :], m_[:, oc, :], AF.Sqrt, bias=0.25, scale=-0.25)

            def B2(hb):
                t_r, t_i, a_, m_ = TR[hb % 2], TI[hb % 2], AA[hb % 2], MM[hb % 2]
                hs = t_r
                XC = xc2[hb % 2]
                for oc in range(2):
                    c = hb * 2 + oc
                    stt(t_i[:, oc, :], t_i[:, oc, :], 1.0, XC[:, oc, :], ALU.add, ALU.mult)
                    tt(m_[:, oc, :], m_[:, oc, :], t_i[:, oc, :], ALU.mult)
                    scan(hs[:, oc, :], a_[:, oc, :], m_[:, oc, :], carry[:, c:c + 1], ALU.mult, ALU.add)
                    cp(carry[:, c:c + 1], hs[:, oc, 511:512], eng="dve")
                    tt(yT[:, c, :], hs[:, oc, :], yT[:, c, :], ALU.mult)

            Gmm(0); A1mm(0); A1(0); A1mm(1); A2(0)
            for hb in range(4):
                if hb + 1 < 4:
                    A1(hb + 1)
                if hb + 2 < 4:
                    A1mm(hb + 2)
                B1(hb)
                if hb + 1 < 4:
                    Gmm(hb + 1)
                    A2(hb + 1)
                elif jb == NB - 1 and after_gate is not None:
                    after_gate()
                B2(hb)
            if jb + 1 < NB:
                norm_a0(jb + 1)
            for m in range(8):
                if m == 4 and jb + 1 < NB:
                    norm_a(jb + 1, b, l, 0, do_sq=False)
                pb = ps[m % 2]
                for kc in range(8):
                    mm(pb[:], wout[:, kc, m * 128:(m + 1) * 128], yT[:, kc, :], start=(kc == 0), stop=(kc == 7))
                resid_update(pb[:], m, jb, ada_col(l, 2, m, b))

    def moe_wviews(slot):
        base = slot * 24576
        return (b16v(W, base, [128, 8, 512]), b16v(W, base + 8192, [128, 8, 512]),
                b16v(W, base + 16384, [128, 4, 1024]))

    moe_state = {"slot": 0}

    def moe_load(l, e, slot, parts=("g", "u", "d")):
        wg, wu, wd = moe_wviews(slot)
        if "g" in parts:
            dma("pool", wg, wg_d[l, e].rearrange("(c p) n -> p c n", p=128))
        if "u" in parts:
            dma("pool", wu, wu_d[l, e].rearrange("(c p) n -> p c n", p=128))
        if "d" in parts:
            dma("pool", wd, wd_d[l, e].rearrange("(c p) n -> p c n", p=128))

    SLOTS = [1, 2, 0, 1, 2, 0, 1, 2, 0, 1, 2, 0, 1, 2, 1, 0]

    def moe_layer(b, l, pre_last=None):
        hT = b16v(hbuf, 0, [128, 8, S])
        for jb in range(NB):
            norm_block(jb, b, l, 1, 3, hT[:, :, jb * TB:(jb + 1) * TB])
        for t in range(16):
            for kc in range(8):
                mm(ps[6][:, t * 16:(t + 1) * 16], hT[:, kc, t * 128:(t + 1) * 128], rw[:, kc, :],
                   start=(kc == 0), stop=(kc == 7))
        R = lambda i: rt[:, i * 256:(i + 1) * 256]
        R3 = lambda i: rt[:, i * 256:(i + 1) * 256].rearrange("p (t e) -> p t e", e=16)
        R4 = lambda i: rt[:, i * 256:(i + 1) * 256].rearrange("p (t g j) -> p t g j", g=4, j=4)
        G = lambda i: rt[:, 2304 + i * 64:2304 + (i + 1) * 64]
        G3 = lambda i: rt[:, 2304 + i * 64:2304 + (i + 1) * 64].rearrange("p (t g) -> p t g", g=4)
        sc_, sel_, msk, tmp_ = 0, 1, 2, 3
        act(R(sc_), ps[6][:, 0:256], AF.Sigmoid)
        tt(R3(sel_), R3(sc_), rb[:, :].unsqueeze(1).to_broadcast([128, 16, 16]), ALU.add)
        red(G(0), R4(sel_), ALU.max)
        tt(R4(msk), R4(sel_), G3(0).unsqueeze(3).to_broadcast([128, 16, 4, 4]), ALU.is_ge)
        stt(R(tmp_), R(msk), -1e30, R(sel_), ALU.mult, ALU.add)
        red(G(1), R4(tmp_), ALU.max)
        tt(G(2), G(0), G(1), ALU.add)
        red(rt[:, 1664:1680], G3(2), ALU.max)
        tt(G3(3), G3(2), rt[:, 1664:1680].unsqueeze(2).to_broadcast([128, 16, 4]), ALU.is_ge)
        tt(R4(msk), R4(sel_), G3(1).unsqueeze(3).to_broadcast([128, 16, 4, 4]), ALU.is_ge)
        tt(R4(msk), R4(msk), G3(3).unsqueeze(3).to_broadcast([128, 16, 4, 4]), ALU.mult)
        tt(R(tmp_), R(msk), R(sc_), ALU.mult)
        red(rt[:, 1680:1696], R3(tmp_), ALU.add)
        recip(rt[:, 1680:1696], rt[:, 1680:1696])
        tt(R3(tmp_), R3(tmp_), rt[:, 1680:1696].unsqueeze(2).to_broadcast([128, 16, 16]), ALU.mult)
        pk = rt[:, 1024:1536].rearrange("p (t e) -> p t e", e=32)
        hib = rt[:, 1536:1664].bitcast(BF16).rearrange("p (t e) -> p t e", e=16)
        cp(hib, R3(tmp_), eng="dve")
        cp(pk[:, :, 0:16], hib, eng="dve")
        tt(pk[:, :, 16:32], R3(tmp_), pk[:, :, 0:16], ALU.subtract)
        combT = cTr[0:32, 0:1024].bitcast(BF16)
        for t4 in range(4):
            for q in range(4):
                t = t4 * 4 + q
                tr(ps[7][0:32, q * 128:(q + 1) * 128], pk[:, t, :], ident[:])
            act(combT[:, t4 * 512:(t4 + 1) * 512], ps[7][0:32, :], AF.Copy)
        if "comb" in dbg_d and b == dbg.get("_b", 0) and l == dbg.get("_l", 0):
            dma("sp", dbg_d["comb"], combT)
        he = [b16v(scr, 0, [128, 4, 512]), b16v(scr, 4096, [128, 4, 512])]
        sg = [f32v(scr, 8192, [128, 512]), f32v(scr, 10240, [128, 512])]
        tb = [f32v(scr, 12288, [128, 512]), f32v(scr, 14336, [128, 512])]
        cbs = [f32v(scr, 16384, [128, 512]), f32v(scr, 18432, [128, 512])]
        units = [(e, jb) for e in range(NE) for jb in range(NB)]
        DB = (4, 5, 7)

        def cb_and_gu0(i):
            e, jb = units[i]
            t0 = jb * TB
            wg, wu, wd = moe_wviews(SLOTS[e])
            mm(ps[6][:], i32b[:, e:e + 1].to_broadcast([32, 128]), combT[:, t0:t0 + TB])
            act(cbs[i % 2], ps[6][:], AF.Copy)
            gu_mm(i, 0)

        def gu_mm(i, c):
            e, jb = units[i]
            t0 = jb * TB
            wg, wu, wd = moe_wviews(SLOTS[e])
            pg = ps[0 + c % 2]
            pu = ps[2 + c % 2]
            for kc in range(8):
                mm(pg[:], wg[:, kc, c * 128:(c + 1) * 128], hT[:, kc, t0:t0 + TB],
                   start=(kc == 0), stop=(kc == 7))
            for kc in range(8):
                mm(pu[:], wu[:, kc, c * 128:(c + 1) * 128], hT[:, kc, t0:t0 + TB],
                   start=(kc == 0), stop=(kc == 7))

        def load_hook(i):
            e, jb = units[i]
            if jb == 0:
                if e + 1 < NE:
                    moe_load(l, e + 1, SLOTS[e + 1])
                elif pre_last is not None:
                    pre_last()

        load_hook(0)
        cb_and_gu0(0)
        for i, (e, jb) in enumerate(units):
            wg, wu, wd = moe_wviews(SLOTS[e])
            k2 = i % 2
            for c in range(4):
                if c >= 1:
                    gu_mm(i, c)
                pg = ps[0 + c % 2]
                pu = ps[2 + c % 2]
                act(sg[c % 2], pg[:], AF.Silu)
                tt(tb[c % 2], pu[:], sg[c % 2], ALU.mult)
                tt(he[k2][:, c, :], tb[c % 2], cbs[k2], ALU.mult)
            if i + 1 < len(units):
                cb_and_gu0(i + 1)
            for m in range(8):
                pd = ps[DB[m % 3]]
                for kc in range(4):
                    mm(pd[:], wd[:, kc, m * 128:(m + 1) * 128], he[k2][:, kc, :],
                       start=(kc == 0), stop=(kc == 3))
                resid_update(pd[:], m, jb, ada_col(l, 5, m, b))
            if i + 1 < len(units):
                load_hook(i + 1)

    def final_store(b, si):
        sq = b16v(scr, 0, [128, 8, 512])
        rstd = f32v(scr, 8192, [128, 512])
        ob = [f32v(hbuf, 0, [128, 8, 512]), f32v(hbuf, 16384, [128, 8, 512])]
        outs = []
        for jb in range(NB):
            t0 = jb * TB
            act(sq[:, :, :], xT[:, :, t0:t0 + TB], AF.Square)
            for c in range(8):
                mm(ps[6][:], ones_d[:], sq[:, c, :], start=(c == 0), stop=(c == 7))
            act(rstd, ps[6][:], AF.Ln, bias=EPS)
            act(rstd, rstd, AF.Exp, scale=-0.5)
            o = ob[jb % 2]
            for c in range(8):
                stt(o[:, c, :], xT[:, c, t0:t0 + TB], vcol(V_FNG + c), rstd, ALU.mult, ALU.mult)
            outs.append(dma("sp", outT_d[si, :, t0:t0 + TB].rearrange("(c p) t -> p c t", p=128), o))
        return outs

    out_ops = []
    phases = dbg.get("_phases", ("gla", "moe0", "lru", "moe1", "final"))
    for si in range(n_seq):
        b = si
        for jb in range(NB):
            dma("sp", xT[:, :, jb * TB:(jb + 1) * TB],
                xT_d[si, :, jb * TB:(jb + 1) * TB].rearrange("(c p) t -> p c t", p=128))
        if si == 0 and "gla" in phases:
            gla_prefetch()
        if "gla" in phases:
            gla_layer(b, after_proj=(lambda: moe_load(0, 0, SLOTS[0])) if "moe0" in phases else None)
        if "xmid0" in dbg_d and si == dbg.get("_b", 0):
            out_ops.append(dma("sp", dbg_d["xmid0"].rearrange("(c p) t -> p c t", p=128), xT[:, :, :]))
        if "moe0" in phases:
            if "gla" not in phases:
                moe_load(0, 0, SLOTS[0])
            moe_layer(b, 0, pre_last=lru_prefetch if "lru" in phases else None)
        if "xmid1" in dbg_d and si == dbg.get("_b", 0):
            out_ops.append(dma("sp", dbg_d["xmid1"].rearrange("(c p) t -> p c t", p=128), xT[:, :, :]))
        if "lru" in phases:
            if "moe0" not in phases:
                lru_prefetch()
            lru_layer(b, after_gate=(lambda: moe_load(1, 0, SLOTS[0], ("g", "u"))) if "moe1" in phases else None)
            if "moe1" in phases:
                moe_load(1, 0, SLOTS[0], ("d",))
        if "xmid2" in dbg_d and si == dbg.get("_b", 0):
            out_ops.append(dma("sp", dbg_d["xmid2"].rearrange("(c p) t -> p c t", p=128), xT[:, :, :]))
        if "moe1" in phases:
            if "lru" not in phases:
                moe_load(1, 0, SLOTS[0])
            moe_layer(b, 1, pre_last=gla_prefetch if (si + 1 < n_seq and "gla" in phases) else None)
        if "final" in phases:
            out_ops += final_store(b, si)

    Sd.add("sp", lambda e: e.nop(), reads=[], writes=[]).deps.extend(
        [o for o in Sd.dma_ops["sp"]])

    Sd.finalize()

    sem_stack = contextlib.ExitStack()
    csem = {e: sem_stack.enter_context(nc.semaphore("c_" + e)) for e in Sched.ENGS}
    dsems = {e: [sem_stack.enter_context(nc.semaphore("d_%s_%d" % (e, i))) for i in range(Sched.NDS)]
             for e in ("sp", "pool")}
    with nc.Block() as block:
        @block.tensor
        def _(e):
            Sd.emit("pe", e, csem, dsems)

        @block.scalar
        def _(e):
            Sd.emit("act", e, csem, dsems)

        @block.vector
        def _(e):
            Sd.emit("dve", e, csem, dsems)

        @block.gpsimd
        def _(e):
            Sd.emit("pool", e, csem, dsems)

        @block.sync
        def _(e):
            Sd.emit("sp", e, csem, dsems)
    sem_stack.close()
    stack.close()
    return nc, Sd


V_NMG = 0
V_NFG = 16
V_FNG = 32
V_GNG = 40
V_CW = 42
V_CB = 74
V_BR = 82
V_BI = 90
V_LAM = 98
NV = 106


def _fm(v):
    v = np.asarray(v, np.float32).reshape(-1, 128)
    return np.ascontiguousarray(v.T)


def prep_inputs(inp):
    f = lambda a: np.ascontiguousarray(np.asarray(a, dtype=np.float32))
    x = f(inp["x"])
    c = f(inp["c"])
    vec = np.zeros((128, NV), np.float32)
    for l in range(2):
        vec[:, V_NMG + l * 8:V_NMG + (l + 1) * 8] = _fm(inp["norm_mix_g"][l])
        vec[:, V_NFG + l * 8:V_NFG + (l + 1) * 8] = _fm(inp["norm_ffn_g"][l])
    vec[:, V_FNG:V_FNG + 8] = _fm(inp["final_norm_g"])
    vec[:, V_GNG:V_GNG + 2] = _fm(inp["gla_norm_g"][0])
    for j in range(4):
        vec[:, V_CW + j * 8:V_CW + (j + 1) * 8] = _fm(inp["lru_conv_w"][0, j])
    vec[:, V_CB:V_CB + 8] = _fm(inp["lru_conv_b"][0])
    vec[:, V_BR:V_BR + 8] = _fm(np.asarray(inp["lru_b_r"][0]).reshape(-1))
    vec[:, V_BI:V_BI + 8] = _fm(np.asarray(inp["lru_b_i"][0]).reshape(-1))
    vec[:, V_LAM:V_LAM + 8] = _fm(inp["lru_lambda"][0])
    ada_b = np.concatenate([_fm(inp["ada_b"][0]), _fm(inp["ada_b"][1])], axis=1)
    wgu = np.concatenate([f(inp["gla_w_gate_up"][0]), f(inp["gla_b_gate"][0])[None, :]], axis=0)

    def blk(w):
        w = f(w).reshape(4, 2, 128, 256).transpose(2, 0, 1, 3)
        return np.ascontiguousarray(w).reshape(128, 8 * 256)

    rwl = f(inp["router_w"]).reshape(8, 128, 16).transpose(1, 0, 2).reshape(128, 128)
    shared = {
        "ada_w": f(inp["ada_w"]), "ada_b": ada_b, "vecs": vec,
        "gla_w_in": f(inp["gla_w_in"][0]), "gla_w_out": f(inp["gla_w_out"][0]), "wgu": wgu,
        "lru_w_in": f(inp["lru_w_in"][0]), "lru_w_out": f(inp["lru_w_out"][0]),
        "lru_wr": blk(inp["lru_w_r"][0]), "lru_wi": blk(inp["lru_w_i"][0]),
        "rw": np.ascontiguousarray(rwl), "rb": np.ascontiguousarray(np.tile(f(inp["router_bias"])[None, :], (128, 1))),
        "moe_wg": f(inp["moe_w_gate"]), "moe_wu": f(inp["moe_w_up"]), "moe_wd": f(inp["moe_w_down"]),
        "ident": np.eye(128, dtype=np.float32),
    }
    s_i = np.arange(128)[:, None]
    c_i = np.arange(128)[None, :]
    shared["mtri"] = (((s_i > c_i) & (s_i // 64 == c_i // 64)).astype(np.float32) / 16.0)
    shared["ind"] = ((s_i // 64) == np.arange(2)[None, :]).astype(np.float32) / 16.0
    shared["i16"] = np.eye(16, dtype=np.float32)
    in_maps = []
    for i in range(N_CORES):
        m = dict(shared)
        m["xT"] = np.ascontiguousarray(x[2 * i:2 * i + 2].transpose(0, 2, 1))
        cc = c[2 * i:2 * i + 2]
        m["cT"] = np.ascontiguousarray(cc.reshape(2, 8, 128).transpose(2, 1, 0).reshape(128, 16))
        in_maps.append(m)
    return in_maps


_CACHE = {}


def kernel(**inputs):
    in_maps = prep_inputs(inputs)
    if "nc" not in _CACHE:
        _CACHE["nc"] = build_program()[0]
    nc = _CACHE["nc"]
    res = run_bass_kernel_spmd(nc, in_maps, core_ids=list(range(N_CORES)))
    out = np.empty((16, S, D), np.float32)
    for i in range(N_CORES):
        o = res.results[i]["outT"]
        out[2 * i:2 * i + 2] = o.transpose(0, 2, 1)
    return out
```

```python
import numpy as np
import concourse.bass as bass
import concourse.mybir as mybir
from concourse.bass_utils import run_bass_kernel_spmd

F32 = mybir.dt.float32
BF16 = mybir.dt.bfloat16
AF = mybir.ActivationFunctionType
ALU = mybir.AluOpType
ESZ = {F32: 4, BF16: 2}

D = 1024
S = 2048
NB = 4
TB = 512
NE = 16
DE = 512
EPS = 1e-6
GRAN = 512
N_CORES = 8


class Op:
    __slots__ = ("eng", "idx", "fn", "deps", "dma", "dsem", "dval", "sig", "sigval", "waits", "gidx")


class Sched:
    ENGS = ("pe", "act", "dve", "pool", "sp")
    NDS = 12

    def __init__(self, nc):
        self.nc = nc
        self.ops = {e: [] for e in self.ENGS}
        self.last_w = {}
        self.readers = {}
        self.ndma = {e: 0 for e in self.ENGS}
        self.dma_ops = {e: [] for e in self.ENGS}
        self.tok_cache = {}
        self.gcount = 0

    def tokens(self, ap):
        sp = str(ap.space)
        if "SB" not in sp and "PSUM" not in sp:
            return ()
        if "PSUM" in sp:
            return ((ap.tensor.name, 0),)
        key = (ap.tensor.name, ap.offset, ap.ap, ap.dtype)
        t = self.tok_cache.get(key)
        if t is None:
            es = ESZ[ap.dtype]
            pstride = ap.ap[0][0]
            off = ap.offset % pstride if pstride > 0 else ap.offset
            name = ap.tensor.name
            dims = [(abs(st), cnt) for st, cnt in ap.ap[1:] if cnt > 1 and st != 0]
            dims.sort(reverse=True)
            outer = dims[:-1] if dims else []
            n_outer = 1
            for _, cnt in outer:
                n_outer *= cnt
            gs = set()
            if dims and n_outer <= 512:
                lst, lcnt = dims[-1]
                bases = [off]
                for st, cnt in outer:
                    bases = [b0 + i * st for b0 in bases for i in range(cnt)]
                for b0 in bases:
                    lo = b0 * es
                    hi = (b0 + (lcnt - 1) * lst + 1) * es
                    gs.update(range(lo // GRAN, (hi - 1) // GRAN + 1))
            else:
                span = 0
                for st, cnt in dims:
                    span += (cnt - 1) * st
                lo = off * es
                hi = (off + span + 1) * es
                gs.update(range(lo // GRAN, (hi - 1) // GRAN + 1))
            t = tuple((name, g) for g in sorted(gs))
            self.tok_cache[key] = t
        return t

    def add(self, eng, fn, reads=(), writes=(), dma=False):
        op = Op()
        op.eng = eng
        op.idx = len(self.ops[eng])
        op.fn = fn
        op.dma = dma
        op.sig = False
        op.sigval = 0
        op.gidx = self.gcount
        self.gcount += 1
        deps = {}
        rt = []
        for ap in reads:
            rt.extend(self.tokens(ap))
        wt = []
        for ap in writes:
            wt.extend(self.tokens(ap))
        for r in rt:
            w = self.last_w.get(r)
            if w is not None:
                deps[id(w)] = w
        for r in wt:
            w = self.last_w.get(r)
            if w is not None:
                deps[id(w)] = w
            rd = self.readers.get(r)
            if rd:
                for o in rd.values():
                    deps[id(o)] = o
        deps.pop(id(op), None)
        op.deps = list(deps.values())
        for r in rt:
            d = self.readers.get(r)
            if d is None:
                d = {}
                self.readers[r] = d
            if dma:
                d[(eng, op.idx)] = op
            else:
                d[eng] = op
        for r in wt:
            self.last_w[r] = op
            self.readers[r] = {}
        if dma:
            i = self.ndma[eng]
            self.ndma[eng] = i + 1
            op.dsem = i % self.NDS
            op.dval = 16 * (i // self.NDS + 1)
            if i >= self.NDS:
                op.deps.append(self.dma_ops[eng][i - self.NDS])
            self.dma_ops[eng].append(op)
        self.ops[eng].append(op)
        return op

    def finalize(self):
        for eng in self.ENGS:
            known = {}
            for op in self.ops[eng]:
                need = {}
                for d in op.deps:
                    if d.dma:
                        k = ("d", d.eng, d.dsem)
                        v = d.dval
                        if v > need.get(k, (0, None))[0]:
                            need[k] = (v, d)
                    else:
                        if d.eng == "pe" and eng == "pe" and not op.dma:
                            continue
                        k = ("c", d.eng)
                        v = d.idx + 1
                        if v > need.get(k, (0, None))[0]:
                            need[k] = (v, d)
                waits = []
                for k, (v, d) in need.items():
                    if known.get(k, 0) >= v:
                        continue
                    known[k] = v
                    waits.append(d)
                    if not d.dma:
                        d.sig = True
                op.waits = waits
        for eng in self.ENGS:
            c = 0
            for op in self.ops[eng]:
                if op.sig and not op.dma:
                    c += 1
                    op.sigval = c

    def emit(self, eng, e, csem, dsems):
        for op in self.ops[eng]:
            for d in op.waits:
                if d.dma:
                    e.wait_ge(dsems[d.eng][d.dsem], d.dval)
                else:
                    e.wait_ge(csem[d.eng], d.sigval)
            ins = op.fn(e)
            if op.dma:
                ins.then_inc(dsems[eng][op.dsem], 16)
            elif op.sig:
                ins.then_inc(csem[eng], 1)


def build_program(n_seq=2, dbg=None):
    dbg = dbg or {}
    nc = bass.Bass("TRN2", target_bir_lowering=False)
    Sd = Sched(nc)

    def din(name, shape):
        return nc.dram_tensor(name, list(shape), F32, kind="ExternalInput").ap()

    xT_d = din("xT", [2, D, S])
    cT_d = din("cT", [128, 16])
    ada_w_d = din("ada_w", [2, D, 6 * D])
    ada_b_d = din("ada_b", [128, 96])
    vecs_d = din("vecs", [128, NV])
    gla_w_in_d = din("gla_w_in", [D, 3088])
    gla_w_out_d = din("gla_w_out", [D, D])
    wgu_d = din("wgu", [17, 512])
    lru_w_in_d = din("lru_w_in", [D, 2 * D])
    lru_w_out_d = din("lru_w_out", [D, D])
    lru_wr_d = din("lru_wr", [128, 8 * 256])
    lru_wi_d = din("lru_wi", [128, 8 * 256])
    rw_d = din("rw", [128, 8 * 16])
    rb_d = din("rb", [128, 16])
    wg_d = din("moe_wg", [2, NE, D, DE])
    wu_d = din("moe_wu", [2, NE, D, DE])
    wd_d = din("moe_wd", [2, NE, DE, D])
    ident_d = din("ident", [128, 128])
    mtri_d = din("mtri", [128, 128])
    ind_d = din("ind", [128, 2])
    i16_d = din("i16", [16, 16])
    outT_d = nc.dram_tensor("outT", [2, D, S], F32, kind="ExternalOutput").ap()
    dbg_d = {}
    for k, shp in dbg.items():
        if k.startswith("_"):
            continue
        dbg_d[k] = nc.dram_tensor("dbg_" + k, list(shp), F32, kind="ExternalOutput").ap()

    import contextlib
    stack = contextlib.ExitStack()

    def SB(name, shape, dt=F32):
        return stack.enter_context(nc.sbuf_tensor("s_" + name, list(shape), dt))

    def PS(name):
        return stack.enter_context(nc.psum_tensor(name, [128, 512], F32))

    xT = SB("xT", [128, 8, S])
    hbuf = SB("hbuf", [128, 16384], BF16)
    W = SB("W", [128, 36864], BF16)
    scr = SB("scr", [128, 10240], BF16)
    cTr = SB("cTr", [128, 2048])
    ident = SB("ident", [128, 128])
    mtri = SB("mtri", [128, 128])
    ind = SB("ind", [128, 2])
    i32f = SB("i32f", [32, 16])
    i32b = SB("i32b", [32, 16], BF16)
    ones_d = SB("ones_d", [128, 128], BF16)
    ones_v = SB("ones_v", [128, 128], BF16)
    vecs = SB("vecs", [128, NV])
    cT_s = SB("cT_s", [128, 16])
    cond = SB("cond", [128, 16], BF16)
    ada_b = SB("ada_b", [128, 96])
    ada = SB("ada", [128, 2, 48, 2])
    mod = SB("mod", [128, 2, 4, 8, 2])
    rw = SB("rw", [128, 8, 16], BF16)
    rb = SB("rb", [128, 16])
    wgu = SB("wgu", [32, 512])
    smallw = SB("smallw", [128, 256])
    halo = SB("halo", [128, 8, 4])
    carry = SB("carry", [128, 8])
    rt = scr[:, 0:5120].bitcast(F32)

    ps = [PS("ps%d" % i) for i in range(8)]

    def mm(out, lhsT, rhs, start=True, stop=True):
        return Sd.add("pe", lambda e: e.matmul(out, lhsT, rhs, start=start, stop=stop),
                      reads=[lhsT, rhs], writes=[out])

    def tr(out, in_, idn):
        return Sd.add("pe", lambda e: e.transpose(out, in_, idn), reads=[in_, idn], writes=[out])

    def act(out, in_, func, bias=None, scale=None, eng="act"):
        rd = [in_]
        kw = {}
        if bias is not None:
            kw["bias"] = bias
            if not isinstance(bias, (int, float)):
                rd.append(bias)
        if scale is not None:
            kw["scale"] = scale
            if not isinstance(scale, (int, float)):
                rd.append(scale)
        return Sd.add("act", lambda e: e.activation(out, in_, func, **kw), reads=rd, writes=[out])

    def stt(out, in0, scalar, in1, op0, op1, eng="dve"):
        rd = [in0, in1]
        if not isinstance(scalar, (int, float)):
            rd.append(scalar)
        return Sd.add(eng, lambda e: e.scalar_tensor_tensor(out, in0, scalar, in1, op0, op1),
                      reads=rd, writes=[out])

    def ts(out, in0, s1, s2, op0, op1=None, eng="dve"):
        rd = [in0]
        for s in (s1, s2):
            if s is not None and not isinstance(s, (int, float)):
                rd.append(s)
        if op1 is None:
            return Sd.add(eng, lambda e: e.tensor_scalar(out, in0, s1, None, op0), reads=rd, writes=[out])
        return Sd.add(eng, lambda e: e.tensor_scalar(out, in0, s1, s2, op0, op1), reads=rd, writes=[out])

    def tt(out, in0, in1, op, eng="dve"):
        return Sd.add(eng, lambda e: e.tensor_tensor(out, in0, in1, op), reads=[in0, in1], writes=[out])

    def cp(out, in_, eng="dve"):
        return Sd.add(eng, lambda e: e.tensor_copy(out, in_), reads=[in_], writes=[out])

    def recip(out, in_):
        return Sd.add("dve", lambda e: e.reciprocal(out, in_), reads=[in_], writes=[out])

    def red(out, in_, op, eng="dve"):
        return Sd.add(eng, lambda e: e.tensor_reduce(out, in_, mybir.AxisListType.X, op),
                      reads=[in_], writes=[out])

    def scan(out, d0, d1, init, op0, op1):
        rd = [d0, d1]
        if not isinstance(init, (int, float)):
            rd.append(init)
        return Sd.add("dve", lambda e: e.tensor_tensor_scan(out, d0, d1, init, op0, op1),
                      reads=rd, writes=[out])

    def memset(ap, val, eng="dve"):
        return Sd.add(eng, lambda e: e.memset(ap, val), writes=[ap])

    def dma(q, out, in_, **kw):
        return Sd.add(q, lambda e: e.dma_start(out=out, in_=in_, **kw), reads=[in_], writes=[out], dma=True)

    def f32v(t, off_b, shape):
        n = int(np.prod(shape[1:]))
        v = t[0:shape[0], off_b // 2: off_b // 2 + 2 * n].bitcast(F32)
        if len(shape) == 3:
            v = v.rearrange("p (a b) -> p a b", a=shape[1])
        elif len(shape) == 4:
            v = v.rearrange("p (a b c) -> p a b c", a=shape[1], b=shape[2])
        return v

    def b16v(t, off_b, shape):
        n = int(np.prod(shape[1:]))
        v = t[0:shape[0], off_b // 2: off_b // 2 + n]
        if len(shape) == 3:
            v = v.rearrange("p (a b) -> p a b", a=shape[1])
        elif len(shape) == 4:
            v = v.rearrange("p (a b c) -> p a b c", a=shape[1], b=shape[2])
        return v

    def vcol(i):
        return vecs[:, i:i + 1]

    dma("sp", ident[:], ident_d)
    dma("sp", mtri[:], mtri_d)
    dma("sp", ind[:], ind_d)
    dma("sp", i32f[0:16, :], i16_d)
    dma("sp", i32f[16:32, :], i16_d)
    cp(i32b[:], i32f[:], eng="dve")
    dma("sp", vecs[:], vecs_d)
    dma("sp", cT_s[:], cT_d)
    dma("sp", ada_b[:], ada_b_d)
    dma("sp", rb[:], rb_d)
    dma("sp", wgu[0:17, :], wgu_d)
    dma("pool", rw[:].rearrange("p a b -> p (a b)"), rw_d)
    memset(ones_d[:], 1.0 / 1024.0)
    memset(ones_v[:], 1.0 / 256.0)
    act(cond[:], cT_s[:], AF.Silu)

    NPIECE = 12
    for l in range(2):
        for pc in range(NPIECE):
            slot = (l * NPIECE + pc) % 3
            wv = b16v(hbuf, slot * 8192, [128, 8, 512])
            dma("pool", wv, ada_w_d[l, :, pc * 512:(pc + 1) * 512].rearrange("(c p) n -> p c n", p=128))
            for oc4 in range(4):
                oc = pc * 4 + oc4
                for kc in range(8):
                    mm(ps[7][:, (l * 48 + oc) * 2:(l * 48 + oc) * 2 + 2],
                       wv[:, kc, oc4 * 128:(oc4 + 1) * 128],
                       cond[:, kc * 2:kc * 2 + 2], start=(kc == 0), stop=(kc == 7))
    for l in range(2):
        tt(ada[:, l, :, :], ps[7][:, l * 96:(l + 1) * 96].rearrange("p (a b) -> p a b", b=2),
           ada_b[:, l * 48:(l + 1) * 48].unsqueeze(2).to_broadcast([128, 48, 2]), ALU.add)
    for l in range(2):
        for j, (which, gcol) in enumerate(((1, V_NMG + l * 8), (4, V_NFG + l * 8))):
            ts(mod[:, l, j, :, :], ada[:, l, which * 8:(which + 1) * 8, :], 1.0, None, ALU.add)
            tt(mod[:, l, j, :, :], mod[:, l, j, :, :],
               vecs[:, gcol:gcol + 8].unsqueeze(2).to_broadcast([128, 8, 2]), ALU.mult)

    def A_of(l, j, c, b):
        return mod[:, l, j, c, b:b + 1]

    def ada_col(l, which, c, b):
        return ada[:, l, which * 8 + c, b:b + 1]

    lam = vecs[:, V_LAM:V_LAM + 8]
    sw_a = smallw[:, 0:8]
    sw_b = smallw[:, 8:16]
    cneg = smallw[:, 16:24]
    cneg2 = smallw[:, 24:32]
    ts(sw_b, lam, 0.0, None, ALU.min)
    stt(sw_a, sw_b, 2.0, lam, ALU.mult, ALU.subtract)
    act(sw_a, sw_a, AF.Exp)
    act(sw_a, sw_a, AF.Ln, bias=1.0)
    tt(sw_a, sw_a, sw_b, ALU.subtract)
    ts(cneg, sw_a, -8.0, None, ALU.mult)
    ts(cneg2, sw_a, -16.0, None, ALU.mult)

    def norm_a0(jb):
        t0 = jb * TB
        sq = b16v(scr, 0, [128, 8, 512])
        act(sq[:, :, :], xT[:, :, t0:t0 + TB], AF.Square)

    def norm_a(jb, b, l, j, npre=2, do_sq=True):
        t0 = jb * TB
        sq = b16v(scr, 0, [128, 8, 512])
        rstd = f32v(scr, 8192, [128, 512])
        tmp = [f32v(scr, 10240, [128, 512]), f32v(scr, 12288, [128, 512])]
        if do_sq:
            norm_a0(jb)
        for c in range(8):
            mm(ps[6][:], ones_d[:], sq[:, c, :], start=(c == 0), stop=(c == 7))
        act(rstd, ps[6][:], AF.Ln, bias=EPS)
        act(rstd, rstd, AF.Exp, scale=-0.5)
        for c in range(npre):
            stt(tmp[c % 2], xT[:, c, t0:t0 + TB], A_of(l, j, c, b), rstd, ALU.mult, ALU.mult)

    def norm_b(jb, b, l, j, shift_which, dst, npre=2):
        t0 = jb * TB
        rstd = f32v(scr, 8192, [128, 512])
        tmp = [f32v(scr, 10240, [128, 512]), f32v(scr, 12288, [128, 512])]
        for c in range(8):
            tm = tmp[c % 2]
            if c >= npre:
                stt(tm, xT[:, c, t0:t0 + TB], A_of(l, j, c, b), rstd, ALU.mult, ALU.mult)
            act(dst[:, c, :], tm, AF.Identity, bias=ada_col(l, shift_which, c, b))

    def norm_block(jb, b, l, j, shift_which, dst):
        norm_a(jb, b, l, j, npre=0)
        norm_b(jb, b, l, j, shift_which, dst, npre=0)

    def resid_update(pbank, m, jb, gcol):
        t0 = jb * TB
        stt(xT[:, m, t0:t0 + TB], pbank, gcol, xT[:, m, t0:t0 + TB], ALU.mult, ALU.add)

    wa = SB("wa", [128, 8, 128], BF16)
    memset(wa[:], 0.0)

    def gla_views():
        return dict(
            wout=b16v(W, 0, [128, 8, 1024]),
            Sst=f32v(W, 16384, [128, 4, 256]),
            Sb=b16v(W, 20480, [128, 4, 256]),
            gam=f32v(W, 22528, [128, 4, 32]),
            wq=b16v(W, 24576, [128, 8, 512]),
            wk=b16v(W, 32768, [128, 8, 512]),
            wv=b16v(W, 40960, [128, 8, 1024]),
            wg=b16v(W, 57344, [128, 8, 1024]),
        )

    def gla_prefetch():
        v = gla_views()
        src = gla_w_in_d.rearrange("(c p) n -> p c n", p=128)
        dma("pool", wa[:, :, 0:16], src[:, :, 3072:3088])
        dma("pool", v["wq"], src[:, :, 0:512])
        dma("pool", v["wg"], src[:, :, 2048:3072])
        dma("pool", v["wk"], src[:, :, 512:1024])
        dma("pool", v["wv"], src[:, :, 1024:2048])

    def gla_layer(b, l=0, after_proj=None):
        V = gla_views()
        wout, Sst, Sb, gam, wq, wk, wv, wg = (V[k] for k in ("wout", "Sst", "Sb", "gam", "wq", "wk", "wv", "wg"))
        dma("pool", wout, gla_w_out_d.rearrange("(c p) n -> p c n", p=128))
        memset(Sst, 0.0)
        hT = b16v(hbuf, 0, [128, 8, 512])
        qT = b16v(hbuf, 8192, [128, 4, 512])
        kdec = b16v(hbuf, 12288, [128, 4, 512])
        vv = b16v(hbuf, 16384, [128, 4, 1024])
        sgT = b16v(hbuf, 24576, [128, 8, 512])
        oT = f32v(scr, 0, [128, 8, 512])
        m4 = f32v(scr, 0, [128, 4, 512])
        u4 = f32v(scr, 8192, [128, 4, 512])
        alrT = f32v(scr, 16384, [32, 512])
        expD = f32v(scr, 18432, [128, 512])
        la = cTr[:, :].rearrange("p (a b) -> p a b", a=4)
        yT = hT
        rstd4 = f32v(hbuf, 8192, [128, 4, 512])
        sq2 = b16v(hbuf, 16384, [128, 8, 512])
        tn = [f32v(scr, 16384, [128, 512]), f32v(scr, 18432, [128, 512])]
        QS = 128.0 ** -0.5
        norm_a(0, b, l, 0)
        for jb in range(NB):
            t0 = jb * TB
            norm_b(jb, b, l, 0, 0, hT)
            QB = (ps[0], ps[1], ps[4], ps[5])
            for kc in range(8):
                mm(ps[2][:], wa[:, kc, :], hT[:, kc, :], start=(kc == 0), stop=(kc == 7))
                for hd in range(4):
                    mm(QB[hd][:], wq[:, kc, hd * 128:(hd + 1) * 128], hT[:, kc, :],
                       start=(kc == 0), stop=(kc == 7))
            memset(alrT, 1.0)
            cp(alrT[0:16, :], ps[2][0:16, :], eng="dve")
            for hd in range(4):
                act(qT[:, hd, :], QB[hd][:], AF.Copy, scale=QS)
            for tt_ in range(4):
                pz = ps[2 + tt_ % 2]
                mm(pz[:], alrT[0:17, tt_ * 128:(tt_ + 1) * 128], wgu[0:17, :])
                ts(m4[:, tt_, :], pz[:], 0.0, None, ALU.min)
                stt(u4[:, tt_, :], m4[:, tt_, :], 2.0, pz[:], ALU.mult, ALU.subtract)
            act(u4[:, :, :], u4[:, :, :], AF.Exp)
            act(u4[:, :, :], u4[:, :, :], AF.Ln, bias=1.0)
            tt(la[:, :, :], m4[:, :, :], u4[:, :, :], ALU.subtract)
            for gc in range(8):
                pb = ps[gc % 2]
                for kc in range(8):
                    mm(pb[:], wg[:, kc, gc * 128:(gc + 1) * 128], hT[:, kc, :], start=(kc == 0), stop=(kc == 7))
                act(sgT[:, gc, :], pb[:], AF.Silu)
            for tt_ in range(4):
                mm(ps[3][:], mtri[:], la[:, tt_, :])
                for hd in range(4):
                    mm(ps[7][:, hd * 2:hd * 2 + 2], la[:, tt_, hd * 128:(hd + 1) * 128], ind[:])
                for kc in range(8):
                    mm(ps[4 + tt_ % 2][:], hT[:, kc, tt_ * 128:(tt_ + 1) * 128], wk[:, kc, :],
                       start=(kc == 0), stop=(kc == 7))
                act(expD, ps[3][:], AF.Exp)
                n0 = jb * 8 + tt_ * 2
                act(gam[:, :, n0:n0 + 2], ps[7][:, 0:8].rearrange("p (a b) -> p a b", b=2), AF.Exp)
                tt(kdec[:, tt_, :], ps[4 + tt_ % 2][:], expD, ALU.mult)
                for vh in range(2):
                    pb = ps[vh]
                    for kc in range(8):
                        mm(pb[:], hT[:, kc, tt_ * 128:(tt_ + 1) * 128], wv[:, kc, vh * 512:(vh + 1) * 512],
                           start=(kc == 0), stop=(kc == 7))
                    act(vv[:, tt_, vh * 512:(vh + 1) * 512], pb[:], AF.Copy)
            if jb == NB - 1 and after_proj is not None:
                after_proj()
            def kv_mm(n):
                tt_ = n // 2
                p0 = (n % 2) * 64
                kvb = (ps[0], ps[1]) if n % 2 == 0 else (ps[4], ps[5])
                for hd in range(4):
                    mm(kvb[hd // 2][:, (hd % 2) * 256:(hd % 2 + 1) * 256],
                       kdec[p0:p0 + 64, tt_, hd * 128:(hd + 1) * 128],
                       vv[p0:p0 + 64, tt_, hd * 256:(hd + 1) * 256])
            kv_mm(0)
            for n in range(8):
                ng = jb * 8 + n
                kvb = (ps[0], ps[1]) if n % 2 == 0 else (ps[4], ps[5])
                if n + 1 < 8:
                    kv_mm(n + 1)
                for hd in range(4):
                    stt(Sst[:, hd, :], Sst[:, hd, :], gam[:, hd, ng:ng + 1],
                        kvb[hd // 2][:, (hd % 2) * 256:(hd % 2 + 1) * 256], ALU.mult, ALU.add)
                    act(Sb[:, hd, :], Sst[:, hd, :], AF.Copy)
                    for dvc in range(2):
                        ch = hd * 2 + dvc
                        mm(ps[2 + n % 2][:, ch * 64:(ch + 1) * 64], Sb[:, hd, dvc * 128:(dvc + 1) * 128],
                           qT[:, hd, n * 64:(n + 1) * 64])
                if n >= 1:
                    cp(oT[:, :, (n - 1) * 64:n * 64], ps[2 + (n - 1) % 2][:].rearrange("p (a b) -> p a b", a=8),
                       eng="dve")
            cp(oT[:, :, 7 * 64:8 * 64], ps[2 + 7 % 2][:].rearrange("p (a b) -> p a b", a=8), eng="dve")
            for hd in range(4):
                act(sq2[:, 2 * hd:2 * hd + 2, :], oT[:, 2 * hd:2 * hd + 2, :], AF.Square)
                pn = ps[6 + hd % 2]
                for dvc in range(2):
                    mm(pn[:], ones_v[:], sq2[:, hd * 2 + dvc, :], start=(dvc == 0), stop=(dvc == 1))
                act(rstd4[:, hd, :], pn[:], AF.Ln, bias=EPS)
                act(rstd4[:, hd, :], rstd4[:, hd, :], AF.Exp, scale=-0.5)
            for hd in range(4):
                for dvc in range(2):
                    ch = hd * 2 + dvc
                    stt(tn[dvc], oT[:, ch, :], vcol(V_GNG + dvc), rstd4[:, hd, :], ALU.mult, ALU.mult)
                    tt(yT[:, ch, :], tn[dvc], sgT[:, ch, :], ALU.mult)
            if jb + 1 < NB:
                norm_a0(jb + 1)
            for m in range(8):
                if m == 4 and jb + 1 < NB:
                    norm_a(jb + 1, b, l, 0, do_sq=False)
                pb = ps[m % 2]
                for kc in range(8):
                    mm(pb[:], wout[:, kc, m * 128:(m + 1) * 128], yT[:, kc, :], start=(kc == 0), stop=(kc == 7))
                resid_update(pb[:], m, jb, ada_col(l, 2, m, b))

    def lru_views():
        return dict(
            wout=b16v(W, 0, [128, 8, 1024]),
            wgt=b16v(W, 24576, [128, 8, 1024]),
            wx=b16v(W, 40960, [128, 8, 1024]),
            wr=b16v(W, 57344, [128, 8, 256]),
            wi=b16v(W, 61440, [128, 8, 256]),
        )

    def lru_prefetch():
        v = lru_views()
        src = lru_w_in_d.rearrange("(c p) n -> p c n", p=128)
        dma("pool", v["wgt"], src[:, :, 0:1024])
        dma("pool", v["wx"], src[:, :, 1024:2048])
        dma("pool", v["wr"], lru_wr_d.rearrange("p (a b) -> p a b", a=8))
        dma("pool", v["wi"], lru_wi_d.rearrange("p (a b) -> p a b", a=8))

    hbr = smallw[:, 32:40]
    hbi = smallw[:, 40:48]
    cnh = smallw[:, 48:56]
    ts(hbr, vecs[:, V_BR:V_BR + 8], 0.5, None, ALU.mult)
    ts(hbi, vecs[:, V_BI:V_BI + 8], 0.5, None, ALU.mult)
    ts(cnh, cneg, 0.5, None, ALU.mult)

    def lru_layer(b, l=1, after_gate=None):
        V = lru_views()
        wout, wgt, wx, wr, wi = (V[k] for k in ("wout", "wgt", "wx", "wr", "wi"))
        dma("pool", wout, lru_w_out_d.rearrange("(c p) n -> p c n", p=128))
        hT = b16v(hbuf, 0, [128, 8, 512])
        yT = b16v(hbuf, 8192, [128, 8, 512])
        xb2 = [f32v(hbuf, 16384, [128, 2, 516]), f32v(hbuf, 27136, [128, 2, 516])]
        xc2 = [f32v(hbuf, 20992, [128, 2, 512]), cTr[:, 0:1024].rearrange("p (a b) -> p a b", a=2)]
        xcb = [b16v(hbuf, 25088, [128, 2, 512]),
               cTr[:, 1024:1536].bitcast(BF16).rearrange("p (a b) -> p a b", a=2)]
        TR = [f32v(scr, 0, [128, 2, 512]), f32v(W, 16384, [128, 2, 512])]
        TI = [f32v(scr, 4096, [128, 2, 512]), f32v(W, 20480, [128, 2, 512])]
        AA = [f32v(scr, 8192, [128, 2, 512]), f32v(W, 65536, [128, 2, 512])]
        MM = [f32v(scr, 12288, [128, 2, 512]), f32v(W, 69632, [128, 2, 512])]
        memset(halo[:], 0.0)
        memset(carry[:], 0.0)
        norm_a(0, b, l, 0)
        for jb in range(NB):
            t0 = jb * TB
            norm_b(jb, b, l, 0, 0, hT)
            def Gmm(hb):
                for cc in range(2):
                    c = hb * 2 + cc
                    for kc in range(8):
                        mm(ps[cc][:], wgt[:, kc, c * 128:(c + 1) * 128], hT[:, kc, :],
                           start=(kc == 0), stop=(kc == 7))

            def A1mm(hb):
                for cc in range(2):
                    c = hb * 2 + cc
                    pb = ps[2 + cc]
                    for kc in range(8):
                        mm(pb[:], wx[:, kc, c * 128:(c + 1) * 128], hT[:, kc, :], start=(kc == 0), stop=(kc == 7))

            def A1(hb):
                X, XC, XB = xb2[hb % 2], xc2[hb % 2], xcb[hb % 2]
                for cc in range(2):
                    c = hb * 2 + cc
                    act(X[:, cc, 0:3], halo[:, c, 0:3], AF.Copy)
                for cc in range(2):
                    act(X[:, cc, 3:515], ps[2 + cc][:], AF.Copy)
                for cc in range(2):
                    c = hb * 2 + cc
                    cp(halo[:, c, 0:3], X[:, cc, 512:515], eng="dve")
                    ts(XC[:, cc, :], X[:, cc, 0:512], vcol(V_CW + 0 * 8 + c), vcol(V_CB + c), ALU.mult, ALU.add)
                    for j in range(1, 4):
                        stt(XC[:, cc, :], X[:, cc, j:j + 512], vcol(V_CW + j * 8 + c), XC[:, cc, :],
                            ALU.mult, ALU.add)

            def A2(hb):
                X, XC, XB = xb2[hb % 2], xc2[hb % 2], xcb[hb % 2]
                for cc in range(2):
                    act(XB[:, cc, :], XC[:, cc, :], AF.Copy)
                for oc in range(2):
                    for kk in range(2):
                        mm(ps[4 + 2 * oc][:], wr[:, hb * 2 + kk, oc * 128:(oc + 1) * 128], XB[:, kk, :],
                           start=(kk == 0), stop=(kk == 1))
                    for kk in range(2):
                        mm(ps[5 + 2 * oc][:], wi[:, hb * 2 + kk, oc * 128:(oc + 1) * 128], XB[:, kk, :],
                           start=(kk == 0), stop=(kk == 1))

            def B1(hb):
                t_r, t_i, a_, m_ = TR[hb % 2], TI[hb % 2], AA[hb % 2], MM[hb % 2]
                for cc in range(2):
                    act(yT[:, hb * 2 + cc, :], ps[cc][:], AF.Gelu_apprx_tanh)
                for oc in range(2):
                    c = hb * 2 + oc
                    act(t_r[:, oc, :], ps[4 + 2 * oc][:], AF.Tanh, bias=hbr[:, c:c + 1], scale=0.5)
                    act(t_i[:, oc, :], ps[5 + 2 * oc][:], AF.Tanh, bias=hbi[:, c:c + 1], scale=0.5)
                for oc in range(2):
                    c = hb * 2 + oc
                    act(a_[:, oc, :], t_r[:, oc, :], AF.Exp, bias=cnh[:, c:c + 1], scale=cnh[:, c:c + 1])
                    act(m_[:, oc, :], t_r[:, oc, :], AF.Exp, bias=cneg[:, c:c + 1], scale=cneg[:, c:c + 1])
                for oc in range(2):
                    act(m_[:, oc, :], m_[:, oc, :], AF.Sqrt, bias=0.25, scale=-0.25)

            def B2(hb):
                t_r, t_i, a_, m_ = TR[hb % 2], TI[hb % 2], AA[hb % 2], MM[hb % 2]
                hs = t_r
                XC = xc2[hb % 2]
                for oc in range(2):
                    c = hb * 2 + oc
                    stt(t_i[:, oc, :], t_i[:, oc, :], 1.0, XC[:, oc, :], ALU.add, ALU.mult)
                    tt(m_[:, oc, :], m_[:, oc, :], t_i[:, oc, :], ALU.mult)
                    scan(hs[:, oc, :], a_[:, oc, :], m_[:, oc, :], carry[:, c:c + 1], ALU.mult, ALU.add)
                    cp(carry[:, c:c + 1], hs[:, oc, 511:512], eng="dve")
                    tt(yT[:, c, :], hs[:, oc, :], yT[:, c, :], ALU.mult)

            for kc in range(8):
                for cc in range(2):
                    mm(ps[cc][:], wgt[:, kc, cc * 128:(cc + 1) * 128], hT[:, kc, :], start=(kc == 0), stop=(kc == 7))
                    mm(ps[2 + cc][:], wx[:, kc, cc * 128:(cc + 1) * 128], hT[:, kc, :],
                       start=(kc == 0), stop=(kc == 7))
            A1(0); A1mm(1); A2(0)
            for hb in range(4):
                if hb + 1 < 4:
                    A1(hb + 1)
                if hb + 2 < 4:
                    A1mm(hb + 2)
                B1(hb)
                if hb + 1 < 4:
                    Gmm(hb + 1)
                    A2(hb + 1)
                elif jb == NB - 1 and after_gate is not None:
                    after_gate()
                B2(hb)
            if jb + 1 < NB:
                norm_a0(jb + 1)
            for m in range(8):
                if m == 4 and jb + 1 < NB:
                    norm_a(jb + 1, b, l, 0, do_sq=False)
                pb = ps[m % 2]
                for kc in range(8):
                    mm(pb[:], wout[:, kc, m * 128:(m + 1) * 128], yT[:, kc, :], start=(kc == 0), stop=(kc == 7))
                resid_update(pb[:], m, jb, ada_col(l, 2, m, b))

    def moe_wviews(slot):
        base = slot * 24576
        return (b16v(W, base, [128, 8, 512]), b16v(W, base + 8192, [128, 8, 512]),
                b16v(W, base + 16384, [128, 4, 1024]))

    moe_state = {"slot": 0}

    def moe_load(l, e, slot, parts=("g", "u", "d")):
        wg, wu, wd = moe_wviews(slot)
        if "g" in parts:
            dma("pool", wg, wg_d[l, e].rearrange("(c p) n -> p c n", p=128))
        if "u" in parts:
            dma("pool", wu, wu_d[l, e].rearrange("(c p) n -> p c n", p=128))
        if "d" in parts:
            dma("pool", wd, wd_d[l, e].rearrange("(c p) n -> p c n", p=128))

    SLOTS = [1, 2, 0, 1, 2, 0, 1, 2, 0, 1, 2, 0, 1, 2, 1, 0]

    def moe_layer(b, l, pre_last=None):
        hT = b16v(hbuf, 0, [128, 8, S])
        for jb in range(NB):
            norm_block(jb, b, l, 1, 3, hT[:, :, jb * TB:(jb + 1) * TB])
        for t in range(16):
            for kc in range(8):
                mm(ps[6][:, t * 16:(t + 1) * 16], hT[:, kc, t * 128:(t + 1) * 128], rw[:, kc, :],
                   start=(kc == 0), stop=(kc == 7))
        R = lambda i: rt[:, i * 256:(i + 1) * 256]
        R3 = lambda i: rt[:, i * 256:(i + 1) * 256].rearrange("p (t e) -> p t e", e=16)
        R4 = lambda i: rt[:, i * 256:(i + 1) * 256].rearrange("p (t g j) -> p t g j", g=4, j=4)
        G = lambda i: rt[:, 2304 + i * 64:2304 + (i + 1) * 64]
        G3 = lambda i: rt[:, 2304 + i * 64:2304 + (i + 1) * 64].rearrange("p (t g) -> p t g", g=4)
        sc_, sel_, msk, tmp_ = 0, 1, 2, 3
        act(R(sc_), ps[6][:, 0:256], AF.Sigmoid)
        tt(R3(sel_), R3(sc_), rb[:, :].unsqueeze(1).to_broadcast([128, 16, 16]), ALU.add)
        red(G(0), R4(sel_), ALU.max)
        tt(R4(msk), R4(sel_), G3(0).unsqueeze(3).to_broadcast([128, 16, 4, 4]), ALU.is_ge)
        stt(R(tmp_), R(msk), -1e30, R(sel_), ALU.mult, ALU.add)
        red(G(1), R4(tmp_), ALU.max)
        tt(G(2), G(0), G(1), ALU.add)
        red(rt[:, 1664:1680], G3(2), ALU.max)
        tt(G3(3), G3(2), rt[:, 1664:1680].unsqueeze(2).to_broadcast([128, 16, 4]), ALU.is_ge)
        tt(R4(msk), R4(sel_), G3(1).unsqueeze(3).to_broadcast([128, 16, 4, 4]), ALU.is_ge)
        tt(R4(msk), R4(msk), G3(3).unsqueeze(3).to_broadcast([128, 16, 4, 4]), ALU.mult)
        tt(R(tmp_), R(msk), R(sc_), ALU.mult)
        red(rt[:, 1680:1696], R3(tmp_), ALU.add)
        recip(rt[:, 1680:1696], rt[:, 1680:1696])
        tt(R3(tmp_), R3(tmp_), rt[:, 1680:1696].unsqueeze(2).to_broadcast([128, 16, 16]), ALU.mult)
        pk = rt[:, 1024:1536].rearrange("p (t e) -> p t e", e=32)
        hib = rt[:, 1536:1664].bitcast(BF16).rearrange("p (t e) -> p t e", e=16)
        cp(hib, R3(tmp_), eng="dve")
        cp(pk[:, :, 0:16], hib, eng="dve")
        tt(pk[:, :, 16:32], R3(tmp_), pk[:, :, 0:16], ALU.subtract)
        combT = cTr[0:32, 0:1024].bitcast(BF16)
        for t4 in range(4):
            for q in range(4):
                t = t4 * 4 + q
                tr(ps[7][0:32, q * 128:(q + 1) * 128], pk[:, t, :], ident[:])
            act(combT[:, t4 * 512:(t4 + 1) * 512], ps[7][0:32, :], AF.Copy)
        if "comb" in dbg_d and b == dbg.get("_b", 0) and l == dbg.get("_l", 0):
            dma("sp", dbg_d["comb"], combT)
        he = [b16v(scr, 0, [128, 4, 512]), b16v(scr, 4096, [128, 4, 512])]
        sg = [f32v(scr, 8192, [128, 512]), f32v(scr, 10240, [128, 512])]
        tb = [f32v(scr, 12288, [128, 512]), f32v(scr, 14336, [128, 512])]
        cbs = [f32v(scr, 16384, [128, 512]), f32v(scr, 18432, [128, 512])]
        units = [(e, jb) for e in range(NE) for jb in range(NB)]
        DB = (4, 5, 7)

        def cb_and_gu0(i):
            e, jb = units[i]
            t0 = jb * TB
            wg, wu, wd = moe_wviews(SLOTS[e])
            mm(ps[6][:], i32b[:, e:e + 1].to_broadcast([32, 128]), combT[:, t0:t0 + TB])
            act(cbs[i % 2], ps[6][:], AF.Copy)
            gu_mm(i, 0)

        def gu_mm(i, c):
            e, jb = units[i]
            t0 = jb * TB
            wg, wu, wd = moe_wviews(SLOTS[e])
            pg = ps[0 + c % 2]
            pu = ps[2 + c % 2]
            for kc in range(8):
                mm(pg[:], wg[:, kc, c * 128:(c + 1) * 128], hT[:, kc, t0:t0 + TB],
                   start=(kc == 0), stop=(kc == 7))
            for kc in range(8):
                mm(pu[:], wu[:, kc, c * 128:(c + 1) * 128], hT[:, kc, t0:t0 + TB],
                   start=(kc == 0), stop=(kc == 7))

        def load_hook(i):
            e, jb = units[i]
            if jb == 0:
                if e + 1 < NE:
                    moe_load(l, e + 1, SLOTS[e + 1])
                elif pre_last is not None:
                    pre_last()

        load_hook(0)
        cb_and_gu0(0)
        for i, (e, jb) in enumerate(units):
            wg, wu, wd = moe_wviews(SLOTS[e])
            k2 = i % 2
            for c in range(4):
                if c >= 1:
                    gu_mm(i, c)
                pg = ps[0 + c % 2]
                pu = ps[2 + c % 2]
                act(sg[c % 2], pg[:], AF.Silu)
                tt(tb[c % 2], pu[:], sg[c % 2], ALU.mult)
                tt(he[k2][:, c, :], tb[c % 2], cbs[k2], ALU.mult)
            if i + 1 < len(units):
                cb_and_gu0(i + 1)
            for m in range(8):
                pd = ps[DB[m % 3]]
                for kc in range(4):
                    mm(pd[:], wd[:, kc, m * 128:(m + 1) * 128], he[k2][:, kc, :],
                       start=(kc == 0), stop=(kc == 3))
                resid_update(pd[:], m, jb, ada_col(l, 5, m, b))
            if i + 1 < len(units):
                load_hook(i + 1)

    def final_store(b, si):
        sq = b16v(scr, 0, [128, 8, 512])
        rstd = f32v(scr, 8192, [128, 512])
        ob = [f32v(hbuf, 0, [128, 8, 512]), f32v(hbuf, 16384, [128, 8, 512])]
        outs = []
        for jb in range(NB):
            t0 = jb * TB
            act(sq[:, :, :], xT[:, :, t0:t0 + TB], AF.Square)
            for c in range(8):
                mm(ps[6][:], ones_d[:], sq[:, c, :], start=(c == 0), stop=(c == 7))
            act(rstd, ps[6][:], AF.Ln, bias=EPS)
            act(rstd, rstd, AF.Exp, scale=-0.5)
            o = ob[jb % 2]
            for c in range(8):
                stt(o[:, c, :], xT[:, c, t0:t0 + TB], vcol(V_FNG + c), rstd, ALU.mult, ALU.mult)
            outs.append(dma("sp", outT_d[si, :, t0:t0 + TB].rearrange("(c p) t -> p c t", p=128), o))
        return outs

    out_ops = []
    phases = dbg.get("_phases", ("gla", "moe0", "lru", "moe1", "final"))
    for si in range(n_seq):
        b = si
        for jb in range(NB):
            dma("sp", xT[:, :, jb * TB:(jb + 1) * TB],
                xT_d[si, :, jb * TB:(jb + 1) * TB].rearrange("(c p) t -> p c t", p=128))
        if si == 0 and "gla" in phases:
            gla_prefetch()
        if "gla" in phases:
            gla_layer(b, after_proj=(lambda: moe_load(0, 0, SLOTS[0])) if "moe0" in phases else None)
        if "xmid0" in dbg_d and si == dbg.get("_b", 0):
            out_ops.append(dma("sp", dbg_d["xmid0"].rearrange("(c p) t -> p c t", p=128), xT[:, :, :]))
        if "moe0" in phases:
            if "gla" not in phases:
                moe_load(0, 0, SLOTS[0])
            moe_layer(b, 0, pre_last=lru_prefetch if "lru" in phases else None)
        if "xmid1" in dbg_d and si == dbg.get("_b", 0):
            out_ops.append(dma("sp", dbg_d["xmid1"].rearrange("(c p) t -> p c t", p=128), xT[:, :, :]))
        if "lru" in phases:
            if "moe0" not in phases:
                lru_prefetch()
            lru_layer(b, after_gate=(lambda: moe_load(1, 0, SLOTS[0], ("g", "u"))) if "moe1" in phases else None)
            if "moe1" in phases:
                moe_load(1, 0, SLOTS[0], ("d",))
        if "xmid2" in dbg_d and si == dbg.get("_b", 0):
            out_ops.append(dma("sp", dbg_d["xmid2"].rearrange("(c p) t -> p c t", p=128), xT[:, :, :]))
        if "moe1" in phases:
            if "lru" not in phases:
                moe_load(1, 0, SLOTS[0])
            moe_layer(b, 1, pre_last=gla_prefetch if (si + 1 < n_seq and "gla" in phases) else None)
        if "final" in phases:
            out_ops += final_store(b, si)

    Sd.add("sp", lambda e: e.nop(), reads=[], writes=[]).deps.extend(
        [o for o in Sd.dma_ops["sp"]])

    Sd.finalize()

    sem_stack = contextlib.ExitStack()
    csem = {e: sem_stack.enter_context(nc.semaphore("c_" + e)) for e in Sched.ENGS}
    dsems = {e: [sem_stack.enter_context(nc.semaphore("d_%s_%d" % (e, i))) for i in range(Sched.NDS)]
             for e in ("sp", "pool")}
    with nc.Block() as block:
        @block.tensor
        def _(e):
            Sd.emit("pe", e, csem, dsems)

        @block.scalar
        def _(e):
            Sd.emit("act", e, csem, dsems)

        @block.vector
        def _(e):
            Sd.emit("dve", e, csem, dsems)

        @block.gpsimd
        def _(e):
            Sd.emit("pool", e, csem, dsems)

        @block.sync
        def _(e):
            Sd.emit("sp", e, csem, dsems)
    sem_stack.close()
    stack.close()
    return nc, Sd


V_NMG = 0
V_NFG = 16
V_FNG = 32
V_GNG = 40
V_CW = 42
V_CB = 74
V_BR = 82
V_BI = 90
V_LAM = 98
NV = 106


def _fm(v):
    v = np.asarray(v, np.float32).reshape(-1, 128)
    return np.ascontiguousarray(v.T)


def prep_inputs(inp):
    f = lambda a: np.ascontiguousarray(np.asarray(a, dtype=np.float32))
    x = f(inp["x"])
    c = f(inp["c"])
    vec = np.zeros((128, NV), np.float32)
    for l in range(2):
        vec[:, V_NMG + l * 8:V_NMG + (l + 1) * 8] = _fm(inp["norm_mix_g"][l])
        vec[:, V_NFG + l * 8:V_NFG + (l + 1) * 8] = _fm(inp["norm_ffn_g"][l])
    vec[:, V_FNG:V_FNG + 8] = _fm(inp["final_norm_g"])
    vec[:, V_GNG:V_GNG + 2] = _fm(inp["gla_norm_g"][0])
    for j in range(4):
        vec[:, V_CW + j * 8:V_CW + (j + 1) * 8] = _fm(inp["lru_conv_w"][0, j])
    vec[:, V_CB:V_CB + 8] = _fm(inp["lru_conv_b"][0])
    vec[:, V_BR:V_BR + 8] = _fm(np.asarray(inp["lru_b_r"][0]).reshape(-1))
    vec[:, V_BI:V_BI + 8] = _fm(np.asarray(inp["lru_b_i"][0]).reshape(-1))
    vec[:, V_LAM:V_LAM + 8] = _fm(inp["lru_lambda"][0])
    ada_b = np.concatenate([_fm(inp["ada_b"][0]), _fm(inp["ada_b"][1])], axis=1)
    wgu = np.concatenate([f(inp["gla_w_gate_up"][0]), f(inp["gla_b_gate"][0])[None, :]], axis=0)

    def blk(w):
        w = f(w).reshape(4, 2, 128, 256).transpose(2, 0, 1, 3)
        return np.ascontiguousarray(w).reshape(128, 8 * 256)

    rwl = f(inp["router_w"]).reshape(8, 128, 16).transpose(1, 0, 2).reshape(128, 128)
    shared = {
        "ada_w": f(inp["ada_w"]), "ada_b": ada_b, "vecs": vec,
        "gla_w_in": f(inp["gla_w_in"][0]), "gla_w_out": f(inp["gla_w_out"][0]), "wgu": wgu,
        "lru_w_in": f(inp["lru_w_in"][0]), "lru_w_out": f(inp["lru_w_out"][0]),
        "lru_wr": blk(inp["lru_w_r"][0]), "lru_wi": blk(inp["lru_w_i"][0]),
        "rw": np.ascontiguousarray(rwl), "rb": np.ascontiguousarray(np.tile(f(inp["router_bias"])[None, :], (128, 1))),
        "moe_wg": f(inp["moe_w_gate"]), "moe_wu": f(inp["moe_w_up"]), "moe_wd": f(inp["moe_w_down"]),
        "ident": np.eye(128, dtype=np.float32),
    }
    s_i = np.arange(128)[:, None]
    c_i = np.arange(128)[None, :]
    shared["mtri"] = (((s_i > c_i) & (s_i // 64 == c_i // 64)).astype(np.float32) / 16.0)
    shared["ind"] = ((s_i // 64) == np.arange(2)[None, :]).astype(np.float32) / 16.0
    shared["i16"] = np.eye(16, dtype=np.float32)
    in_maps = []
    for i in range(N_CORES):
        m = dict(shared)
        m["xT"] = np.ascontiguousarray(x[2 * i:2 * i + 2].transpose(0, 2, 1))
        cc = c[2 * i:2 * i + 2]
        m["cT"] = np.ascontiguousarray(cc.reshape(2, 8, 128).transpose(2, 1, 0).reshape(128, 16))
        in_maps.append(m)
    return in_maps


_CACHE = {}


def kernel(**inputs):
    in_maps = prep_inputs(inputs)
    if "nc" not in _CACHE:
        _CACHE["nc"] = build_program()[0]
    nc = _CACHE["nc"]
    res = run_bass_kernel_spmd(nc, in_maps, core_ids=list(range(N_CORES)))
    out = np.empty((16, S, D), np.float32)
    for i in range(N_CORES):
        o = res.results[i]["outT"]
        out[2 * i:2 * i + 2] = o.transpose(0, 2, 1)
    return out
```

```python
import numpy as np
import concourse.bass as bass
import concourse.mybir as mybir
from concourse.bass_utils import run_bass_kernel_spmd

F32 = mybir.dt.float32
BF16 = mybir.dt.bfloat16
AF = mybir.ActivationFunctionType
ALU = mybir.AluOpType
ESZ = {F32: 4, BF16: 2}

D = 1024
S = 2048
NB = 4
TB = 512
NE = 16
DE = 512
EPS = 1e-6
GRAN = 512
N_CORES = 8


class Op:
    __slots__ = ("eng", "idx", "fn", "deps", "dma", "dsem", "dval", "sig", "sigval", "waits", "gidx")


class Sched:
    ENGS = ("pe", "act", "dve", "pool", "sp")
    NDS = 12

    def __init__(self, nc):
        self.nc = nc
        self.ops = {e: [] for e in self.ENGS}
        self.last_w = {}
        self.readers = {}
        self.ndma = {e: 0 for e in self.ENGS}
        self.dma_ops = {e: [] for e in self.ENGS}
        self.tok_cache = {}
        self.gcount = 0

    def tokens(self, ap):
        sp = str(ap.space)
        if "SB" not in sp and "PSUM" not in sp:
            return ()
        if "PSUM" in sp:
            return ((ap.tensor.name, 0),)
        key = (ap.tensor.name, ap.offset, ap.ap, ap.dtype)
        t = self.tok_cache.get(key)
        if t is None:
            es = ESZ[ap.dtype]
            pstride = ap.ap[0][0]
            off = ap.offset % pstride if pstride > 0 else ap.offset
            name = ap.tensor.name
            dims = [(abs(st), cnt) for st, cnt in ap.ap[1:] if cnt > 1 and st != 0]
            dims.sort(reverse=True)
            outer = dims[:-1] if dims else []
            n_outer = 1
            for _, cnt in outer:
                n_outer *= cnt
            gs = set()
            if dims and n_outer <= 512:
                lst, lcnt = dims[-1]
                bases = [off]
                for st, cnt in outer:
                    bases = [b0 + i * st for b0 in bases for i in range(cnt)]
                for b0 in bases:
                    lo = b0 * es
                    hi = (b0 + (lcnt - 1) * lst + 1) * es
                    gs.update(range(lo // GRAN, (hi - 1) // GRAN + 1))
            else:
                span = 0
                for st, cnt in dims:
                    span += (cnt - 1) * st
                lo = off * es
                hi = (off + span + 1) * es
                gs.update(range(lo // GRAN, (hi - 1) // GRAN + 1))
            t = tuple((name, g) for g in sorted(gs))
            self.tok_cache[key] = t
        return t

    def add(self, eng, fn, reads=(), writes=(), dma=False):
        op = Op()
        op.eng = eng
        op.idx = len(self.ops[eng])
        op.fn = fn
        op.dma = dma
        op.sig = False
        op.sigval = 0
        op.gidx = self.gcount
        self.gcount += 1
        deps = {}
        rt = []
        for ap in reads:
            rt.extend(self.tokens(ap))
        wt = []
        for ap in writes:
            wt.extend(self.tokens(ap))
        for r in rt:
            w = self.last_w.get(r)
            if w is not None:
                deps[id(w)] = w
        for r in wt:
            w = self.last_w.get(r)
            if w is not None:
                deps[id(w)] = w
            rd = self.readers.get(r)
            if rd:
                for o in rd.values():
                    deps[id(o)] = o
        deps.pop(id(op), None)
        op.deps = list(deps.values())
        for r in rt:
            d = self.readers.get(r)
            if d is None:
                d = {}
                self.readers[r] = d
            if dma:
                d[(eng, op.idx)] = op
            else:
                d[eng] = op
        for r in wt:
            self.last_w[r] = op
            self.readers[r] = {}
        if dma:
            i = self.ndma[eng]
            self.ndma[eng] = i + 1
            op.dsem = i % self.NDS
            op.dval = 16 * (i // self.NDS + 1)
            if i >= self.NDS:
                op.deps.append(self.dma_ops[eng][i - self.NDS])
            self.dma_ops[eng].append(op)
        self.ops[eng].append(op)
        return op

    def finalize(self):
        for eng in self.ENGS:
            known = {}
            for op in self.ops[eng]:
                need = {}
                for d in op.deps:
                    if d.dma:
                        k = ("d", d.eng, d.dsem)
                        v = d.dval
                        if v > need.get(k, (0, None))[0]:
                            need[k] = (v, d)
                    else:
                        if d.eng == "pe" and eng == "pe" and not op.dma:
                            continue
                        k = ("c", d.eng)
                        v = d.idx + 1
                        if v > need.get(k, (0, None))[0]:
                            need[k] = (v, d)
                waits = []
                for k, (v, d) in need.items():
                    if known.get(k, 0) >= v:
                        continue
                    known[k] = v
                    waits.append(d)
                    if not d.dma:
                        d.sig = True
                op.waits = waits
        for eng in self.ENGS:
            c = 0
            for op in self.ops[eng]:
                if op.sig and not op.dma:
                    c += 1
                    op.sigval = c

    def emit(self, eng, e, csem, dsems):
        for op in self.ops[eng]:
            for d in op.waits:
                if d.dma:
                    e.wait_ge(dsems[d.eng][d.dsem], d.dval)
                else:
                    e.wait_ge(csem[d.eng], d.sigval)
            ins = op.fn(e)
            if op.dma:
                ins.then_inc(dsems[eng][op.dsem], 16)
            elif op.sig:
                ins.then_inc(csem[eng], 1)


def build_program(n_seq=2, dbg=None):
    dbg = dbg or {}
    nc = bass.Bass("TRN2", target_bir_lowering=False)
    Sd = Sched(nc)

    def din(name, shape):
        return nc.dram_tensor(name, list(shape), F32, kind="ExternalInput").ap()

    xT_d = din("xT", [2, D, S])
    cT_d = din("cT", [128, 16])
    ada_w_d = din("ada_w", [2, D, 6 * D])
    ada_b_d = din("ada_b", [128, 96])
    vecs_d = din("vecs", [128, NV])
    gla_w_in_d = din("gla_w_in", [D, 3088])
    gla_w_out_d = din("gla_w_out", [D, D])
    wgu_d = din("wgu", [17, 512])
    lru_w_in_d = din("lru_w_in", [D, 2 * D])
    lru_w_out_d = din("lru_w_out", [D, D])
    lru_wr_d = din("lru_wr", [128, 8 * 256])
    lru_wi_d = din("lru_wi", [128, 8 * 256])
    rw_d = din("rw", [128, 8 * 16])
    rb_d = din("rb", [128, 16])
    wg_d = din("moe_wg", [2, NE, D, DE])
    wu_d = din("moe_wu", [2, NE, D, DE])
    wd_d = din("moe_wd", [2, NE, DE, D])
    ident_d = din("ident", [128, 128])
    mtri_d = din("mtri", [128, 128])
    ind_d = din("ind", [128, 2])
    i16_d = din("i16", [16, 16])
    outT_d = nc.dram_tensor("outT", [2, D, S], F32, kind="ExternalOutput").ap()
    dbg_d = {}
    for k, shp in dbg.items():
        if k.startswith("_"):
            continue
        dbg_d[k] = nc.dram_tensor("dbg_" + k, list(shp), F32, kind="ExternalOutput").ap()

    import contextlib
    stack = contextlib.ExitStack()

    def SB(name, shape, dt=F32):
        return stack.enter_context(nc.sbuf_tensor("s_" + name, list(shape), dt))

    def PS(name):
        return stack.enter_context(nc.psum_tensor(name, [128, 512], F32))

    xT = SB("xT", [128, 8, S])
    hbuf = SB("hbuf", [128, 16384], BF16)
    W = SB("W", [128, 36864], BF16)
    scr = SB("scr", [128, 10240], BF16)
    cTr = SB("cTr", [128, 2048])
    ident = SB("ident", [128, 128])
    mtri = SB("mtri", [128, 128])
    ind = SB("ind", [128, 2])
    i32f = SB("i32f", [32, 16])
    i32b = SB("i32b", [32, 16], BF16)
    ones_d = SB("ones_d", [128, 128], BF16)
    ones_v = SB("ones_v", [128, 128], BF16)
    vecs = SB("vecs", [128, NV])
    cT_s = SB("cT_s", [128, 16])
    cond = SB("cond", [128, 16], BF16)
    ada_b = SB("ada_b", [128, 96])
    ada = SB("ada", [128, 2, 48, 2])
    mod = SB("mod", [128, 2, 4, 8, 2])
    rw = SB("rw", [128, 8, 16], BF16)
    rb = SB("rb", [128, 16])
    wgu = SB("wgu", [32, 512])
    smallw = SB("smallw", [128, 256])
    halo = SB("halo", [128, 8, 4])
    carry = SB("carry", [128, 8])
    rt = scr[:, 0:5120].bitcast(F32)

    ps = [PS("ps%d" % i) for i in range(8)]

    def mm(out, lhsT, rhs, start=True, stop=True):
        return Sd.add("pe", lambda e: e.matmul(out, lhsT, rhs, start=start, stop=stop),
                      reads=[lhsT, rhs], writes=[out])

    def tr(out, in_, idn):
        return Sd.add("pe", lambda e: e.transpose(out, in_, idn), reads=[in_, idn], writes=[out])

    def act(out, in_, func, bias=None, scale=None, eng="act"):
        rd = [in_]
        kw = {}
        if bias is not None:
            kw["bias"] = bias
            if not isinstance(bias, (int, float)):
                rd.append(bias)
        if scale is not None:
            kw["scale"] = scale
            if not isinstance(scale, (int, float)):
                rd.append(scale)
        return Sd.add("act", lambda e: e.activation(out, in_, func, **kw), reads=rd, writes=[out])

    def stt(out, in0, scalar, in1, op0, op1, eng="dve"):
        rd = [in0, in1]
        if not isinstance(scalar, (int, float)):
            rd.append(scalar)
        return Sd.add(eng, lambda e: e.scalar_tensor_tensor(out, in0, scalar, in1, op0, op1),
                      reads=rd, writes=[out])

    def ts(out, in0, s1, s2, op0, op1=None, eng="dve"):
        rd = [in0]
        for s in (s1, s2):
            if s is not None and not isinstance(s, (int, float)):
                rd.append(s)
        if op1 is None:
            return Sd.add(eng, lambda e: e.tensor_scalar(out, in0, s1, None, op0), reads=rd, writes=[out])
        return Sd.add(eng, lambda e: e.tensor_scalar(out, in0, s1, s2, op0, op1), reads=rd, writes=[out])

    def tt(out, in0, in1, op, eng="dve"):
        return Sd.add(eng, lambda e: e.tensor_tensor(out, in0, in1, op), reads=[in0, in1], writes=[out])

    def cp(out, in_, eng="dve"):
        return Sd.add(eng, lambda e: e.tensor_copy(out, in_), reads=[in_], writes=[out])

    def recip(out, in_):
        return Sd.add("dve", lambda e: e.reciprocal(out, in_), reads=[in_], writes=[out])

    def red(out, in_, op, eng="dve"):
        return Sd.add(eng, lambda e: e.tensor_reduce(out, in_, mybir.AxisListType.X, op),
                      reads=[in_], writes=[out])

    def scan(out, d0, d1, init, op0, op1):
        rd = [d0, d1]
        if not isinstance(init, (int, float)):
            rd.append(init)
        return Sd.add("dve", lambda e: e.tensor_tensor_scan(out, d0, d1, init, op0, op1),
                      reads=rd, writes=[out])

    def memset(ap, val, eng="dve"):
        return Sd.add(eng, lambda e: e.memset(ap, val), writes=[ap])

    def dma(q, out, in_, **kw):
        return Sd.add(q, lambda e: e.dma_start(out=out, in_=in_, **kw), reads=[in_], writes=[out], dma=True)

    def f32v(t, off_b, shape):
        n = int(np.prod(shape[1:]))
        v = t[0:shape[0], off_b // 2: off_b // 2 + 2 * n].bitcast(F32)
        if len(shape) == 3:
            v = v.rearrange("p (a b) -> p a b", a=shape[1])
        elif len(shape) == 4:
            v = v.rearrange("p (a b c) -> p a b c", a=shape[1], b=shape[2])
        return v

    def b16v(t, off_b, shape):
        n = int(np.prod(shape[1:]))
        v = t[0:shape[0], off_b // 2: off_b // 2 + n]
        if len(shape) == 3:
            v = v.rearrange("p (a b) -> p a b", a=shape[1])
        elif len(shape) == 4:
            v = v.rearrange("p (a b c) -> p a b c", a=shape[1], b=shape[2])
        return v

    def vcol(i):
        return vecs[:, i:i + 1]

    dma("sp", ident[:], ident_d)
    dma("sp", mtri[:], mtri_d)
    dma("sp", ind[:], ind_d)
    dma("sp", i32f[0:16, :], i16_d)
    dma("sp", i32f[16:32, :], i16_d)
    cp(i32b[:], i32f[:], eng="dve")
    dma("sp", vecs[:], vecs_d)
    dma("sp", cT_s[:], cT_d)
    dma("sp", ada_b[:], ada_b_d)
    dma("sp", rb[:], rb_d)
    dma("sp", wgu[0:17, :], wgu_d)
    dma("pool", rw[:].rearrange("p a b -> p (a b)"), rw_d)
    memset(ones_d[:], 1.0 / 1024.0)
    memset(ones_v[:], 1.0 / 256.0)
    act(cond[:], cT_s[:], AF.Silu)

    NPIECE = 12
    for l in range(2):
        for pc in range(NPIECE):
            slot = (l * NPIECE + pc) % 3
            wv = b16v(hbuf, slot * 8192, [128, 8, 512])
            dma("pool", wv, ada_w_d[l, :, pc * 512:(pc + 1) * 512].rearrange("(c p) n -> p c n", p=128))
            for oc4 in range(4):
                oc = pc * 4 + oc4
                for kc in range(8):
                    mm(ps[7][:, (l * 48 + oc) * 2:(l * 48 + oc) * 2 + 2],
                       wv[:, kc, oc4 * 128:(oc4 + 1) * 128],
                       cond[:, kc * 2:kc * 2 + 2], start=(kc == 0), stop=(kc == 7))
    for l in range(2):
        tt(ada[:, l, :, :], ps[7][:, l * 96:(l + 1) * 96].rearrange("p (a b) -> p a b", b=2),
           ada_b[:, l * 48:(l + 1) * 48].unsqueeze(2).to_broadcast([128, 48, 2]), ALU.add)
    for l in range(2):
        for j, (which, gcol) in enumerate(((1, V_NMG + l * 8), (4, V_NFG + l * 8))):
            ts(mod[:, l, j, :, :], ada[:, l, which * 8:(which + 1) * 8, :], 1.0, None, ALU.add)
            tt(mod[:, l, j, :, :], mod[:, l, j, :, :],
               vecs[:, gcol:gcol + 8].unsqueeze(2).to_broadcast([128, 8, 2]), ALU.mult)

    def A_of(l, j, c, b):
        return mod[:, l, j, c, b:b + 1]

    def ada_col(l, which, c, b):
        return ada[:, l, which * 8 + c, b:b + 1]

    lam = vecs[:, V_LAM:V_LAM + 8]
    sw_a = smallw[:, 0:8]
    sw_b = smallw[:, 8:16]
    cneg = smallw[:, 16:24]
    cneg2 = smallw[:, 24:32]
    ts(sw_b, lam, 0.0, None, ALU.min)
    stt(sw_a, sw_b, 2.0, lam, ALU.mult, ALU.subtract)
    act(sw_a, sw_a, AF.Exp)
    act(sw_a, sw_a, AF.Ln, bias=1.0)
    tt(sw_a, sw_a, sw_b, ALU.subtract)
    ts(cneg, sw_a, -8.0, None, ALU.mult)
    ts(cneg2, sw_a, -16.0, None, ALU.mult)

    def norm_a0(jb):
        t0 = jb * TB
        sq = b16v(scr, 0, [128, 8, 512])
        act(sq[:, :, :], xT[:, :, t0:t0 + TB], AF.Square)

    def norm_a(jb, b, l, j, npre=2, do_sq=True):
        t0 = jb * TB
        sq = b16v(scr, 0, [128, 8, 512])
        rstd = f32v(scr, 8192, [128, 512])
        tmp = [f32v(scr, 10240, [128, 512]), f32v(scr, 12288, [128, 512])]
        if do_sq:
            norm_a0(jb)
        for c in range(8):
            mm(ps[6][:], ones_d[:], sq[:, c, :], start=(c == 0), stop=(c == 7))
        act(rstd, ps[6][:], AF.Ln, bias=EPS)
        act(rstd, rstd, AF.Exp, scale=-0.5)
        for c in range(npre):
            stt(tmp[c % 2], xT[:, c, t0:t0 + TB], A_of(l, j, c, b), rstd, ALU.mult, ALU.mult)

    def norm_b(jb, b, l, j, shift_which, dst, npre=2):
        t0 = jb * TB
        rstd = f32v(scr, 8192, [128, 512])
        tmp = [f32v(scr, 10240, [128, 512]), f32v(scr, 12288, [128, 512])]
        for c in range(8):
            tm = tmp[c % 2]
            if c >= npre:
                stt(tm, xT[:, c, t0:t0 + TB], A_of(l, j, c, b), rstd, ALU.mult, ALU.mult)
            act(dst[:, c, :], tm, AF.Identity, bias=ada_col(l, shift_which, c, b))

    def norm_block(jb, b, l, j, shift_which, dst):
        norm_a(jb, b, l, j, npre=0)
        norm_b(jb, b, l, j, shift_which, dst, npre=0)

    def resid_update(pbank, m, jb, gcol):
        t0 = jb * TB
        stt(xT[:, m, t0:t0 + TB], pbank, gcol, xT[:, m, t0:t0 + TB], ALU.mult, ALU.add)

    wa = SB("wa", [128, 8, 128], BF16)
    memset(wa[:], 0.0)

    def gla_views():
        return dict(
            wout=b16v(W, 0, [128, 8, 1024]),
            Sst=f32v(W, 16384, [128, 4, 256]),
            Sb=b16v(W, 20480, [128, 4, 256]),
            gam=f32v(W, 22528, [128, 4, 32]),
            wq=b16v(W, 24576, [128, 8, 512]),
            wk=b16v(W, 32768, [128, 8, 512]),
            wv=b16v(W, 40960, [128, 8, 1024]),
            wg=b16v(W, 57344, [128, 8, 1024]),
        )

    def gla_prefetch():
        v = gla_views()
        src = gla_w_in_d.rearrange("(c p) n -> p c n", p=128)
        dma("pool", wa[:, :, 0:16], src[:, :, 3072:3088])
        dma("pool", v["wq"], src[:, :, 0:512])
        dma("pool", v["wg"], src[:, :, 2048:3072])
        dma("pool", v["wk"], src[:, :, 512:1024])
        dma("pool", v["wv"], src[:, :, 1024:2048])

    def gla_layer(b, l=0, after_proj=None):
        V = gla_views()
        wout, Sst, Sb, gam, wq, wk, wv, wg = (V[k] for k in ("wout", "Sst", "Sb", "gam", "wq", "wk", "wv", "wg"))
        dma("pool", wout, gla_w_out_d.rearrange("(c p) n -> p c n", p=128))
        memset(Sst, 0.0)
        hT = b16v(hbuf, 0, [128, 8, 512])
        qT = b16v(hbuf, 8192, [128, 4, 512])
        kdec = b16v(hbuf, 12288, [128, 4, 512])
        vv = b16v(hbuf, 16384, [128, 4, 1024])
        sgT = b16v(hbuf, 24576, [128, 8, 512])
        oT = f32v(scr, 0, [128, 8, 512])
        m4 = f32v(scr, 0, [128, 4, 512])
        u4 = f32v(scr, 8192, [128, 4, 512])
        alrT = f32v(scr, 16384, [32, 512])
        expD = f32v(scr, 18432, [128, 512])
        la = cTr[:, :].rearrange("p (a b) -> p a b", a=4)
        yT = hT
        rstd4 = f32v(hbuf, 8192, [128, 4, 512])
        sq2 = b16v(hbuf, 16384, [128, 8, 512])
        tn = [f32v(scr, 16384, [128, 512]), f32v(scr, 18432, [128, 512])]
        QS = 128.0 ** -0.5
        norm_a(0, b, l, 0)
        for jb in range(NB):
            t0 = jb * TB
            norm_b(jb, b, l, 0, 0, hT)
            QB = (ps[0], ps[1], ps[4], ps[5])
            for kc in range(8):
                mm(ps[2][:], wa[:, kc, :], hT[:, kc, :], start=(kc == 0), stop=(kc == 7))
                for hd in range(4):
                    mm(QB[hd][:], wq[:, kc, hd * 128:(hd + 1) * 128], hT[:, kc, :],
                       start=(kc == 0), stop=(kc == 7))
            memset(alrT, 1.0)
            cp(alrT[0:16, :], ps[2][0:16, :], eng="dve")
            for hd in range(4):
                act(qT[:, hd, :], QB[hd][:], AF.Copy, scale=QS)
            for tt_ in range(4):
                pz = ps[2 + tt_ % 2]
                mm(pz[:], alrT[0:17, tt_ * 128:(tt_ + 1) * 128], wgu[0:17, :])
                ts(m4[:, tt_, :], pz[:], 0.0, None, ALU.min)
                stt(u4[:, tt_, :], m4[:, tt_, :], 2.0, pz[:], ALU.mult, ALU.subtract)
            act(u4[:, :, :], u4[:, :, :], AF.Exp)
            act(u4[:, :, :], u4[:, :, :], AF.Ln, bias=1.0)
            tt(la[:, :, :], m4[:, :, :], u4[:, :, :], ALU.subtract)
            for gc in range(8):
                pb = ps[gc % 2]
                for kc in range(8):
                    mm(pb[:], wg[:, kc, gc * 128:(gc + 1) * 128], hT[:, kc, :], start=(kc == 0), stop=(kc == 7))
                act(sgT[:, gc, :], pb[:], AF.Silu)
            for tt_ in range(4):
                mm(ps[3][:], mtri[:], la[:, tt_, :])
                for hd in range(4):
                    mm(ps[7][:, hd * 2:hd * 2 + 2], la[:, tt_, hd * 128:(hd + 1) * 128], ind[:])
                for kc in range(8):
                    mm(ps[4 + tt_ % 2][:], hT[:, kc, tt_ * 128:(tt_ + 1) * 128], wk[:, kc, :],
                       start=(kc == 0), stop=(kc == 7))
                act(expD, ps[3][:], AF.Exp)
                n0 = jb * 8 + tt_ * 2
                act(gam[:, :, n0:n0 + 2], ps[7][:, 0:8].rearrange("p (a b) -> p a b", b=2), AF.Exp)
                tt(kdec[:, tt_, :], ps[4 + tt_ % 2][:], expD, ALU.mult)
                for vh in range(2):
                    pb = ps[vh]
                    for kc in range(8):
                        mm(pb[:], hT[:, kc, tt_ * 128:(tt_ + 1) * 128], wv[:, kc, vh * 512:(vh + 1) * 512],
                           start=(kc == 0), stop=(kc == 7))
                    act(vv[:, tt_, vh * 512:(vh + 1) * 512], pb[:], AF.Copy)
            if jb == NB - 1 and after_proj is not None:
                after_proj()
            def kv_mm(n):
                tt_ = n // 2
                p0 = (n % 2) * 64
                kvb = (ps[0], ps[1]) if n % 2 == 0 else (ps[4], ps[5])
                for hd in range(4):
                    mm(kvb[hd // 2][:, (hd % 2) * 256:(hd % 2 + 1) * 256],
                       kdec[p0:p0 + 64, tt_, hd * 128:(hd + 1) * 128],
                       vv[p0:p0 + 64, tt_, hd * 256:(hd + 1) * 256])
            kv_mm(0)
            for n in range(8):
                ng = jb * 8 + n
                kvb = (ps[0], ps[1]) if n % 2 == 0 else (ps[4], ps[5])
                if n + 1 < 8:
                    kv_mm(n + 1)
                for hd in range(4):
                    stt(Sst[:, hd, :], Sst[:, hd, :], gam[:, hd, ng:ng + 1],
                        kvb[hd // 2][:, (hd % 2) * 256:(hd % 2 + 1) * 256], ALU.mult, ALU.add)
                    act(Sb[:, hd, :], Sst[:, hd, :], AF.Copy)
                    for dvc in range(2):
                        ch = hd * 2 + dvc
                        mm(ps[2 + n % 2][:, ch * 64:(ch + 1) * 64], Sb[:, hd, dvc * 128:(dvc + 1) * 128],
                           qT[:, hd, n * 64:(n + 1) * 64])
                if n >= 1:
                    cp(oT[:, :, (n - 1) * 64:n * 64], ps[2 + (n - 1) % 2][:].rearrange("p (a b) -> p a b", a=8),
                       eng="dve")
            cp(oT[:, :, 7 * 64:8 * 64], ps[2 + 7 % 2][:].rearrange("p (a b) -> p a b", a=8), eng="dve")
            for hd in range(4):
                act(sq2[:, 2 * hd:2 * hd + 2, :], oT[:, 2 * hd:2 * hd + 2, :], AF.Square)
                pn = ps[6 + hd % 2]
                for dvc in range(2):
                    mm(pn[:], ones_v[:], sq2[:, hd * 2 + dvc, :], start=(dvc == 0), stop=(dvc == 1))
                act(rstd4[:, hd, :], pn[:], AF.Ln, bias=EPS)
                act(rstd4[:, hd, :], rstd4[:, hd, :], AF.Exp, scale=-0.5)
            for hd in range(4):
                for dvc in range(2):
                    ch = hd * 2 + dvc
                    stt(tn[dvc], oT[:, ch, :], vcol(V_GNG + dvc), rstd4[:, hd, :], ALU.mult, ALU.mult)
                    tt(yT[:, ch, :], tn[dvc], sgT[:, ch, :], ALU.mult)
            if jb + 1 < NB:
                norm_a0(jb + 1)
            for m in range(8):
                if m == 4 and jb + 1 < NB:
                    norm_a(jb + 1, b, l, 0, do_sq=False)
                pb = ps[m % 2]
                for kc in range(8):
                    mm(pb[:], wout[:, kc, m * 128:(m + 1) * 128], yT[:, kc, :], start=(kc == 0), stop=(kc == 7))
                resid_update(pb[:], m, jb, ada_col(l, 2, m, b))

    def lru_views():
        return dict(
            wout=b16v(W, 0, [128, 8, 1024]),
            wgt=b16v(W, 24576, [128, 8, 1024]),
            wx=b16v(W, 40960, [128, 8, 1024]),
            wr=b16v(W, 57344, [128, 8, 256]),
            wi=b16v(W, 61440, [128, 8, 256]),
        )

    def lru_prefetch():
        v = lru_views()
        src = lru_w_in_d.rearrange("(c p) n -> p c n", p=128)
        dma("pool", v["wgt"], src[:, :, 0:1024])
        dma("pool", v["wx"], src[:, :, 1024:2048])
        dma("pool", v["wr"], lru_wr_d.rearrange("p (a b) -> p a b", a=8))
        dma("pool", v["wi"], lru_wi_d.rearrange("p (a b) -> p a b", a=8))

    hbr = smallw[:, 32:40]
    hbi = smallw[:, 40:48]
    cnh = smallw[:, 48:56]
    ts(hbr, vecs[:, V_BR:V_BR + 8], 0.5, None, ALU.mult)
    ts(hbi, vecs[:, V_BI:V_BI + 8], 0.5, None, ALU.mult)
    ts(cnh, cneg, 0.5, None, ALU.mult)

    def lru_layer(b, l=1, after_gate=None):
        V = lru_views()
        wout, wgt, wx, wr, wi = (V[k] for k in ("wout", "wgt", "wx", "wr", "wi"))
        dma("pool", wout, lru_w_out_d.rearrange("(c p) n -> p c n", p=128))
        hT = b16v(hbuf, 0, [128, 8, 512])
        yT = b16v(hbuf, 8192, [128, 8, 512])
        xb2 = [f32v(hbuf, 16384, [128, 2, 516]), f32v(hbuf, 27136, [128, 2, 516])]
        xc2 = [f32v(hbuf, 20992, [128, 2, 512]), cTr[:, 0:1024].rearrange("p (a b) -> p a b", a=2)]
        xcb = [b16v(hbuf, 25088, [128, 2, 512]),
               cTr[:, 1024:1536].bitcast(BF16).rearrange("p (a b) -> p a b", a=2)]
        TR = [f32v(scr, 0, [128, 2, 512]), f32v(W, 16384, [128, 2, 512])]
        TI = [f32v(scr, 4096, [128, 2, 512]), f32v(W, 20480, [128, 2, 512])]
        AA = [f32v(scr, 8192, [128, 2, 512]), f32v(W, 65536, [128, 2, 512])]
        MM = [f32v(scr, 12288, [128, 2, 512]), f32v(W, 69632, [128, 2, 512])]
        memset(halo[:], 0.0)
        memset(carry[:], 0.0)
        norm_a(0, b, l, 0)
        for jb in range(NB):
            t0 = jb * TB
            norm_b(jb, b, l, 0, 0, hT)
            def Gmm(hb):
                for cc in range(2):
                    c = hb * 2 + cc
                    for kc in range(8):
                        mm(ps[cc][:], wgt[:, kc, c * 128:(c + 1) * 128], hT[:, kc, :],
                           start=(kc == 0), stop=(kc == 7))

            def A1mm(hb):
                for cc in range(2):
                    c = hb * 2 + cc
                    pb = ps[2 + cc]
                    for kc in range(8):
                        mm(pb[:], wx[:, kc, c * 128:(c + 1) * 128], hT[:, kc, :], start=(kc == 0), stop=(kc == 7))

            def A1(hb):
                X, XC, XB = xb2[hb % 2], xc2[hb % 2], xcb[hb % 2]
                for cc in range(2):
                    c = hb * 2 + cc
                    act(X[:, cc, 0:3], halo[:, c, 0:3], AF.Copy)
                for cc in range(2):
                    act(X[:, cc, 3:515], ps[2 + cc][:], AF.Copy)
                for cc in range(2):
                    c = hb * 2 + cc
                    cp(halo[:, c, 0:3], X[:, cc, 512:515], eng="dve")
                    ts(XC[:, cc, :], X[:, cc, 0:512], vcol(V_CW + 0 * 8 + c), vcol(V_CB + c), ALU.mult, ALU.add)
                    for j in range(1, 4):
                        stt(XC[:, cc, :], X[:, cc, j:j + 512], vcol(V_CW + j * 8 + c), XC[:, cc, :],
                            ALU.mult, ALU.add)

            def A2(hb):
                X, XC, XB = xb2[hb % 2], xc2[hb % 2], xcb[hb % 2]
                for cc in range(2):
                    cp(XB[:, cc, :], XC[:, cc, :], eng="dve")
                for oc in range(2):
                    for kk in range(2):
                        mm(ps[4 + 2 * oc][:], wr[:, hb * 2 + kk, oc * 128:(oc + 1) * 128], XB[:, kk, :],
                           start=(kk == 0), stop=(kk == 1))
                    for kk in range(2):
                        mm(ps[5 + 2 * oc][:], wi[:, hb * 2 + kk, oc * 128:(oc + 1) * 128], XB[:, kk, :],
                           start=(kk == 0), stop=(kk == 1))

            def B1(hb):
                t_r, t_i, a_, m_ = TR[hb % 2], TI[hb % 2], AA[hb % 2], MM[hb % 2]
                for cc in range(2):
                    act(yT[:, hb * 2 + cc, :], ps[cc][:], AF.Gelu_apprx_tanh)
                for oc in range(2):
                    c = hb * 2 + oc
                    act(t_r[:, oc, :], ps[4 + 2 * oc][:], AF.Tanh, bias=hbr[:, c:c + 1], scale=0.5)
                    act(t_i[:, oc, :], ps[5 + 2 * oc][:], AF.Tanh, bias=hbi[:, c:c + 1], scale=0.5)
                for oc in range(2):
                    c = hb * 2 + oc
                    act(a_[:, oc, :], t_r[:, oc, :], AF.Exp, bias=cnh[:, c:c + 1], scale=cnh[:, c:c + 1])
                    act(m_[:, oc, :], t_r[:, oc, :], AF.Exp, bias=cneg[:, c:c + 1], scale=cneg[:, c:c + 1])
                for oc in range(2):
                    act(m_[:, oc, :], m_[:, oc, :], AF.Sqrt, bias=0.25, scale=-0.25)

            def B2(hb):
                t_r, t_i, a_, m_ = TR[hb % 2], TI[hb % 2], AA[hb % 2], MM[hb % 2]
                hs = t_r
                XC = xc2[hb % 2]
                for oc in range(2):
                    c = hb * 2 + oc
                    stt(t_i[:, oc, :], t_i[:, oc, :], 1.0, XC[:, oc, :], ALU.add, ALU.mult)
                    tt(m_[:, oc, :], m_[:, oc, :], t_i[:, oc, :], ALU.mult)
                    scan(hs[:, oc, :], a_[:, oc, :], m_[:, oc, :], carry[:, c:c + 1], ALU.mult, ALU.add)
                    cp(carry[:, c:c + 1], hs[:, oc, 511:512], eng="dve")
                    tt(yT[:, c, :], hs[:, oc, :], yT[:, c, :], ALU.mult)

            for kc in range(8):
                for cc in range(2):
                    mm(ps[cc][:], wgt[:, kc, cc * 128:(cc + 1) * 128], hT[:, kc, :], start=(kc == 0), stop=(kc == 7))
                    mm(ps[2 + cc][:], wx[:, kc, cc * 128:(cc + 1) * 128], hT[:, kc, :],
                       start=(kc == 0), stop=(kc == 7))
            A1(0); A1mm(1); A2(0)
            for hb in range(4):
                if hb + 1 < 4:
                    A1(hb + 1)
                if hb + 2 < 4:
                    A1mm(hb + 2)
                B1(hb)
                if hb + 1 < 4:
                    Gmm(hb + 1)
                    A2(hb + 1)
                elif jb == NB - 1 and after_gate is not None:
                    after_gate()
                B2(hb)
            if jb + 1 < NB:
                norm_a0(jb + 1)
            for m in range(8):
                if m == 4 and jb + 1 < NB:
                    norm_a(jb + 1, b, l, 0, do_sq=False)
                pb = ps[m % 2]
                for kc in range(8):
                    mm(pb[:], wout[:, kc, m * 128:(m + 1) * 128], yT[:, kc, :], start=(kc == 0), stop=(kc == 7))
                resid_update(pb[:], m, jb, ada_col(l, 2, m, b))

    def moe_wviews(slot):
        base = slot * 24576
        return (b16v(W, base, [128, 8, 512]), b16v(W, base + 8192, [128, 8, 512]),
                b16v(W, base + 16384, [128, 4, 1024]))

    moe_state = {"slot": 0}

    def moe_load(l, e, slot, parts=("g", "u", "d")):
        wg, wu, wd = moe_wviews(slot)
        if "g" in parts:
            dma("pool", wg, wg_d[l, e].rearrange("(c p) n -> p c n", p=128))
        if "u" in parts:
            dma("pool", wu, wu_d[l, e].rearrange("(c p) n -> p c n", p=128))
        if "d" in parts:
            dma("pool", wd, wd_d[l, e].rearrange("(c p) n -> p c n", p=128))

    SLOTS = [1, 2, 0, 1, 2, 0, 1, 2, 0, 1, 2, 0, 1, 2, 1, 0]

    def moe_layer(b, l, pre_last=None):
        hT = b16v(hbuf, 0, [128, 8, S])
        for jb in range(NB):
            norm_block(jb, b, l, 1, 3, hT[:, :, jb * TB:(jb + 1) * TB])
        for t in range(16):
            for kc in range(8):
                mm(ps[6][:, t * 16:(t + 1) * 16], hT[:, kc, t * 128:(t + 1) * 128], rw[:, kc, :],
                   start=(kc == 0), stop=(kc == 7))
        R = lambda i: rt[:, i * 256:(i + 1) * 256]
        R3 = lambda i: rt[:, i * 256:(i + 1) * 256].rearrange("p (t e) -> p t e", e=16)
        R4 = lambda i: rt[:, i * 256:(i + 1) * 256].rearrange("p (t g j) -> p t g j", g=4, j=4)
        G = lambda i: rt[:, 2304 + i * 64:2304 + (i + 1) * 64]
        G3 = lambda i: rt[:, 2304 + i * 64:2304 + (i + 1) * 64].rearrange("p (t g) -> p t g", g=4)
        sc_, sel_, msk, tmp_ = 0, 1, 2, 3
        act(R(sc_), ps[6][:, 0:256], AF.Sigmoid)
        tt(R3(sel_), R3(sc_), rb[:, :].unsqueeze(1).to_broadcast([128, 16, 16]), ALU.add)
        red(G(0), R4(sel_), ALU.max)
        tt(R4(msk), R4(sel_), G3(0).unsqueeze(3).to_broadcast([128, 16, 4, 4]), ALU.is_ge)
        stt(R(tmp_), R(msk), -1e30, R(sel_), ALU.mult, ALU.add)
        red(G(1), R4(tmp_), ALU.max)
        tt(G(2), G(0), G(1), ALU.add)
        red(rt[:, 1664:1680], G3(2), ALU.max)
        tt(G3(3), G3(2), rt[:, 1664:1680].unsqueeze(2).to_broadcast([128, 16, 4]), ALU.is_ge)
        tt(R4(msk), R4(sel_), G3(1).unsqueeze(3).to_broadcast([128, 16, 4, 4]), ALU.is_ge)
        tt(R4(msk), R4(msk), G3(3).unsqueeze(3).to_broadcast([128, 16, 4, 4]), ALU.mult)
        tt(R(tmp_), R(msk), R(sc_), ALU.mult)
        red(rt[:, 1680:1696], R3(tmp_), ALU.add)
        recip(rt[:, 1680:1696], rt[:, 1680:1696])
        tt(R3(tmp_), R3(tmp_), rt[:, 1680:1696].unsqueeze(2).to_broadcast([128, 16, 16]), ALU.mult)
        pk = rt[:, 1024:1536].rearrange("p (t e) -> p t e", e=32)
        hib = rt[:, 1536:1664].bitcast(BF16).rearrange("p (t e) -> p t e", e=16)
        cp(hib, R3(tmp_), eng="dve")
        cp(pk[:, :, 0:16], hib, eng="dve")
        tt(pk[:, :, 16:32], R3(tmp_), pk[:, :, 0:16], ALU.subtract)
        combT = cTr[0:32, 0:1024].bitcast(BF16)
        for t4 in range(4):
            for q in range(4):
                t = t4 * 4 + q
                tr(ps[7][0:32, q * 128:(q + 1) * 128], pk[:, t, :], ident[:])
            act(combT[:, t4 * 512:(t4 + 1) * 512], ps[7][0:32, :], AF.Copy)
        if "comb" in dbg_d and b == dbg.get("_b", 0) and l == dbg.get("_l", 0):
            dma("sp", dbg_d["comb"], combT)
        he = [b16v(scr, 0, [128, 4, 512]), b16v(scr, 4096, [128, 4, 512])]
        sg = [f32v(scr, 8192, [128, 512]), f32v(scr, 10240, [128, 512])]
        tb = [f32v(scr, 12288, [128, 512]), f32v(scr, 14336, [128, 512])]
        cbs = [f32v(scr, 16384, [128, 512]), f32v(scr, 18432, [128, 512])]
        units = [(e, jb) for e in range(NE) for jb in range(NB)]
        DB = (4, 5, 7)

        def cb_and_gu0(i):
            e, jb = units[i]
            t0 = jb * TB
            wg, wu, wd = moe_wviews(SLOTS[e])
            mm(ps[6][:], i32b[:, e:e + 1].to_broadcast([32, 128]), combT[:, t0:t0 + TB])
            act(cbs[i % 2], ps[6][:], AF.Copy)
            gu_mm(i, 0)

        def gu_mm(i, c):
            e, jb = units[i]
            t0 = jb * TB
            wg, wu, wd = moe_wviews(SLOTS[e])
            pg = ps[0 + c % 2]
            pu = ps[2 + c % 2]
            for kc in range(8):
                mm(pg[:], wg[:, kc, c * 128:(c + 1) * 128], hT[:, kc, t0:t0 + TB],
                   start=(kc == 0), stop=(kc == 7))
            for kc in range(8):
                mm(pu[:], wu[:, kc, c * 128:(c + 1) * 128], hT[:, kc, t0:t0 + TB],
                   start=(kc == 0), stop=(kc == 7))

        def load_hook(i):
            e, jb = units[i]
            if jb == 0:
                if e + 1 < NE:
                    moe_load(l, e + 1, SLOTS[e + 1])
                elif pre_last is not None:
                    pre_last()

        load_hook(0)
        cb_and_gu0(0)
        for i, (e, jb) in enumerate(units):
            wg, wu, wd = moe_wviews(SLOTS[e])
            k2 = i % 2
            for c in range(4):
                if c >= 1:
                    gu_mm(i, c)
                pg = ps[0 + c % 2]
                pu = ps[2 + c % 2]
                act(sg[c % 2], pg[:], AF.Silu)
                tt(tb[c % 2], pu[:], sg[c % 2], ALU.mult)
                tt(he[k2][:, c, :], tb[c % 2], cbs[k2], ALU.mult)
            if i + 1 < len(units):
                cb_and_gu0(i + 1)
            for m in range(8):
                pd = ps[DB[m % 3]]
                for kc in range(4):
                    mm(pd[:], wd[:, kc, m * 128:(m + 1) * 128], he[k2][:, kc, :],
                       start=(kc == 0), stop=(kc == 3))
                resid_update(pd[:], m, jb, ada_col(l, 5, m, b))
            if i + 1 < len(units):
                load_hook(i + 1)

    def final_store(b, si):
        sq = b16v(scr, 0, [128, 8, 512])
        rstd = f32v(scr, 8192, [128, 512])
        ob = [f32v(hbuf, 0, [128, 8, 512]), f32v(hbuf, 16384, [128, 8, 512])]
        outs = []
        for jb in range(NB):
            t0 = jb * TB
            act(sq[:, :, :], xT[:, :, t0:t0 + TB], AF.Square)
            for c in range(8):
                mm(ps[6][:], ones_d[:], sq[:, c, :], start=(c == 0), stop=(c == 7))
            act(rstd, ps[6][:], AF.Ln, bias=EPS)
            act(rstd, rstd, AF.Exp, scale=-0.5)
            o = ob[jb % 2]
            for c in range(8):
                stt(o[:, c, :], xT[:, c, t0:t0 + TB], vcol(V_FNG + c), rstd, ALU.mult, ALU.mult)
            outs.append(dma("sp", outT_d[si, :, t0:t0 + TB].rearrange("(c p) t -> p c t", p=128), o))
        return outs

    out_ops = []
    phases = dbg.get("_phases", ("gla", "moe0", "lru", "moe1", "final"))
    for si in range(n_seq):
        b = si
        for jb in range(NB):
            dma("sp", xT[:, :, jb * TB:(jb + 1) * TB],
                xT_d[si, :, jb * TB:(jb + 1) * TB].rearrange("(c p) t -> p c t", p=128))
        if si == 0 and "gla" in phases:
            gla_prefetch()
        if "gla" in phases:
            gla_layer(b, after_proj=(lambda: moe_load(0, 0, SLOTS[0])) if "moe0" in phases else None)
        if "xmid0" in dbg_d and si == dbg.get("_b", 0):
            out_ops.append(dma("sp", dbg_d["xmid0"].rearrange("(c p) t -> p c t", p=128), xT[:, :, :]))
        if "moe0" in phases:
            if "gla" not in phases:
                moe_load(0, 0, SLOTS[0])
            moe_layer(b, 0, pre_last=lru_prefetch if "lru" in phases else None)
        if "xmid1" in dbg_d and si == dbg.get("_b", 0):
            out_ops.append(dma("sp", dbg_d["xmid1"].rearrange("(c p) t -> p c t", p=128), xT[:, :, :]))
        if "lru" in phases:
            if "moe0" not in phases:
                lru_prefetch()
            lru_layer(b, after_gate=(lambda: moe_load(1, 0, SLOTS[0], ("g", "u"))) if "moe1" in phases else None)
            if "moe1" in phases:
                moe_load(1, 0, SLOTS[0], ("d",))
        if "xmid2" in dbg_d and si == dbg.get("_b", 0):
            out_ops.append(dma("sp", dbg_d["xmid2"].rearrange("(c p) t -> p c t", p=128), xT[:, :, :]))
        if "moe1" in phases:
            if "lru" not in phases:
                moe_load(1, 0, SLOTS[0])
            moe_layer(b, 1, pre_last=gla_prefetch if (si + 1 < n_seq and "gla" in phases) else None)
        if "final" in phases:
            out_ops += final_store(b, si)

    Sd.add("sp", lambda e: e.nop(), reads=[], writes=[]).deps.extend(
        [o for o in Sd.dma_ops["sp"]])

    Sd.finalize()

    sem_stack = contextlib.ExitStack()
    csem = {e: sem_stack.enter_context(nc.semaphore("c_" + e)) for e in Sched.ENGS}
    dsems = {e: [sem_stack.enter_context(nc.semaphore("d_%s_%d" % (e, i))) for i in range(Sched.NDS)]
             for e in ("sp", "pool")}
    with nc.Block() as block:
        @block.tensor
        def _(e):
            Sd.emit("pe", e, csem, dsems)

        @block.scalar
        def _(e):
            Sd.emit("act", e, csem, dsems)

        @block.vector
        def _(e):
            Sd.emit("dve", e, csem, dsems)

        @block.gpsimd
        def _(e):
            Sd.emit("pool", e, csem, dsems)

        @block.sync
        def _(e):
            Sd.emit("sp", e, csem, dsems)
    sem_stack.close()
    stack.close()
    return nc, Sd


V_NMG = 0
V_NFG = 16
V_FNG = 32
V_GNG = 40
V_CW = 42
V_CB = 74
V_BR = 82
V_BI = 90
V_LAM = 98
NV = 106


def _fm(v):
    v = np.asarray(v, np.float32).reshape(-1, 128)
    return np.ascontiguousarray(v.T)


def prep_inputs(inp):
    f = lambda a: np.ascontiguousarray(np.asarray(a, dtype=np.float32))
    x = f(inp["x"])
    c = f(inp["c"])
    vec = np.zeros((128, NV), np.float32)
    for l in range(2):
        vec[:, V_NMG + l * 8:V_NMG + (l + 1) * 8] = _fm(inp["norm_mix_g"][l])
        vec[:, V_NFG + l * 8:V_NFG + (l + 1) * 8] = _fm(inp["norm_ffn_g"][l])
    vec[:, V_FNG:V_FNG + 8] = _fm(inp["final_norm_g"])
    vec[:, V_GNG:V_GNG + 2] = _fm(inp["gla_norm_g"][0])
    for j in range(4):
        vec[:, V_CW + j * 8:V_CW + (j + 1) * 8] = _fm(inp["lru_conv_w"][0, j])
    vec[:, V_CB:V_CB + 8] = _fm(inp["lru_conv_b"][0])
    vec[:, V_BR:V_BR + 8] = _fm(np.asarray(inp["lru_b_r"][0]).reshape(-1))
    vec[:, V_BI:V_BI + 8] = _fm(np.asarray(inp["lru_b_i"][0]).reshape(-1))
    vec[:, V_LAM:V_LAM + 8] = _fm(inp["lru_lambda"][0])
    ada_b = np.concatenate([_fm(inp["ada_b"][0]), _fm(inp["ada_b"][1])], axis=1)
    wgu = np.concatenate([f(inp["gla_w_gate_up"][0]), f(inp["gla_b_gate"][0])[None, :]], axis=0)

    def blk(w):
        w = f(w).reshape(4, 2, 128, 256).transpose(2, 0, 1, 3)
        return np.ascontiguousarray(w).reshape(128, 8 * 256)

    rwl = f(inp["router_w"]).reshape(8, 128, 16).transpose(1, 0, 2).reshape(128, 128)
    shared = {
        "ada_w": f(inp["ada_w"]), "ada_b": ada_b, "vecs": vec,
        "gla_w_in": f(inp["gla_w_in"][0]), "gla_w_out": f(inp["gla_w_out"][0]), "wgu": wgu,
        "lru_w_in": f(inp["lru_w_in"][0]), "lru_w_out": f(inp["lru_w_out"][0]),
        "lru_wr": blk(inp["lru_w_r"][0]), "lru_wi": blk(inp["lru_w_i"][0]),
        "rw": np.ascontiguousarray(rwl), "rb": np.ascontiguousarray(np.tile(f(inp["router_bias"])[None, :], (128, 1))),
        "moe_wg": f(inp["moe_w_gate"]), "moe_wu": f(inp["moe_w_up"]), "moe_wd": f(inp["moe_w_down"]),
        "ident": np.eye(128, dtype=np.float32),
    }
    s_i = np.arange(128)[:, None]
    c_i = np.arange(128)[None, :]
    shared["mtri"] = (((s_i > c_i) & (s_i // 64 == c_i // 64)).astype(np.float32) / 16.0)
    shared["ind"] = ((s_i // 64) == np.arange(2)[None, :]).astype(np.float32) / 16.0
    shared["i16"] = np.eye(16, dtype=np.float32)
    in_maps = []
    for i in range(N_CORES):
        m = dict(shared)
        m["xT"] = np.ascontiguousarray(x[2 * i:2 * i + 2].transpose(0, 2, 1))
        cc = c[2 * i:2 * i + 2]
        m["cT"] = np.ascontiguousarray(cc.reshape(2, 8, 128).transpose(2, 1, 0).reshape(128, 16))
        in_maps.append(m)
    return in_maps


_CACHE = {}


def kernel(**inputs):
    in_maps = prep_inputs(inputs)
    if "nc" not in _CACHE:
        _CACHE["nc"] = build_program()[0]
    nc = _CACHE["nc"]
    res = run_bass_kernel_spmd(nc, in_maps, core_ids=list(range(N_CORES)))
    out = np.empty((16, S, D), np.float32)
    for i in range(N_CORES):
        o = res.results[i]["outT"]
        out[2 * i:2 * i + 2] = o.transpose(0, 2, 1)
    return out
```
